# Optimizing a Trainium2 kernel written in Bass

```python
import math
import jax, jax.numpy as jnp
from jax import lax
import numpy as np

D_MODEL = 1024
BATCH = 4
SEQ = 4096
DEPTH = 1

ATT_HEADS = 8
ATT_KV_HEADS = 2
ATT_HEAD_DIM = 64
ATT_GROUP = ATT_HEADS // ATT_KV_HEADS
ATT_Q_DIM = ATT_HEADS * ATT_HEAD_DIM
ATT_KV_DIM = ATT_KV_HEADS * ATT_HEAD_DIM
WINDOW = 128
ATT_BLOCK = 128
ROPE_DIM = ATT_HEAD_DIM // 4
ROPE_THETA = 500000.0

GLA_HEADS = 4
GLA_KEY_DIM = D_MODEL // 2
GLA_VAL_DIM = D_MODEL
GLA_DK = GLA_KEY_DIM // GLA_HEADS
GLA_DV = GLA_VAL_DIM // GLA_HEADS
GLA_GATE_RANK = 16
GLA_GATE_NORM = 16.0
GLA_CHUNK = 64

N_EXPERTS = 16
EXPERT_FF = D_MODEL
CAPACITY_FACTOR = 2

PLE_DIM = 256

EPS = 1e-6

IN_SIZES = (ATT_Q_DIM, ATT_KV_DIM, ATT_KV_DIM,
            GLA_KEY_DIM, GLA_KEY_DIM, GLA_VAL_DIM, GLA_VAL_DIM,
            GLA_GATE_RANK, GLA_GATE_RANK, D_MODEL, D_MODEL)
IN_DIM = ATT_Q_DIM + 2 * ATT_KV_DIM + 2 * GLA_KEY_DIM + 2 * GLA_VAL_DIM + 2 * GLA_GATE_RANK + 2 * D_MODEL

kernel_name = "hybrid_swa_gla_ecmoe_block"


def rmsnorm(x, g):
    xf = x.astype(jnp.float32)
    y = xf * lax.rsqrt(jnp.mean(xf * xf, axis=-1, keepdims=True) + EPS)
    return (y * g.astype(jnp.float32)).astype(x.dtype)


def split_columns(proj):
    parts, start = [], 0
    for size in IN_SIZES:
        parts.append(proj[..., start:start + size])
        start += size
    return parts


def partial_rope(t, positions):
    half = ROPE_DIM // 2
    inv_freq = ROPE_THETA ** (-jnp.arange(0, ROPE_DIM, 2, dtype=jnp.float32) / ROPE_DIM)
    ang = positions.astype(jnp.float32)[..., None] * inv_freq
    cos = jnp.cos(ang)[:, :, None, :].astype(t.dtype)
    sin = jnp.sin(ang)[:, :, None, :].astype(t.dtype)
    t1 = t[..., :half]
    t2 = t[..., half:ROPE_DIM]
    rot = jnp.concatenate([t1 * cos - t2 * sin, t2 * cos + t1 * sin], axis=-1)
    return jnp.concatenate([rot, t[..., ROPE_DIM:]], axis=-1)


def windowed_gqa(q, k, v, sink):
    B, S = q.shape[0], q.shape[1]
    nb = S // ATT_BLOCK
    qb = q.reshape(B, nb, ATT_BLOCK, ATT_KV_HEADS, ATT_GROUP, ATT_HEAD_DIM)

    def band(t):
        tp = jnp.pad(t, ((0, 0), (ATT_BLOCK, ATT_BLOCK), (0, 0), (0, 0)))
        tp = tp.reshape(B, nb + 2, ATT_BLOCK, ATT_KV_HEADS, ATT_HEAD_DIM)
        return jnp.concatenate([tp[:, :-2], tp[:, 1:-1], tp[:, 2:]], axis=2)

    kb, vb = band(k), band(v)
    scores = jnp.einsum('bnqhgd,bnkhd->bnhgqk', qb, kb).astype(jnp.float32) * (ATT_HEAD_DIM ** -0.5)
    blk = jnp.arange(nb)[:, None, None]
    qi = blk * ATT_BLOCK + jnp.arange(ATT_BLOCK)[None, :, None]
    kj = (blk - 1) * ATT_BLOCK + jnp.arange(3 * ATT_BLOCK)[None, None, :]
    valid = (jnp.abs(qi - kj) <= WINDOW) & (kj >= 0) & (kj < S)
    scores = jnp.where(valid[None, :, None, None], scores, -jnp.inf)
    sink_b = sink.astype(jnp.float32).reshape(1, 1, ATT_KV_HEADS, ATT_GROUP, 1, 1)
    m = jnp.maximum(jnp.max(scores, axis=-1, keepdims=True), sink_b)
    e = jnp.exp(scores - m)
    probs = e / (jnp.sum(e, axis=-1, keepdims=True) + jnp.exp(sink_b - m))
    out = jnp.einsum('bnhgqk,bnkhd->bnqhgd', probs.astype(v.dtype), vb)
    return out.reshape(B, S, ATT_Q_DIM)


def gla_chunked(q, k, v, log_a, strict):
    B, S, H, dk = q.shape
    dv = v.shape[-1]
    nc = S // GLA_CHUNK

    def chunk(t):
        return t.reshape(B, nc, GLA_CHUNK, H, t.shape[-1])

    qc, kc, vc, ac = chunk(q), chunk(k), chunk(v), chunk(log_a)
    b = jnp.cumsum(ac, axis=2)
    g = b[:, :, -1]
    q_t = qc * jnp.exp(b)
    k_t = kc * jnp.exp(-b)
    k_end = kc * jnp.exp(g[:, :, None] - b)
    attn = jnp.einsum('bclhd,bcmhd->bchlm', q_t, k_t)
    mask = jnp.tril(jnp.ones((GLA_CHUNK, GLA_CHUNK), dtype=bool), -1 if strict else 0)
    attn = jnp.where(mask, attn, 0.0)
    o_intra = jnp.einsum('bchlm,bcmhe->bclhe', attn, vc)
    kv = jnp.einsum('bclhd,bclhe->bchde', k_end, vc)
    decay = jnp.exp(g)

    def step(state, xs):
        dec, kv_c = xs
        return dec[..., None] * state + kv_c, state

    s0 = jnp.zeros((B, H, dk, dv), dtype=jnp.float32)
    _, s_prev = lax.scan(step, s0, (jnp.moveaxis(decay, 1, 0), jnp.moveaxis(kv, 1, 0)))
    s_prev = jnp.moveaxis(s_prev, 0, 1)
    o_inter = jnp.einsum('bclhd,bchde->bclhe', q_t, s_prev)
    return (o_intra + o_inter).reshape(B, S, H, dv)


def gla_branch(gq, gk, gv, gr, z_f, z_b, up_f, bias_f, up_b, bias_b, norm_gain):
    B, S = gq.shape[0], gq.shape[1]
    f32 = jnp.float32
    q = gq.astype(f32).reshape(B, S, GLA_HEADS, GLA_DK) * (GLA_DK ** -0.5)
    k = gk.astype(f32).reshape(B, S, GLA_HEADS, GLA_DK)
    v = gv.astype(f32).reshape(B, S, GLA_HEADS, GLA_DV)
    la_f = (jax.nn.log_sigmoid(z_f.astype(f32) @ up_f.astype(f32) + bias_f.astype(f32)) / GLA_GATE_NORM).reshape(B, S, GLA_HEADS, GLA_DK)
    la_b = (jax.nn.log_sigmoid(z_b.astype(f32) @ up_b.astype(f32) + bias_b.astype(f32)) / GLA_GATE_NORM).reshape(B, S, GLA_HEADS, GLA_DK)
    o_f = gla_chunked(q, k, v, la_f, strict=False)
    flip = lambda t: jnp.flip(t, axis=1)
    o_b = flip(gla_chunked(flip(q), flip(k), flip(v), flip(la_b), strict=True))
    o = o_f + o_b
    o = o * lax.rsqrt(jnp.mean(o * o, axis=-1, keepdims=True) + EPS) * norm_gain.astype(f32).reshape(GLA_HEADS, GLA_DV)
    o = o.reshape(B, S, GLA_VAL_DIM) * jax.nn.silu(gr.astype(f32))
    return o.astype(gq.dtype)


def expert_choice_ffn(xn, w_router, w_gate, w_up, w_down):
    B, S, D = xn.shape
    cap = CAPACITY_FACTOR * S // N_EXPERTS
    aff = jax.nn.softmax(jnp.einsum('bsd,de->bse', xn, w_router).astype(jnp.float32), axis=-1)
    top_aff, top_idx = lax.top_k(jnp.swapaxes(aff, 1, 2), cap)
    bidx = jnp.arange(B)[:, None, None]
    xg = xn[bidx, top_idx]
    hid = jax.nn.silu(jnp.einsum('becd,edf->becf', xg, w_gate)) * jnp.einsum('becd,edf->becf', xg, w_up)
    y = jnp.einsum('becf,efd->becd', hid, w_down) * top_aff[..., None].astype(xn.dtype)
    return jnp.zeros((B, S, D), dtype=y.dtype).at[bidx, top_idx].add(y)


def setup_inputs(seed: int = 0) -> dict:
    key = jax.random.key(seed)
    ks = jax.random.split(key, 32)
    f32 = jnp.float32
    L, D = DEPTH, D_MODEL

    def w(k, shape, fan_in):
        return jax.random.normal(k, shape, f32) * (fan_in ** -0.5)

    def gain(k, shape):
        return 1.0 + 0.1 * jax.random.normal(k, shape, f32)

    x = jax.random.normal(ks[0], (BATCH, SEQ, D), f32)
    p = jax.random.normal(ks[1], (L, BATCH, SEQ, PLE_DIM), f32)
    offsets = jax.random.randint(ks[2], (BATCH, 1), 0, 1024, dtype=jnp.int32)
    positions = jnp.arange(SEQ, dtype=jnp.int32)[None, :] + offsets
    return {
        "x": x,
        "p": p,
        "positions": positions,
        "norm_mix": gain(ks[3], (L, D)),
        "w_in": w(ks[4], (L, D, IN_DIM), D),
        "gla_gate_up_fwd": w(ks[5], (L, GLA_GATE_RANK, GLA_KEY_DIM), GLA_GATE_RANK),
        "gla_gate_bias_fwd": 1.0 + 0.5 * jax.random.normal(ks[6], (L, GLA_KEY_DIM), f32),
        "gla_gate_up_bwd": w(ks[7], (L, GLA_GATE_RANK, GLA_KEY_DIM), GLA_GATE_RANK),
        "gla_gate_bias_bwd": 1.0 + 0.5 * jax.random.normal(ks[8], (L, GLA_KEY_DIM), f32),
        "attn_sink": 0.5 * jax.random.normal(ks[9], (L, ATT_HEADS), f32),
        "gla_norm": gain(ks[10], (L, GLA_VAL_DIM)),
        "w_branch_attn": w(ks[11], (L, ATT_Q_DIM, D), ATT_Q_DIM),
        "w_branch_gla": w(ks[12], (L, GLA_VAL_DIM, D), GLA_VAL_DIM),
        "w_out": w(ks[13], (L, D, D), D),
        "norm_ffn": gain(ks[14], (L, D)),
        "w_router": w(ks[15], (L, D, N_EXPERTS), D),
        "w_exp_gate": w(ks[16], (L, N_EXPERTS, D, EXPERT_FF), D),
        "w_exp_up": w(ks[17], (L, N_EXPERTS, D, EXPERT_FF), D),
        "w_exp_down": w(ks[18], (L, N_EXPERTS, EXPERT_FF, D), EXPERT_FF),
        "norm_ple": gain(ks[19], (L, D)),
        "w_ple_gate": w(ks[20], (L, D, D), D),
        "w_ple": w(ks[21], (L, PLE_DIM, D), PLE_DIM),
        "norm_final": gain(ks[22], (D,)),
    }


def reference(x, p, positions, norm_mix, w_in, gla_gate_up_fwd, gla_gate_bias_fwd,
              gla_gate_up_bwd, gla_gate_bias_bwd, attn_sink, gla_norm, w_branch_attn,
              w_branch_gla, w_out, norm_ffn, w_router, w_exp_gate, w_exp_up, w_exp_down,
              norm_ple, w_ple_gate, w_ple, norm_final):
    B, S, _ = x.shape
    h = x
    for l in range(DEPTH):
        a = rmsnorm(h, norm_mix[l])
        proj = a @ w_in[l]
        aq, ak, av, gq, gk, gv, gr, z_f, z_b, g_att, g_gla = split_columns(proj)
        aq = partial_rope(aq.reshape(B, S, ATT_HEADS, ATT_HEAD_DIM), positions)
        ak = partial_rope(ak.reshape(B, S, ATT_KV_HEADS, ATT_HEAD_DIM), positions)
        av = av.reshape(B, S, ATT_KV_HEADS, ATT_HEAD_DIM)
        y_att = windowed_gqa(aq, ak, av, attn_sink[l]) @ w_branch_attn[l]
        y_gla = gla_branch(gq, gk, gv, gr, z_f, z_b,
                           gla_gate_up_fwd[l], gla_gate_bias_fwd[l],
                           gla_gate_up_bwd[l], gla_gate_bias_bwd[l], gla_norm[l]) @ w_branch_gla[l]
        merged = jax.nn.sigmoid(g_att) * y_att + jax.nn.sigmoid(g_gla) * y_gla
        h = h + merged @ w_out[l]
        h = h + expert_choice_ffn(rmsnorm(h, norm_ffn[l]), w_router[l],
                                  w_exp_gate[l], w_exp_up[l], w_exp_down[l])
        ple_gate = jax.nn.sigmoid(rmsnorm(h, norm_ple[l]) @ w_ple_gate[l])
        h = h + ple_gate * (p[l] @ w_ple[l])
    return rmsnorm(h, norm_final)
```

```python
import math
import numpy as np
from contextlib import ExitStack
import concourse.bass as bass
import concourse.mybir as mybir
from concourse.bass_utils import run_bass_kernel_spmd

F32 = mybir.dt.float32
BF16 = mybir.dt.bfloat16
I32 = mybir.dt.int32
U32 = mybir.dt.uint32
AF = mybir.ActivationFunctionType
ALU = mybir.AluOpType
AX = mybir.AxisListType

D = 1024
IN_DIM = 5920
EPS = 1e-6
NEXP = 16
C_AQ, C_AK, C_AV, C_GQ, C_GK, C_GV, C_GR, C_ZF, C_ZB, C_GA, C_GG = (
    0, 512, 640, 768, 1280, 1792, 2816, 3840, 3856, 3872, 4896)


class Buf:
    __slots__ = ("name", "w", "r", "dkey", "dram")

    def __init__(self, name, dram=False):
        self.name = name
        self.w = None
        self.r = {}
        self.dkey = None
        self.dram = dram


class Prog:
    ENGS = ("pe", "act", "dve", "pool", "sp")

    def __init__(self, nc, es):
        self.nc = nc
        self.es = es
        self.streams = {e: [] for e in self.ENGS}
        self.cnt = {e: 0 for e in self.ENGS}
        self.known = {e: {} for e in self.ENGS}
        self.dtot = {}
        self.sems = {}
        self.nbuf = 0
        self.ninst = 0
        self.free_keys = []
        self.free_keys_sw = []
        self.phase_owners = []
        for e in self.ENGS:
            self.sem(e)

    def sem(self, key):
        if key not in self.sems:
            self.sems[key] = self.es.enter_context(self.nc.semaphore(f"s_{key}"))
        return self.sems[key]

    def uniq(self, n):
        self.nbuf += 1
        return f"{n}_u{self.nbuf}"

    def buf(self, name=None):
        self.nbuf += 1
        return Buf(name or f"b{self.nbuf}")

    def _deps(self, eng, reads, writes):
        need = {}

        def add(k, v):
            if k == eng and eng == "pe":
                return
            if need.get(k, 0) < v:
                need[k] = v

        for b in reads:
            if b.w is not None:
                add(*b.w)
        for b in writes:
            if b.w is not None:
                add(*b.w)
            for k, v in b.r.items():
                add(k, v)
        waits = []
        kn = self.known[eng]
        for k, v in need.items():
            if k in self.dtot:
                v = self.dtot[k]
            if kn.get(k, 0) >= v:
                continue
            kn[k] = v
            waits.append((k, v))
        return waits

    def _mark(self, ev, reads, writes):
        k, v = ev
        for b in reads:
            if b.r.get(k, 0) < v:
                b.r[k] = v
        for b in writes:
            b.w = ev
            b.r = {}

    def op(self, eng, fn, r=(), w=()):
        waits = self._deps(eng, r, w)
        self.cnt[eng] += 1
        ev = (eng, self.cnt[eng])
        self.streams[eng].append((waits, fn, (eng, 1)))
        self._mark(ev, r, w)
        self.ninst += 1

    def dma_fn(self, q, fn, r=(), w=(), owner=None, inc=16):
        if owner is None:
            cands = [b for b in w if not b.dram] or [b for b in r if not b.dram]
            owner = cands[0]
        if owner.dkey is None:
            fk = self.free_keys_sw if q == "pool" else self.free_keys
            if fk:
                owner.dkey = fk.pop()
            else:
                owner.dkey = f"d{len(self.dtot)}"
                self.dtot[owner.dkey] = 0
                self.sem(owner.dkey)
            self.phase_owners.append((owner, q == "pool"))
        waits = self._deps(q, r, w)
        self.dtot[owner.dkey] += inc
        ev = (owner.dkey, self.dtot[owner.dkey])
        self.streams[q].append((waits, fn, (owner.dkey, inc)))
        self._mark(ev, r, w)
        self.ninst += 1

    def barrier(self):
        for e in self.ENGS:
            waits = []
            kn = self.known[e]
            for k in self.ENGS:
                v = self.cnt[k]
                if k == e or v == 0 or kn.get(k, 0) >= v:
                    continue
                kn[k] = v
                waits.append((k, v))
            for k, v in self.dtot.items():
                if v == 0 or kn.get(k, 0) >= v:
                    continue
                kn[k] = v
                waits.append((k, v))
            if waits:
                self.streams[e].append((waits, None, None))

    def end_phase(self):
        for o, sw in self.phase_owners:
            (self.free_keys_sw if sw else self.free_keys).append(o.dkey)
            o.dkey = None
        self.phase_owners = []

    def emit(self):
        nc = self.nc
        streams = self.streams
        sems = self.sems

        def run(name):
            def f(e):
                for waits, fn, inc in streams[name]:
                    for k, v in waits:
                        e.wait_ge(sems[k], v)
                    if fn is None:
                        continue
                    ins = fn(e)
                    if inc is not None:
                        ins.then_inc(sems[inc[0]], inc[1])
            return f

        with nc.Block() as block:
            block.tensor(run("pe"))
            block.scalar(run("act"))
            block.vector(run("dve"))
            block.gpsimd(run("pool"))
            block.sync(run("sp"))
        self.streams = {e: [] for e in self.ENGS}

    def dma(self, q, out, in_, r=(), w=(), owner=None):
        self.dma_fn(q, lambda e: e.dma_start(out=out, in_=in_), r, w, owner)

    def matmul(self, out, lhsT, rhs, start=True, stop=True, r=(), w=()):
        self.op("pe", lambda e: e.matmul(out, lhsT=lhsT, rhs=rhs, start=start, stop=stop), r, w)

    def transpose(self, out, in_, ident, r=(), w=()):
        self.op("pe", lambda e: e.transpose(out, in_, ident), r, w)

    def act(self, out, in_, func, r=(), w=(), bias=None, scale=None, accum_out=None):
        kw = {}
        if bias is not None:
            kw["bias"] = bias
        if scale is not None:
            kw["scale"] = scale
        if accum_out is not None:
            kw["accum_out"] = accum_out
        self.op("act", lambda e: e.activation(out=out, in_=in_, func=func, **kw), r, w)

    def tt(self, eng, out, in0, in1, op, r=(), w=()):
        self.op(eng, lambda e: e.tensor_tensor(out=out, in0=in0, in1=in1, op=op), r, w)

    def ts(self, eng, out, in0, s1, s2=None, op0=ALU.mult, op1=None, r=(), w=()):
        if op1 is None:
            self.op(eng, lambda e: e.tensor_scalar(out=out, in0=in0, scalar1=s1, scalar2=None, op0=op0), r, w)
        else:
            self.op(eng, lambda e: e.tensor_scalar(out=out, in0=in0, scalar1=s1, scalar2=s2, op0=op0, op1=op1), r, w)

    def stt(self, out, in0, scalar, in1, op0, op1, r=(), w=()):
        self.op("dve", lambda e: e.scalar_tensor_tensor(out=out, in0=in0, scalar=scalar, in1=in1, op0=op0, op1=op1), r, w)

    def copy(self, eng, out, in_, r=(), w=()):
        if eng == "act":
            self.op("act", lambda e: e.activation(out=out, in_=in_, func=AF.Copy), r, w)
        else:
            self.op(eng, lambda e: e.tensor_copy(out=out, in_=in_), r, w)

    def memset(self, eng, ap, val, w=()):
        self.op(eng, lambda e: e.memset(ap, val), (), w)


class TPool:
    def __init__(self, P, ph, nc, name, shape, dtype, n, psum=False):
        self.P = P
        self.items = []
        P.nbuf += 1
        name = f"{name}_u{P.nbuf}_"
        for i in range(n):
            if psum:
                t = ph.enter_context(nc.psum_tensor(f"{name}{i}", shape, dtype))
            else:
                t = ph.enter_context(nc.sbuf_tensor(f"{name}{i}", shape, dtype))
            self.items.append((t, P.buf(f"{name}{i}")))
        self.i = 0
        self.owner = P.buf(name + "_own")

    def next(self):
        t = self.items[self.i % len(self.items)]
        self.i += 1
        return t


NCOL = 4000
C_AQ, C_AK, C_AV, C_GQ, C_GK, C_GV, C_GR, C_ZF, C_ZB, C_GA, C_GG = (
    0, 256, 320, 384, 640, 896, 1408, 1920, 1936, 1952, 2976)
NEL = 8


def run_pipelined(gens, gap):
    active = []
    it = iter(gens)
    rnd = 0
    exhausted = False
    while True:
        if not exhausted and rnd % gap == 0:
            g = next(it, None)
            if g is None:
                exhausted = True
            else:
                active.append(g)
        if exhausted and not active:
            break
        for g in list(active):
            try:
                next(g)
            except StopIteration:
                active.remove(g)
        rnd += 1
GROUPS = [[0, 1], [2, 3], [4, 5], [6, 7]]


def build(S, debug=False):
    NT = S // 128
    CAP = S // 8
    NJ = CAP // 128
    NG = S // 512
    assert NJ >= 1
    nc = bass.Bass("TRN2", target_bir_lowering=False)

    def din(name, shape, dt=F32):
        return nc.dram_tensor(name, shape, dt, kind="ExternalInput").ap()

    def dscr(name, shape, dt, dbg=True):
        return nc.dram_tensor(name, shape, dt, kind="ExternalOutput" if (debug and dbg) else "Internal").ap()

    x = din("x", [S, D])
    pin = din("p", [S, 256])
    pos_l = din("pos_l", [128, NT], I32)
    invf_rep = din("invf_rep", [128, 8])
    gmix_l = din("gmix_l", [128, 8])
    gple_l = din("gple_l", [128, 8])
    ggla_l = din("ggla_l", [128, 4])
    gffn_rep = din("gffn_rep", [128, D])
    gfin_rep = din("gfin_rep", [128, D])
    sink_rep = din("sink_rep", [1, 512])
    upext_f = din("upext_f", [17, 256])
    upext_b = din("upext_b", [17, 256])
    w_in = din("w_in", [D, NCOL])
    w_ba = din("w_ba", [256, D])
    w_bg = din("w_bg", [512, D])
    w_out = din("w_out", [D, D])
    w_router = din("w_router", [D, NEXP])
    rowmask_in = din("rowmask", [NEXP, 2])
    w_eg = din("w_eg", [NEL, D, D])
    w_eu = din("w_eu", [NEL, D, D])
    w_ed = din("w_ed", [NEL, D, D])
    w_pg = din("w_pg", [D, D])
    w_ple = din("w_ple", [256, D])
    y = nc.dram_tensor("y", [S, D], F32, kind="ExternalOutput").ap()

    qT_d = dscr("qT_d", [256, S], BF16)
    kT_d = dscr("kT_d", [64, S], BF16)
    va_d = dscr("va_d", [S, 64], BF16)
    gqT_d = dscr("gqT_d", [256, S], BF16)
    gkT_d = dscr("gkT_d", [256, S], BF16)
    gk_d = dscr("gk_d", [S, 256], BF16)
    gv_d = dscr("gv_d", [S, 512], BF16)
    grT_d = dscr("grT_d", [512, S], BF16)
    gaT_d = dscr("gaT_d", [1024, S], BF16)
    ggT_d = dscr("ggT_d", [1024, S], BF16)
    laf_d = dscr("laf_d", [S, 256], F32)
    lab_d = dscr("lab_d", [S, 256], F32)
    obT_d = dscr("obT_d", [512, S], F32)
    dpart_d = dscr("dpart_d", [S, D], F32, dbg=False)
    dall_d = dscr("dall_d", [NG, 1024, D], F32, dbg=False)
    h1_d = dscr("h1_d", [S, D], F32)
    xn_d = dscr("xn_d", [S, D], BF16)
    yacc_d = dscr("yacc_d", [S, D], F32, dbg=False)
    yall_d = dscr("yall_d", [NG, 1024, D], F32, dbg=False)
    if debug:
        idx_dbg = dscr("idx_dbg", [128, NJ * NEL], U32)
        val_dbg = dscr("val_dbg", [128, NJ * NEL], F32)

    def fm(dram, c0, nch, t):
        return dram[c0 * 128:(c0 + nch) * 128, t * 128:(t + 1) * 128].rearrange("(c p) s -> p c s", p=128)

    with ExitStack() as es:
        P = Prog(nc, es)
        dbufs = {}

        def db(name, idx=0):
            k = (name, idx)
            if k not in dbufs:
                dbufs[k] = Buf(f"{name}_{idx}", dram=True)
            return dbufs[k]

        pers = lambda n, s, d: es.enter_context(nc.sbuf_tensor(n, s, d))
        ident = pers("ident", [128, 128], F32)
        identb = pers("identb", [128, 128], BF16)
        mLE = pers("mLE", [128, 128], F32)
        mGT = pers("mGT", [128, 128], F32)
        mGE = pers("mGE", [128, 128], F32)
        mLT = pers("mLT", [128, 128], F32)
        onesb = pers("onesb", [128, 128], BF16)
        onesf = pers("onesf", [1, 128], F32)
        epsc = pers("epsc", [128, 1], F32)
        HS = S // 2
        affA = pers("affA", [NEXP, HS], F32)
        affB = pers("affB", [NEXP, HS], F32)
        Jm = pers("Jm", [128, 128], F32)
        rowmask = pers("rowmask_sb", [NEXP, 2], F32)
        Bconst = P.buf("const")
        BaffT = P.buf("affT")
        valsT = pers("valsT", [128, NJ, NEL], F32)
        idxT = pers("idxT", [128, NJ, NEL], U32)
        Bsel = P.buf("sel")

        def mk_mask(tile, step, cm, cmp):
            P.memset("pool", tile[:], 1.0, w=[Bconst])
            P.op("pool", lambda e: e.affine_select(out=tile[:], in_=tile[:], pattern=[[step, 128]],
                                                   compare_op=cmp, fill=0.0, base=0, channel_multiplier=cm),
                 r=[Bconst], w=[Bconst])

        mk_mask(ident, -1, 1, ALU.is_equal)
        mk_mask(mLE, 1, -1, ALU.is_ge)
        mk_mask(mGT, -1, 1, ALU.is_gt)
        mk_mask(mGE, -1, 1, ALU.is_ge)
        mk_mask(mLT, 1, -1, ALU.is_gt)
        P.copy("pool", identb[:], ident[:], r=[Bconst], w=[Bconst])
        P.memset("pool", onesb[:], 1.0, w=[Bconst])
        P.memset("pool", onesf[:], 1.0, w=[Bconst])
        P.memset("pool", epsc[:], EPS, w=[Bconst])
        P.memset("pool", Jm[:], 1.0, w=[Bconst])
        P.op("pool", lambda e: e.affine_select(out=Jm[:], in_=Jm[:], pattern=[[1, 128]], compare_op=ALU.is_equal,
                                               fill=0.0, base=-127, channel_multiplier=1), r=[Bconst], w=[Bconst])
        P.dma("sp", rowmask[:], rowmask_in, w=[Bconst], owner=Bconst)

        def rstd_from_ss(ss, Bss, a, b_, c_, scale):
            P.act(ss[:, b_:b_ + 1], ss[:, a:a + 1], AF.Sqrt, scale=scale, bias=epsc[:, 0:1], r=[Bss, Bconst], w=[Bss])
            P.op("dve", (lambda ss_: lambda e: e.reciprocal(out=ss_[:, c_:c_ + 1], in_=ss_[:, b_:b_ + 1]))(ss),
                 r=[Bss], w=[Bss])

        with ExitStack() as ph:
            sbt = lambda n, s, d: ph.enter_context(nc.sbuf_tensor(P.uniq(n), s, d))
            mkp = lambda name, shape, dt, n, psum=False: TPool(P, ph, nc, name, shape, dt, n, psum)
            Win = sbt("Win", [128, 8, NCOL], BF16)
            BWin = P.buf("Win")
            gmix = sbt("gmix", [128, 8], F32)
            Bg = P.buf("gmix")
            upf = sbt("upf", [17, 256], F32)
            upb = sbt("upb", [17, 256], F32)
            posi = sbt("posi", [128, NT], I32)
            posf = sbt("posf", [128, NT], F32)
            invf = sbt("invf", [128, 8], F32)
            ang = sbt("ang", [128, NT, 8], F32)
            cosT = sbt("cosT", [128, NT, 8], F32)
            sinT = sbt("sinT", [128, NT, 8], F32)
            Brope = P.buf("rope")
            Bup = P.buf("up")
            for c in range(8):
                P.dma("pool", Win[:, c, :], w_in[c * 128:(c + 1) * 128, :], w=[BWin], owner=BWin)
            P.dma("sp", gmix[:], gmix_l, w=[Bg], owner=Bg)
            P.dma("sp", upf[:], upext_f, w=[Bup], owner=Bup)
            P.dma("sp", upb[:], upext_b, w=[Bup], owner=Bup)
            P.dma("sp", posi[:], pos_l, w=[Brope], owner=Brope)
            for c in range(8):
                if c % 2 == 0:
                    P.ts("dve", Win[:, c, :], Win[:, c, :], gmix[:, c:c + 1], op0=ALU.mult, r=[Bg, BWin], w=[BWin])
                else:
                    P.act(Win[:, c, :], Win[:, c, :], AF.Copy, scale=gmix[:, c:c + 1], r=[Bg, BWin], w=[BWin])
            P.dma("sp", invf[:], invf_rep, w=[Brope], owner=Brope)
            P.copy("dve", posf[:], posi[:], r=[Brope], w=[Brope])
            for t in range(NT):
                P.ts("dve", ang[:, t, :], invf[:], posf[:, t:t + 1], op0=ALU.mult, r=[Brope], w=[Brope])
            TWO_PI = 2.0 * math.pi
            angf = ang[:].rearrange("p t f -> p (t f)")
            tmpa = sbt("tmpa", [128, NT * 8], F32)
            tmpb = sbt("tmpb", [128, NT * 8], F32)
            tmpk = sbt("tmpk", [128, NT * 8], I32)
            for (outT, shift) in ((sinT, 0.0), (cosT, 0.5 * math.pi)):
                P.ts("dve", tmpa[:], angf, shift, None, op0=ALU.add, r=[Brope], w=[Brope])
                P.ts("dve", tmpb[:], tmpa[:], 1.0 / TWO_PI, None, op0=ALU.mult, r=[Brope], w=[Brope])
                P.copy("dve", tmpk[:], tmpb[:], r=[Brope], w=[Brope])
                P.copy("dve", tmpb[:], tmpk[:], r=[Brope], w=[Brope])
                P.stt(tmpa[:], tmpb[:], -TWO_PI, tmpa[:], ALU.mult, ALU.add, r=[Brope], w=[Brope])
                P.ts("dve", tmpb[:], tmpa[:], math.pi, None, op0=ALU.is_gt, r=[Brope], w=[Brope])
                P.stt(tmpa[:], tmpb[:], -TWO_PI, tmpa[:], ALU.mult, ALU.add, r=[Brope], w=[Brope])
                P.ts("dve", tmpb[:], tmpa[:], -math.pi, None, op0=ALU.is_lt, r=[Brope], w=[Brope])
                P.stt(tmpa[:], tmpb[:], TWO_PI, tmpa[:], ALU.mult, ALU.add, r=[Brope], w=[Brope])
                P.act(outT[:].rearrange("p t f -> p (t f)"), tmpa[:], AF.Sin, r=[Brope], w=[Brope])

            psF = mkp("psF", [128, 512], F32, 6, psum=True)
            psB = mkp("psB", [128, 1024], BF16, 2, psum=True)
            xp = mkp("xp", [128, D], F32, 5)
            junk = mkp("junk", [128, D], BF16, 1)
            ssp = mkp("ssp", [128, 4], F32, 9)
            abp = mkp("abp", [128, D], BF16, 9)
            aTp = mkp("aTp", [128, D], BF16, 3)
            qkp = mkp("qkp", [128, 320], F32, 3)
            rtp = mkp("rtp", [128, 4, 5, 8], F32, 3)
            qkbp = mkp("qkbp", [128, 384], BF16, 3)
            qkTp = mkp("qkTp", [128, 3, 128], BF16, 3)
            vbp = mkp("vbp", [128, 64], BF16, 3)
            gkp = mkp("gkp", [128, 256], BF16, 3)
            gvp = mkp("gvp", [128, 512], BF16, 3)
            f2p = mkp("f2p", [128, 2, 128], BF16, 4)
            f4p = mkp("f4p", [128, 4, 128], BF16, 3)
            f8p = mkp("f8p", [128, 8, 128], BF16, 4)
            zfp = mkp("zfp", [32, 128], F32, 3)
            zbp = mkp("zbp", [32, 128], F32, 3)
            lt1 = mkp("lt1", [128, 256], F32, 3)
            lt2 = mkp("lt2", [128, 256], F32, 3)
            lap = mkp("lap", [128, 256], F32, 3)
            for zp in (zfp, zbp):
                for (zt, zB) in zp.items:
                    P.memset("pool", zt[:], 1.0, w=[zB])
            for (qt_, qB) in qkbp.items:
                P.memset("pool", qt_[:], 0.0, w=[qB])

            aT4p = mkp("aT4p", [128, 8, 512], BF16, 2)
            fst = mkp("fst", [128, 512], BF16, 6)

            def bodyA(g):
                aT4, BaT4 = aT4p.next()
                abs_ = []
                for i in range(4):
                    t = g * 4 + i
                    tok = slice(t * 128, (t + 1) * 128)
                    xt, Bx = xp.next()
                    P.dma("act", xt[:], x[tok, :], w=[Bx])
                    jk, Bj = junk.next()
                    ss, Bss = ssp.next()
                    P.act(jk[:], xt[:], AF.Square, accum_out=ss[:, 0:1], r=[Bx], w=[Bj, Bss])
                    rstd_from_ss(ss, Bss, 0, 1, 2, 1.0 / D)
                    ab, Bab = abp.next()
                    P.act(ab[:], xt[:], AF.Copy, scale=ss[:, 2:3], r=[Bx, Bss], w=[Bab])
                    abs_.append((ab, Bab))
                yield
                for i in range(4):
                    ab, Bab = abs_[i]
                    pT, BpT = psB.next()
                    for c in range(8):
                        P.transpose(pT[:, c * 128:(c + 1) * 128], ab[:, c * 128:(c + 1) * 128], identb[:],
                                    r=[Bab, Bconst], w=[BpT])
                    P.copy("dve", aT4[:, :, i * 128:(i + 1) * 128], pT[:].rearrange("p (c s) -> p c s", c=8),
                           r=[BpT], w=[BaT4])
                    yield
                pending = []
                for i in range(4):
                    t = g * 4 + i
                    tok = slice(t * 128, (t + 1) * 128)
                    tsl = slice(i * 128, (i + 1) * 128)
                    for fn_ in pending:
                        fn_()
                    pending = []

                    def proj_tok(c0, n, tsl=tsl):
                        ps, Bps = psF.next()
                        for c in range(8):
                            P.matmul(ps[:, 0:n], aT4[:, c, tsl], Win[:, c, c0:c0 + n],
                                     start=(c == 0), stop=(c == 7), r=[BaT4, BWin], w=[Bps])
                        return ps, Bps

                    psq, Bpsq = proj_tok(C_AQ, 320)
                    qk, Bqk = qkp.next()
                    P.copy("act", qk[:], psq[:, 0:320], r=[Bpsq], w=[Bqk])
                    qk3 = qk[:].rearrange("p (h d) -> p h d", h=5)
                    t1 = qk3[:, :, 0:8]
                    t2 = qk3[:, :, 8:16]
                    rt, Brt = rtp.next()
                    cb = cosT[:, t:t + 1, :].to_broadcast([128, 5, 8])
                    sb_ = sinT[:, t:t + 1, :].to_broadcast([128, 5, 8])
                    P.tt("pool", rt[:, 0], t1, cb, ALU.mult, r=[Bqk, Brope], w=[Brt])
                    P.tt("pool", rt[:, 1], t2, sb_, ALU.mult, r=[Bqk, Brope], w=[Brt])
                    P.tt("pool", rt[:, 2], t2, cb, ALU.mult, r=[Bqk, Brope], w=[Brt])
                    P.tt("pool", rt[:, 3], t1, sb_, ALU.mult, r=[Bqk, Brope], w=[Brt])
                    P.tt("pool", t1, rt[:, 0], rt[:, 1], ALU.subtract, r=[Brt], w=[Bqk])
                    P.tt("pool", t2, rt[:, 2], rt[:, 3], ALU.add, r=[Brt], w=[Bqk])
                    qkb, Bqkb = qkbp.next()
                    P.copy("act", qkb[:, 0:320], qk[:], r=[Bqk], w=[Bqkb])

                    def fin_qk(qkb=qkb, Bqkb=Bqkb, t=t, tok=tok):
                        pT2, BpT2 = psB.next()
                        for c in range(3):
                            P.transpose(pT2[:, c * 128:(c + 1) * 128], qkb[:, c * 128:(c + 1) * 128], identb[:],
                                        r=[Bqkb, Bconst], w=[BpT2])
                        qkT, BqkT = qkTp.next()
                        P.copy("dve", qkT[:].rearrange("p c s -> p (c s)"), pT2[:, 0:384], r=[BpT2], w=[BqkT])
                        P.dma("sp", fm(qT_d, 0, 2, t), qkT[:, 0:2, :], r=[BqkT], w=[db("qT", t)])
                        P.dma("sp", kT_d[:, tok], qkT[0:64, 2, :], r=[BqkT], w=[db("kT", t)])
                    pending.append(fin_qk)
                    ps, Bps = proj_tok(C_AV, 64)
                    vb, Bvb = vbp.next()
                    P.copy("dve", vb[:], ps[:, 0:64], r=[Bps], w=[Bvb])
                    P.dma("sp", va_d[tok, :], vb[:], r=[Bvb], w=[db("va", t)])
                    ps, Bps = proj_tok(C_GK, 256)
                    gkt, Bgk = gkp.next()
                    P.copy("dve", gkt[:], ps[:, 0:256], r=[Bps], w=[Bgk])
                    P.dma("sp", gk_d[tok, :], gkt[:], r=[Bgk], w=[db("gk", t)])
                    gvt, Bgv = gvp.next()
                    ps, Bps = proj_tok(C_GV, 512)
                    P.copy("dve", gvt[:], ps[:, 0:512], r=[Bps], w=[Bgv])
                    P.dma("sp", gv_d[tok, :], gvt[:], r=[Bgv], w=[db("gv", t)])
                    ps, Bps = psF.next()
                    for zi, c0 in enumerate((C_ZF, C_ZB)):
                        for c in range(8):
                            P.matmul(ps[0:16, zi * 128:(zi + 1) * 128], Win[:, c, c0:c0 + 16], aT4[:, c, tsl],
                                     start=(c == 0), stop=(c == 7), r=[BaT4, BWin], w=[Bps])
                    zf, Bzf = zfp.next()
                    zb, Bzb = zbp.next()
                    P.copy("dve", zf[0:16, :], ps[0:16, 0:128], r=[Bps], w=[Bzf])
                    P.copy("dve", zb[0:16, :], ps[0:16, 128:256], r=[Bps], w=[Bzb])

                    def fin_la(zf=zf, Bzf=Bzf, zb=zb, Bzb=Bzb, t=t, tok=tok):
                        for (zt, Bz, up, dram, nm) in ((zf, Bzf, upf, laf_d, "laf"), (zb, Bzb, upb, lab_d, "lab")):
                            ps2, Bps2 = psF.next()
                            P.matmul(ps2[:, 0:256], zt[0:17, :], up[0:17, :], r=[Bz, Bup], w=[Bps2])
                            a1, B1 = lt1.next()
                            P.act(a1[:], ps2[:, 0:256], AF.Exp, scale=-1.0, r=[Bps2], w=[B1])
                            a2, B2 = lt2.next()
                            P.act(a2[:], a1[:], AF.Ln, bias=1.0, r=[B1], w=[B2])
                            a3, B3 = lap.next()
                            P.ts("pool", a3[:], a2[:], -1.0 / 16.0, None, op0=ALU.mult, r=[B2], w=[B3])
                            P.dma("sp", dram[tok, :], a3[:], r=[B3], w=[db(nm, t)])
                    pending.append(fin_la)
                    yield
                for fn_ in pending:
                    fn_()
                yield
                gsl = slice(g * 512, (g + 1) * 512)
                for (c0, nch, dram, nm, fn) in ((C_GQ, 2, gqT_d, "gqT", None), (C_GK, 2, gkT_d, "gkT", None),
                                                (C_GR, 4, grT_d, "grT", AF.Silu), (C_GA, 8, gaT_d, "gaT", AF.Sigmoid),
                                                (C_GG, 8, ggT_d, "ggT", AF.Sigmoid)):
                    for k in range(nch):
                        ps, Bps = psF.next()
                        for c in range(8):
                            P.matmul(ps[:, 0:512], Win[:, c, c0 + k * 128:c0 + (k + 1) * 128], aT4[:, c, :],
                                     start=(c == 0), stop=(c == 7), r=[BaT4, BWin], w=[Bps])
                        st, Bst = fst.next()
                        if fn is None:
                            P.copy("dve", st[:], ps[:, 0:512], r=[Bps], w=[Bst])
                        else:
                            P.act(st[:], ps[:, 0:512], fn, r=[Bps], w=[Bst])
                        P.dma("sp", dram[k * 128:(k + 1) * 128, gsl], st[:], r=[Bst], w=[db(nm + "_c%d" % k, g)])
                        if k % 2 == 1:
                            yield
            run_pipelined([bodyA(g) for g in range(NT // 4)], 11)
            P.barrier()
            P.emit()
            P.end_phase()

        def gla_setup(ph):
            mkp = lambda name, shape, dt, n, psum=False: TPool(P, ph, nc, name, shape, dt, n, psum)
            G = {}
            G["gqT"] = mkp("g_gqT", [128, 2, 128], BF16, 3)
            G["gkT"] = mkp("g_gkT", [128, 2, 128], BF16, 3)
            G["gk"] = mkp("g_gk", [128, 256], BF16, 3)
            G["gv"] = mkp("g_gv", [128, 512], BF16, 3)
            G["la"] = mkp("g_la", [128, 256], F32, 3)
            G["Eq"] = mkp("g_Eq", [128, 256], F32, 3)
            G["Ek"] = mkp("g_Ek", [128, 256], F32, 3)
            G["Ee"] = mkp("g_Ee", [128, 256], F32, 3)
            G["qt"] = mkp("g_qt", [128, 2, 128], BF16, 3)
            G["kt"] = mkp("g_kt", [128, 2, 128], BF16, 3)
            G["ke"] = mkp("g_ke", [128, 256], BF16, 3)
            G["at"] = mkp("g_at", [128, 2, 128], BF16, 3)
            sbt = lambda n, s, d: ph.enter_context(nc.sbuf_tensor(P.uniq(n), s, d))
            G["S"] = sbt("g_S", [128, 2, 256], F32)
            G["Sb"] = sbt("g_Sb", [128, 2, 256], BF16)
            G["BS"] = P.buf("S")
            G["BSb"] = P.buf("Sb")
            return G

        def gla_reset(G):
            P.memset("pool", G["S"][:], 0.0, w=[G["BS"]])
            P.memset("pool", G["Sb"][:], 0.0, w=[G["BSb"]])

        def gla_tile(G, psF, t, fwd):
            tok = slice(t * 128, (t + 1) * 128)
            la_d, la_nm = (laf_d, "laf") if fwd else (lab_d, "lab")
            m_incl, m_strict = (mLE, mGT) if fwd else (mGE, mLT)
            m_attn = mLE if fwd else mGT
            gq, Bgq = G["gqT"].next()
            P.dma("sp", gq[:], fm(gqT_d, 0, 2, t), r=[db("gqT", t)], w=[Bgq])
            gkT, BgkT = G["gkT"].next()
            P.dma("sp", gkT[:], fm(gkT_d, 0, 2, t), r=[db("gkT", t)], w=[BgkT])
            gk, Bgk = G["gk"].next()
            P.dma("sp", gk[:], gk_d[tok, :], r=[db("gk", t)], w=[Bgk])
            gv, Bgv = G["gv"].next()
            P.dma("sp", gv[:], gv_d[tok, :], r=[db("gv", t)], w=[Bgv])
            la, Bla = G["la"].next()
            P.dma("sp", la[:], la_d[tok, :], r=[db(la_nm, t)], w=[Bla])
            yield
            psb, Bpsb = psF.next()
            for h in range(2):
                P.matmul(psb[:, h * 128:(h + 1) * 128], la[:, h * 128:(h + 1) * 128], m_incl[:],
                         r=[Bla, Bconst], w=[Bpsb])
            psg, Bpsg = psF.next()
            P.matmul(psg[:, 0:256], m_strict[:], la[:], r=[Bla, Bconst], w=[Bpsg])
            Eq, BEq = G["Eq"].next()
            P.act(Eq[:], psb[:, 0:256], AF.Exp, r=[Bpsb], w=[BEq])
            Ek, BEk = G["Ek"].next()
            P.act(Ek[:], psb[:, 0:256], AF.Exp, scale=-1.0, r=[Bpsb], w=[BEk])
            Ee, BEe = G["Ee"].next()
            P.act(Ee[:], psg[:, 0:256], AF.Exp, r=[Bpsg], w=[BEe])
            qt, Bqt = G["qt"].next()
            P.stt(qt[:].rearrange("p h l -> p (h l)"), gq[:].rearrange("p h l -> p (h l)"), 128.0 ** -0.5, Eq[:],
                  ALU.mult, ALU.mult, r=[Bgq, BEq], w=[Bqt])
            kt, Bkt = G["kt"].next()
            P.tt("dve", kt[:].rearrange("p h l -> p (h l)"), gkT[:].rearrange("p h l -> p (h l)"), Ek[:], ALU.mult,
                 r=[BgkT, BEk], w=[Bkt])
            ke, Bke = G["ke"].next()
            P.tt("dve", ke[:], gk[:], Ee[:], ALU.mult, r=[Bgk, BEe], w=[Bke])
            yield
            psa, Bpsa = psF.next()
            for h in range(2):
                P.matmul(psa[:, h * 128:(h + 1) * 128], kt[:, h, :], qt[:, h, :], r=[Bkt, Bqt], w=[Bpsa])
            at, Bat = G["at"].next()
            P.tt("dve", at[:], psa[:, 0:256].rearrange("p (h l) -> p h l", h=2),
                 m_attn[:].unsqueeze(1).to_broadcast([128, 2, 128]), ALU.mult, r=[Bpsa, Bconst], w=[Bat])
            dcol = 127 if fwd else 0
            pk, Bpk = psF.next()
            for h in range(2):
                P.matmul(pk[:, h * 256:(h + 1) * 256], ke[:, h * 128:(h + 1) * 128], gv[:, h * 256:(h + 1) * 256],
                         r=[Bke, Bgv], w=[Bpk])
            for h in range(2):
                P.stt(G["S"][:, h, :], G["S"][:, h, :], Eq[:, h * 128 + dcol:h * 128 + dcol + 1],
                      pk[:, h * 256:(h + 1) * 256], ALU.mult, ALU.add, r=[BEq, Bpk, G["BS"]], w=[G["BS"]])
            po, Bpo = psF.next()
            for h in range(2):
                for ec in range(2):
                    o_ap = po[:, (h * 2 + ec) * 128:(h * 2 + ec + 1) * 128]
                    P.matmul(o_ap, gv[:, h * 256 + ec * 128:h * 256 + (ec + 1) * 128], at[:, h, :],
                             start=True, stop=False, r=[Bgv, Bat], w=[Bpo])
                    P.matmul(o_ap, G["Sb"][:, h, ec * 128:(ec + 1) * 128], qt[:, h, :],
                             start=False, stop=True, r=[G["BSb"], Bqt], w=[Bpo])
            P.copy("act", G["Sb"][:].rearrange("p h e -> p (h e)"), G["S"][:].rearrange("p h e -> p (h e)"),
                   r=[G["BS"]], w=[G["BSb"]])
            return po, Bpo

        with ExitStack() as ph:
            mkp = lambda name, shape, dt, n, psum=False: TPool(P, ph, nc, name, shape, dt, n, psum)
            psF = mkp("psFb", [128, 512], F32, 8, psum=True)
            G = gla_setup(ph)
            obp = mkp("obp", [128, 4, 128], F32, 3)
            gla_reset(G)
            def bodyB(t):
                po, Bpo = yield from gla_tile(G, psF, t, False)
                ob, Bob = obp.next()
                P.copy("act", ob[:].rearrange("p c s -> p (c s)"), po[:, 0:512], r=[Bpo], w=[Bob])
                P.dma("pool", fm(obT_d, 0, 4, t), ob[:], r=[Bob], w=[db("obT", t)])
            run_pipelined([bodyB(t) for t in range(NT - 1, -1, -1)], 1)
            P.barrier()
            P.emit()
            P.end_phase()

        with ExitStack() as ph:
            sbt = lambda n, s, d: ph.enter_context(nc.sbuf_tensor(P.uniq(n), s, d))
            mkp = lambda name, shape, dt, n, psum=False: TPool(P, ph, nc, name, shape, dt, n, psum)
            psF = mkp("psFc", [128, 512], F32, 8, psum=True)
            G = gla_setup(ph)
            Wa = sbt("Wa", [64, 4, D], BF16)
            Wb = sbt("Wb", [128, 4, D], BF16)
            Wo = sbt("Wo", [128, 8, D], BF16)
            ggla = sbt("ggla", [128, 4], F32)
            sinkr = sbt("sinkr", [1, 512], F32)
            BW = P.buf("Wc")
            P.dma("pool", Wa[:], w_ba.rearrange("(h d) c -> d h c", d=64), w=[BW], owner=BW)
            P.dma("pool", Wb[:], w_bg.rearrange("(k p) c -> p k c", p=128), w=[BW], owner=BW)
            P.dma("pool", Wo[:], w_out.rearrange("(k p) c -> p k c", p=128), w=[BW], owner=BW)
            P.dma("sp", ggla[:], ggla_l, w=[BW], owner=BW)
            P.dma("sp", sinkr[:], sink_rep, w=[BW], owner=BW)
            P.act(sinkr[:], sinkr[:], AF.Exp, r=[BW], w=[BW])

            obl = mkp("obl", [128, 4, 128], F32, 3)
            grl = mkp("grl", [128, 4, 128], BF16, 3)
            gal = mkp("gal", [128, 8, 128], BF16, 3)
            ggl = mkp("ggl", [128, 8, 128], BF16, 3)
            osb = mkp("osb", [128, 4, 128], F32, 3)
            sqb = mkp("sqb", [128, 4, 128], BF16, 3)
            rsd = mkp("rsd", [128, 2, 128], F32, 3)
            ogp = mkp("ogp", [128, 4, 128], F32, 3)
            ogb = mkp("ogb", [128, 4, 128], BF16, 3)
            qTl = mkp("qTl", [64, 4, 128], BF16, 3)
            kTl = mkp("kTl", [64, 384], BF16, 3)
            val = mkp("val", [128, 3, 64], BF16, 3)
            ptp = mkp("ptp", [128, 4, 128], BF16, 4)
            rdn = mkp("rdn", [64, 512], F32, 3)
            atp = mkp("atp", [64, 4, 128], BF16, 3)
            t1p = mkp("t1p", [128, 8, 128], F32, 3)
            t2p = mkp("t2p", [128, 8, 128], F32, 3)
            mgp = mkp("mgp", [128, 8, 128], BF16, 3)
            dpp = mkp("dpp", [128, D], F32, 3)
            gla_reset(G)
            Bcc = P.buf("ccd")

            def bodyC(t):
                tok = slice(t * 128, (t + 1) * 128)
                po, Bpo = yield from gla_tile(G, psF, t, True)
                ob, Bob = obl.next()
                P.dma("sp", ob[:], fm(obT_d, 0, 4, t), r=[db("obT", t)], w=[Bob])
                gr, Bgr = grl.next()
                P.dma("sp", gr[:], fm(grT_d, 0, 4, t), r=[db("grT", t)], w=[Bgr])
                o, Bo = osb.next()
                P.tt("dve", o[:].rearrange("p c s -> p (c s)"), po[:, 0:512], ob[:].rearrange("p c s -> p (c s)"),
                     ALU.add, r=[Bpo, Bob], w=[Bo])
                sq, Bsq = sqb.next()
                P.act(sq[:].rearrange("p c s -> p (c s)"), o[:].rearrange("p c s -> p (c s)"), AF.Square, r=[Bo], w=[Bsq])
                pss, Bpss = psF.next()
                for h in range(2):
                    for ec in range(2):
                        P.matmul(pss[:, h * 128:(h + 1) * 128], onesb[:], sq[:, h * 2 + ec, :], start=(ec == 0),
                                 stop=(ec == 1), r=[Bconst, Bsq], w=[Bpss])
                rs, Brs = rsd.next()
                rsf = rs[:].rearrange("p h s -> p (h s)")
                P.act(rsf, pss[:, 0:256], AF.Sqrt, scale=1.0 / 256.0, bias=epsc[:, 0:1], r=[Bpss, Bconst], w=[Brs])
                P.op("dve", (lambda rsf: lambda e: e.reciprocal(out=rsf, in_=rsf))(rsf), r=[Brs], w=[Brs])
                og, Bog = ogp.next()
                for c in range(4):
                    P.stt(og[:, c, :], o[:, c, :], ggla[:, c:c + 1], rs[:, c // 2, :], ALU.mult, ALU.mult,
                          r=[Bo, BW, Brs], w=[Bog])
                ogT, BogT = ogb.next()
                P.tt("dve", ogT[:].rearrange("p c s -> p (c s)"), og[:].rearrange("p c s -> p (c s)"),
                     gr[:].rearrange("p c s -> p (c s)"), ALU.mult, r=[Bog, Bgr], w=[BogT])
                yield
                kbs = [kb for kb in (-1, 0, 1) if 0 <= t + kb < NT]
                qTt, BqT = qTl.next()
                P.dma("sp", qTt[:], qT_d[:, tok].rearrange("(h d) s -> d h s", d=64), r=[db("qT", t)], w=[BqT])
                kTt, BkT = kTl.next()
                vat, Bva = val.next()
                for kb in kbs:
                    tk = t + kb
                    P.dma("sp", kTt[:, (kb + 1) * 128:(kb + 2) * 128], kT_d[:, tk * 128:(tk + 1) * 128],
                          r=[db("kT", tk)], w=[BkT])
                    P.dma("sp", vat[:, kb + 1, :], va_d[tk * 128:(tk + 1) * 128, :], r=[db("va", tk)], w=[Bva])
                at_, Bat_ = atp.next()
                pts = []
                for kb in kbs:
                    psc, Bpsc = psF.next()
                    P.matmul(psc[:, 0:512], kTt[:, (kb + 1) * 128:(kb + 2) * 128],
                             qTt[:].rearrange("d h s -> d (h s)"), r=[BkT, BqT], w=[Bpsc])
                    pt, Bpt = ptp.next()
                    P.act(pt[:].rearrange("p h s -> p (h s)"), psc[:, 0:512], AF.Exp, scale=0.125, r=[Bpsc], w=[Bpt])
                    if kb != 0:
                        mk = mGE if kb == -1 else mLE
                        P.tt("dve", pt[:], pt[:], mk[:].unsqueeze(1).to_broadcast([128, 4, 128]), ALU.mult,
                             r=[Bpt, Bconst], w=[Bpt])
                    pts.append((kb, pt, Bpt))
                gg, Bgg = ggl.next()
                P.dma("sp", gg[:], fm(ggT_d, 0, 8, t), r=[db("ggT", t)], w=[Bgg])
                t2_, Bt2 = t2p.next()
                for half in range(2):
                    pyg, Bpyg = psF.next()
                    for cc in range(4):
                        c = half * 4 + cc
                        for k in range(4):
                            P.matmul(pyg[:, cc * 128:(cc + 1) * 128], Wb[:, k, c * 128:(c + 1) * 128], ogT[:, k, :],
                                     start=(k == 0), stop=(k == 3), r=[BW, BogT], w=[Bpyg])
                    P.tt("dve", t2_[:, half * 4:(half + 1) * 4, :].rearrange("p c s -> p (c s)"), pyg[:, 0:512],
                         gg[:, half * 4:(half + 1) * 4, :].rearrange("p c s -> p (c s)"), ALU.mult, r=[Bpyg, Bgg], w=[Bt2])
                pso, Bpso = psF.next()
                psd, Bpsd = psF.next()
                for i, (kb, pt, Bpt) in enumerate(pts):
                    ptf = pt[:].rearrange("p h s -> p (h s)")
                    P.matmul(pso[0:64, 0:512], vat[:, kb + 1, :], ptf, start=(i == 0),
                             stop=(i == len(pts) - 1), r=[Bva, Bpt], w=[Bpso])
                    P.matmul(psd[0:64, 0:512], onesb[:, 0:64], ptf, start=(i == 0), stop=False,
                             r=[Bconst, Bpt], w=[Bpsd])
                P.matmul(psd[0:64, 0:512], onesf[0:1, 0:64], sinkr[0:1, :], start=False,
                         stop=True, r=[Bconst, BW], w=[Bpsd])
                rd, Brd = rdn.next()
                P.op("dve", (lambda rd, psd: lambda e: e.reciprocal(out=rd[:], in_=psd[0:64, 0:512]))(rd, psd),
                     r=[Bpsd], w=[Brd])
                P.tt("dve", at_[:].rearrange("d h s -> d (h s)"), pso[0:64, 0:512], rd[:],
                     ALU.mult, r=[Bpso, Brd], w=[Bat_])
                yield
                ga, Bga = gal.next()
                P.dma("sp", ga[:], fm(gaT_d, 0, 8, t), r=[db("gaT", t)], w=[Bga])
                t1_, Bt1 = t1p.next()
                for half in range(2):
                    pya, Bpya = psF.next()
                    for cc in range(4):
                        c = half * 4 + cc
                        for h in range(4):
                            P.matmul(pya[:, cc * 128:(cc + 1) * 128], Wa[:, h, c * 128:(c + 1) * 128], at_[:, h, :],
                                     start=(h == 0), stop=(h == 3), r=[BW, Bat_], w=[Bpya])
                    P.tt("dve", t1_[:, half * 4:(half + 1) * 4, :].rearrange("p c s -> p (c s)"), pya[:, 0:512],
                         ga[:, half * 4:(half + 1) * 4, :].rearrange("p c s -> p (c s)"), ALU.mult, r=[Bpya, Bga], w=[Bt1])
                mg, Bmg = mgp.next()
                P.tt("dve", mg[:].rearrange("p c s -> p (c s)"), t1_[:].rearrange("p c s -> p (c s)"),
                     t2_[:].rearrange("p c s -> p (c s)"), ALU.add, r=[Bt1, Bt2], w=[Bmg])
                yield
                dp, Bdp = dpp.next()
                for half in range(2):
                    psh, Bpsh = psF.next()
                    for c in range(8):
                        P.matmul(psh[:, 0:512], mg[:, c, :], Wo[:, c, half * 512:(half + 1) * 512], start=(c == 0),
                                 stop=(c == 7), r=[Bmg, BW], w=[Bpsh])
                    P.copy("act" if half == 0 else "dve", dp[:, half * 512:(half + 1) * 512], psh[:, 0:512],
                           r=[Bpsh], w=[Bdp])
                g4 = t // 4
                P.dma("pool", dpart_d[tok, :], dp[:], r=[Bdp], w=[db("dpart", g4)])
                if t % 4 == 3:
                    P.dma_fn("pool", (lambda g4: lambda e: e.collective_compute(
                        "AllGather", op=ALU.bypass, replica_groups=GROUPS,
                        ins=[dpart_d[g4 * 512:(g4 + 1) * 512, :]], outs=[dall_d[g4]]))(g4),
                        r=[db("dpart", g4)], w=[db("dall", g4)], owner=Bcc, inc=1)
            run_pipelined([bodyC(t) for t in range(NT)], 2)
            P.barrier()
            P.emit()
            P.end_phase()

        with ExitStack() as ph:
            sbt = lambda n, s, d: ph.enter_context(nc.sbuf_tensor(P.uniq(n), s, d))
            mkp = lambda name, shape, dt, n, psum=False: TPool(P, ph, nc, name, shape, dt, n, psum)
            psF = mkp("psFc2", [128, 512], F32, 8, psum=True)
            Wr = sbt("Wr", [128, 8, NEXP], F32)
            WrB = sbt("WrB", [128, 8, NEXP], F32)
            gffn = sbt("gffn", [128, D], F32)
            BW = P.buf("Wc2")
            P.dma("sp", Wr[:], w_router.rearrange("(k p) e -> p k e", p=128), w=[BW], owner=BW)
            P.copy("dve", WrB[:, :, 0:8], Wr[:, :, 8:16], r=[BW], w=[BW])
            P.copy("dve", WrB[:, :, 8:16], Wr[:, :, 0:8], r=[BW], w=[BW])
            P.dma("sp", gffn[:], gffn_rep, w=[BW], owner=BW)
            xl = mkp("xl", [128, D], F32, 4)
            d0l = mkp("d0l", [128, D], F32, 4)
            d1l = mkp("d1l", [128, D], F32, 4)
            h1p = mkp("h1p", [128, D], F32, 4)
            jk2 = mkp("jk2", [128, D], BF16, 1)
            ss2 = mkp("ss2", [128, 4], F32, 4)
            xnp = mkp("xnp", [128, D], F32, 4)
            xnb = mkp("xnb", [128, D], BF16, 4)
            xnT = mkp("xnT", [128, 8, 128], F32, 4)
            lgp = mkp("lgp", [128, 16], F32, 4)
            smp = mkp("smp", [128, 4], F32, 4)
            afp = mkp("afp", [128, 16], F32, 4)
            def bodyC2(t):
                tok = slice(t * 128, (t + 1) * 128)
                g4, i4 = t // 4, t % 4
                xt, Bx = xl.next()
                P.dma("sp", xt[:], x[tok, :], w=[Bx])
                d0, Bd0 = d0l.next()
                P.dma("sp", d0[:], dall_d[g4, i4 * 128:(i4 + 1) * 128, :], r=[db("dall", g4)], w=[Bd0])
                d1, Bd1 = d1l.next()
                P.dma("sp", d1[:], dall_d[g4, 512 + i4 * 128:512 + (i4 + 1) * 128, :], r=[db("dall", g4)], w=[Bd1])
                h1, Bh1 = h1p.next()
                P.tt("dve", d0[:], d0[:], d1[:], ALU.add, r=[Bd0, Bd1], w=[Bd0])
                P.tt("dve", h1[:], d0[:], xt[:], ALU.add, r=[Bd0, Bx], w=[Bh1])
                P.dma("pool", h1_d[tok, :], h1[:], r=[Bh1], w=[db("h1", t)])
                jk, Bj = jk2.next()
                ss, Bss = ss2.next()
                P.act(jk[:], h1[:], AF.Square, accum_out=ss[:, 0:1], r=[Bh1], w=[Bj, Bss])
                rstd_from_ss(ss, Bss, 0, 1, 2, 1.0 / D)
                xn, Bxn = xnp.next()
                P.stt(xn[:], h1[:], ss[:, 2:3], gffn[:], ALU.mult, ALU.mult, r=[Bh1, Bss, BW], w=[Bxn])
                xb_, Bxb = xnb.next()
                P.copy("act", xb_[:], xn[:], r=[Bxn], w=[Bxb])
                P.dma("pool", xn_d[tok, :], xb_[:], r=[Bxb], w=[db("xn", t)])
                yield
                xT_, BxT = xnT.next()
                for half in range(2):
                    pst, Bpst = psF.next()
                    for cc in range(4):
                        c = half * 4 + cc
                        P.transpose(pst[:, cc * 128:(cc + 1) * 128], xn[:, c * 128:(c + 1) * 128], ident[:],
                                    r=[Bxn, Bconst], w=[Bpst])
                    P.copy("act", xT_[:, half * 4:(half + 1) * 4, :].rearrange("p c s -> p (c s)"), pst[:, 0:512],
                           r=[Bpst], w=[BxT])
                yield
                psl, Bpsl = psF.next()
                for c in range(8):
                    P.matmul(psl[:, 0:NEXP], xT_[:, c, :], (Wr if t < NT // 2 else WrB)[:, c, :], start=(c == 0),
                             stop=(c == 7), r=[BxT, BW], w=[Bpsl])
                lg, Blg = lgp.next()
                P.copy("dve", lg[:], psl[:, 0:NEXP], r=[Bpsl], w=[Blg])
                sm, Bsm = smp.next()
                P.op("dve", (lambda sm, lg: lambda e: e.reduce_max(out=sm[:, 0:1], in_=lg[:], axis=AX.X))(sm, lg),
                     r=[Blg], w=[Bsm])
                P.ts("dve", sm[:, 1:2], sm[:, 0:1], -1.0, None, op0=ALU.mult, r=[Bsm], w=[Bsm])
                af, Baf = afp.next()
                P.act(af[:], lg[:], AF.Exp, bias=sm[:, 1:2], accum_out=sm[:, 2:3], r=[Blg, Bsm], w=[Baf, Bsm])
                P.op("dve", (lambda sm: lambda e: e.reciprocal(out=sm[:, 3:4], in_=sm[:, 2:3]))(sm), r=[Bsm], w=[Bsm])
                P.ts("dve", af[:], af[:], sm[:, 3:4], None, op0=ALU.mult, r=[Baf, Bsm], w=[Baf])
                psf_, Bpsf = psF.next()
                P.transpose(psf_[0:NEXP, 0:128], af[:], ident[:], r=[Baf, Bconst], w=[Bpsf])
                if t < NT // 2:
                    P.copy("dve", affA[:, t * 128:(t + 1) * 128], psf_[0:NEXP, 0:128], r=[Bpsf], w=[BaffT])
                else:
                    P.copy("dve", affB[:, (t - NT // 2) * 128:(t - NT // 2 + 1) * 128], psf_[0:NEXP, 0:128], r=[Bpsf],
                           w=[BaffT])
            run_pipelined([bodyC2(t) for t in range(NT)], 1)
            P.barrier()
            P.emit()
            P.end_phase()

        phFw = ExitStack()
        Wpg = phFw.enter_context(nc.sbuf_tensor("Wpg", [128, 8, D], BF16))
        Wpl = phFw.enter_context(nc.sbuf_tensor("Wpl", [128, 2, D], BF16))
        gple = phFw.enter_context(nc.sbuf_tensor("gple", [128, 8], F32))
        gfin = phFw.enter_context(nc.sbuf_tensor("gfin", [128, D], F32))
        BWf = P.buf("Wf")
        phDE = ExitStack()
        wgp = TPool(P, phDE, nc, "wgp", [128, 8, D], BF16, 2)
        wup = TPool(P, phDE, nc, "wup", [128, 8, D], BF16, 2)
        wdp = TPool(P, phDE, nc, "wdp", [128, 8, D], BF16, 2)
        wst = TPool(P, phDE, nc, "wst", [128, D], F32, 3)
        cast_rr = [0]

        def load_w(e, engs):
            res = []
            for pool_, dram in ((wgp, w_eg), (wup, w_eu), (wdp, w_ed)):
                wt, Bw = pool_.next()
                for k in range(8):
                    st, Bst = wst.next()
                    P.dma("sp", st[:], dram[e, k * 128:(k + 1) * 128, :], w=[Bst])
                    ce = engs[cast_rr[0] % len(engs)]
                    cast_rr[0] += 1
                    P.copy(ce, wt[:, k, :], st[:], r=[Bst], w=[Bw])
                res.append((wt, Bw))
            return res

        with ExitStack() as ph:
            sbt = lambda n, s, d: ph.enter_context(nc.sbuf_tensor(P.uniq(n), s, d))
            mkp = lambda name, shape, dt, n, psum=False: TPool(P, ph, nc, name, shape, dt, n, psum)
            psF = mkp("psFd", [128, 512], F32, 2, psum=True)
            work = sbt("work", [NEXP, HS], F32)
            vals = sbt("vals", [NEXP, CAP], F32)
            idxs = sbt("idxs", [NEXP, CAP], U32)
            idxf = sbt("idxf", [NEXP, CAP], F32)
            tv = sbt("tv", [128, NJ, NEXP], F32)
            ti = sbt("ti", [128, NJ, NEXP], F32)
            itf = sbt("itf", [128, NJ, NEL], F32)
            rbp = mkp("rbp", [128, 16], F32, 2)
            wk = mkp("wkd", [128, 4, NEL], F32, 2)
            zt = sbt("zt", [128, D], F32)
            Bwork, Bvals, Bidx, Bidxf, Bitf, Bzt, Btv, Bti = [P.buf() for _ in range(8)]
            P.memset("pool", zt[:], 0.0, w=[Bzt])
            for t in range(NT):
                P.dma("pool", yacc_d[t * 128:(t + 1) * 128, :], zt[:], r=[Bzt], w=[db("yacc")], owner=Bzt)
            w_ready = [load_w(0, ("act",)), load_w(1, ("act",))]
            P.ts("dve", work[:], affA[:], rowmask[:, 0:1], op0=ALU.mult, r=[BaffT, Bconst], w=[Bwork])
            P.stt(work[:], affB[:], rowmask[:, 1:2], work[:], ALU.mult, ALU.add, r=[BaffT, Bconst, Bwork], w=[Bwork])
            for it in range(CAP // 8):
                sl = slice(it * 8, it * 8 + 8)
                P.op("dve", (lambda sl: lambda e: e.max(out=vals[:, sl], in_=work[:]))(sl), r=[Bwork], w=[Bvals])
                P.op("dve", (lambda sl: lambda e: e.max_index(out=idxs[:, sl], in_max=vals[:, sl], in_values=work[:]))(sl),
                     r=[Bwork, Bvals], w=[Bidx])
                P.op("dve", (lambda sl: lambda e: e.match_replace(out=work[:], in_to_replace=vals[:, sl],
                                                                 in_values=work[:], imm_value=-1.0))(sl),
                     r=[Bwork, Bvals], w=[Bwork])
            P.copy("dve", idxf[:], idxs[:], r=[Bidx], w=[Bidxf])
            for j in range(NJ):
                ps, Bps = psF.next()
                P.transpose(ps[:, 0:NEXP], vals[:, j * 128:(j + 1) * 128], ident[0:NEXP, 0:NEXP], r=[Bvals, Bconst], w=[Bps])
                P.copy("act", tv[:, j, :], ps[:, 0:NEXP], r=[Bps], w=[Btv])
                ps, Bps = psF.next()
                P.transpose(ps[:, 0:NEXP], idxf[:, j * 128:(j + 1) * 128], ident[0:NEXP, 0:NEXP], r=[Bidxf, Bconst], w=[Bps])
                P.copy("act", ti[:, j, :], ps[:, 0:NEXP], r=[Bps], w=[Bti])
            for j in range(NJ):
                jr = NJ - 1 - j
                ps, Bps = psF.next()
                P.matmul(ps[:, 0:8], Jm[:], tv[:, jr, 8:16], r=[Bconst, Btv], w=[Bps])
                P.matmul(ps[:, 8:16], Jm[:], ti[:, jr, 8:16], r=[Bconst, Bti], w=[Bps])
                rb, Brb = rbp.next()
                P.copy("act", rb[:], ps[:, 0:16], r=[Bps], w=[Brb])
                w4, Bw4 = wk.next()
                P.tt("dve", w4[:, 0, :], tv[:, j, 0:8], rb[:, 0:8], ALU.is_gt, r=[Btv, Brb], w=[Bw4])
                P.tt("dve", valsT[:, j, :], tv[:, j, 0:8], rb[:, 0:8], ALU.max, r=[Btv, Brb], w=[Bsel])
                P.ts("dve", w4[:, 1, :], rb[:, 8:16], float(HS), None, op0=ALU.add, r=[Brb], w=[Bw4])
                P.tt("dve", w4[:, 2, :], ti[:, j, 0:8], w4[:, 1, :], ALU.subtract, r=[Bti, Bw4], w=[Bw4])
                P.tt("dve", w4[:, 3, :], w4[:, 2, :], w4[:, 0, :], ALU.mult, r=[Bw4], w=[Bw4])
                P.tt("dve", itf[:, j, :], w4[:, 3, :], w4[:, 1, :], ALU.add, r=[Bw4], w=[Bitf])
            P.copy("dve", idxT[:], itf[:], r=[Bitf], w=[Bsel])
            if debug:
                P.dma("sp", idx_dbg, idxT[:].rearrange("p j e -> p (j e)"), r=[Bsel], w=[db("idxdbg")], owner=Bidx)
                P.dma("sp", val_dbg, valsT[:].rearrange("p j e -> p (j e)"), r=[Bsel], w=[db("valdbg")], owner=Bvals)
            P.barrier()
            P.emit()
            P.end_phase()

        with ExitStack() as ph:
            sbt = lambda n, s, d: ph.enter_context(nc.sbuf_tensor(P.uniq(n), s, d))
            mkp = lambda name, shape, dt, n, psum=False: TPool(P, ph, nc, name, shape, dt, n, psum)
            psF = mkp("psFe", [128, 512], F32, 6, psum=True)
            psB = mkp("psBe", [128, 1024], BF16, 2, psum=True)
            xgp = mkp("xgp", [128, NJ, D], BF16, 2)
            xgT = mkp("xgT", [128, 8, CAP], BF16, 1)
            sgp = mkp("sgp", [128, CAP], F32, 2)
            hdp = mkp("hdp", [128, 8, CAP], BF16, 2)
            ysb = mkp("ysb", [128, D], F32, 2)
            def gather(e):
                xg, Bxg = xgp.next()
                for j in range(NJ):
                    P.dma_fn("pool", (lambda xg, j, e: lambda en: en.indirect_dma_start(
                        out=xg[:, j, :], out_offset=None, in_=xn_d,
                        in_offset=bass.IndirectOffsetOnAxis(ap=idxT[:, j, e:e + 1], axis=0)))(xg, j, e),
                        r=[Bsel] + [db("xn", tt_) for tt_ in range(NT)], w=[Bxg])
                return xg, Bxg

            P.dma("pool", Wpg[:], w_pg.rearrange("(k p) c -> p k c", p=128), w=[BWf], owner=BWf)
            P.dma("pool", Wpl[:], w_ple.rearrange("(k p) c -> p k c", p=128), w=[BWf], owner=BWf)
            P.dma("sp", gple[:], gple_l, w=[BWf], owner=BWf)
            P.dma("sp", gfin[:], gfin_rep, w=[BWf], owner=BWf)
            for c in range(8):
                if c % 2 == 0:
                    P.ts("dve", Wpg[:, c, :], Wpg[:, c, :], gple[:, c:c + 1], op0=ALU.mult, r=[BWf], w=[BWf])
                else:
                    P.act(Wpg[:, c, :], Wpg[:, c, :], AF.Copy, scale=gple[:, c:c + 1], r=[BWf], w=[BWf])
            nxt_g = gather(0)
            for e in range(NEL):
                (wg, Bwg), (wu, Bwu), (wd, Bwd) = w_ready.pop(0)
                xg, Bxg = nxt_g
                if e + 1 < NEL:
                    nxt_g = gather(e + 1)
                xT_, BxT = xgT.next()
                for c in range(8):
                    pT, BpT = psB.next()
                    for j in range(NJ):
                        P.transpose(pT[:, j * 128:(j + 1) * 128], xg[:, j, c * 128:(c + 1) * 128], identb[:],
                                    r=[Bxg, Bconst], w=[BpT])
                    P.copy("dve" if c % 2 == 0 else "act", xT_[:, c, :], pT[:, 0:CAP], r=[BpT], w=[BxT])
                hd, Bhd = hdp.next()
                for fc in range(8):
                    psg, Bpsg = psF.next()
                    psu, Bpsu = psF.next()
                    for c in range(8):
                        P.matmul(psg[:, 0:CAP], wg[:, c, fc * 128:(fc + 1) * 128], xT_[:, c, :], start=(c == 0),
                                 stop=(c == 7), r=[Bwg, BxT], w=[Bpsg])
                    for c in range(8):
                        P.matmul(psu[:, 0:CAP], wu[:, c, fc * 128:(fc + 1) * 128], xT_[:, c, :], start=(c == 0),
                                 stop=(c == 7), r=[Bwu, BxT], w=[Bpsu])
                    sg, Bsg = sgp.next()
                    P.act(sg[:], psg[:, 0:CAP], AF.Silu, r=[Bpsg], w=[Bsg])
                    P.tt("dve", hd[:, fc, :], sg[:], psu[:, 0:CAP], ALU.mult, r=[Bsg, Bpsu], w=[Bhd])
                for j in range(NJ):
                    ys, Bys = ysb.next()
                    for half in range(2):
                        psy, Bpsy = psF.next()
                        for fc in range(8):
                            P.matmul(psy[:, 0:512], hd[:, fc, j * 128:(j + 1) * 128], wd[:, fc, half * 512:(half + 1) * 512],
                                     start=(fc == 0), stop=(fc == 7), r=[Bhd, Bwd], w=[Bpsy])
                        if half == 0:
                            P.ts("dve", ys[:, 0:512], psy[:, 0:512], valsT[:, j, e:e + 1], None, op0=ALU.mult,
                                 r=[Bpsy, Bsel], w=[Bys])
                        else:
                            P.act(ys[:, 512:1024], psy[:, 0:512], AF.Copy, scale=valsT[:, j, e:e + 1],
                                  r=[Bpsy, Bsel], w=[Bys])
                    P.dma_fn("pool", (lambda ys, j, e: lambda en: en.indirect_dma_start(
                        out=yacc_d, out_offset=bass.IndirectOffsetOnAxis(ap=idxT[:, j, e:e + 1], axis=0),
                        in_=ys[:], in_offset=None, compute_op=ALU.add))(ys, j, e),
                        r=[Bys, Bsel, db("yacc")], w=[db("yacc")])
                if e + 2 < NEL:
                    w_ready.append(load_w(e + 2, ("dve", "act")))
            P.barrier()
            P.emit()
            P.end_phase()

        phDE.close()
        with ExitStack() as ph:
            sbt = lambda n, s, d: ph.enter_context(nc.sbuf_tensor(P.uniq(n), s, d))
            mkp = lambda name, shape, dt, n, psum=False: TPool(P, ph, nc, name, shape, dt, n, psum)
            psF = mkp("psFf", [128, 512], F32, 6, psum=True)
            psB = mkp("psBf", [128, 1024], BF16, 2, psum=True)
            BW = BWf
            Bcc2 = P.buf("ccy")
            for g4 in range(NG):
                P.dma_fn("pool", (lambda g4: lambda e: e.collective_compute(
                    "AllGather", op=ALU.bypass, replica_groups=GROUPS,
                    ins=[yacc_d[g4 * 512:(g4 + 1) * 512, :]], outs=[yall_d[g4]]))(g4),
                    r=[db("yacc")], w=[db("yall", g4)], owner=P.buf(f"ccy{g4}"), inc=1)

            hp_ = mkp("hp_", [128, D], F32, 4)
            y0l = mkp("y0l", [128, D], F32, 4)
            y1l = mkp("y1l", [128, D], F32, 4)
            pp_ = mkp("pp_", [128, 256], F32, 4)
            pb_ = mkp("pb_", [128, 256], BF16, 4)
            pTp = mkp("pTp", [128, 2, 128], BF16, 4)
            jk3 = mkp("jk3", [128, D], BF16, 1)
            ss3 = mkp("ss3", [128, 8], F32, 4)
            ab3 = mkp("ab3", [128, D], BF16, 4)
            aT3 = mkp("aT3", [128, D], BF16, 4)
            gtp = mkp("gtp", [128, D], F32, 4)
            h3p = mkp("h3p", [128, D], F32, 4)
            outp = mkp("outp", [128, D], F32, 4)
            def bodyF(t):
                tok = slice(t * 128, (t + 1) * 128)
                g4, i4 = t // 4, t % 4
                h2, Bh2 = hp_.next()
                P.dma("sp", h2[:], h1_d[tok, :], r=[db("h1", t)], w=[Bh2])
                y0, By0 = y0l.next()
                P.dma("sp", y0[:], yall_d[g4, i4 * 128:(i4 + 1) * 128, :], r=[db("yall", g4)], w=[By0])
                y1, By1 = y1l.next()
                P.dma("sp", y1[:], yall_d[g4, 512 + i4 * 128:512 + (i4 + 1) * 128, :], r=[db("yall", g4)], w=[By1])
                pt_, Bp = pp_.next()
                P.dma("sp", pt_[:], pin[tok, :], w=[Bp])
                P.tt("dve", y0[:], y0[:], y1[:], ALU.add, r=[By0, By1], w=[By0])
                P.tt("dve", h2[:], h2[:], y0[:], ALU.add, r=[Bh2, By0], w=[Bh2])
                jk, Bj = jk3.next()
                ss, Bss = ss3.next()
                P.act(jk[:], h2[:], AF.Square, accum_out=ss[:, 0:1], r=[Bh2], w=[Bj, Bss])
                rstd_from_ss(ss, Bss, 0, 1, 2, 1.0 / D)
                ab, Bab = ab3.next()
                P.act(ab[:], h2[:], AF.Copy, scale=ss[:, 2:3], r=[Bh2, Bss], w=[Bab])
                yield
                pT, BpT = psB.next()
                for c in range(8):
                    P.transpose(pT[:, c * 128:(c + 1) * 128], ab[:, c * 128:(c + 1) * 128], identb[:], r=[Bab, Bconst], w=[BpT])
                aT, BaT = aT3.next()
                P.copy("dve", aT[:], pT[:], r=[BpT], w=[BaT])
                pb, Bpb = pb_.next()
                P.copy("act", pb[:], pt_[:], r=[Bp], w=[Bpb])
                pT2, BpT2 = psB.next()
                for c in range(2):
                    P.transpose(pT2[:, c * 128:(c + 1) * 128], pb[:, c * 128:(c + 1) * 128], identb[:], r=[Bpb, Bconst], w=[BpT2])
                pTt, BpTt = pTp.next()
                P.copy("dve", pTt[:].rearrange("p c s -> p (c s)"), pT2[:, 0:256], r=[BpT2], w=[BpTt])
                yield
                gt, Bgt = gtp.next()
                h3, Bh3 = h3p.next()
                for half in range(2):
                    hs = slice(half * 512, (half + 1) * 512)
                    psg, Bpsg = psF.next()
                    for c in range(8):
                        P.matmul(psg[:, 0:512], aT[:, c * 128:(c + 1) * 128], Wpg[:, c, hs], start=(c == 0), stop=(c == 7),
                                 r=[BaT, BW], w=[Bpsg])
                    P.act(gt[:, hs], psg[:, 0:512], AF.Sigmoid, r=[Bpsg], w=[Bgt])
                    psp, Bpsp = psF.next()
                    for c in range(2):
                        P.matmul(psp[:, 0:512], pTt[:, c, :], Wpl[:, c, hs], start=(c == 0), stop=(c == 1), r=[BpTt, BW], w=[Bpsp])
                    P.tt("dve", gt[:, hs], gt[:, hs], psp[:, 0:512], ALU.mult, r=[Bgt, Bpsp], w=[Bgt])
                    P.tt("dve", h3[:, hs], gt[:, hs], h2[:, hs], ALU.add, r=[Bgt, Bh2], w=[Bh3])
                yield
                jk, Bj = jk3.next()
                P.act(jk[:], h3[:], AF.Square, accum_out=ss[:, 4:5], r=[Bh3], w=[Bj, Bss])
                rstd_from_ss(ss, Bss, 4, 5, 6, 1.0 / D)
                ot, Bot = outp.next()
                P.stt(ot[:], h3[:], ss[:, 6:7], gfin[:], ALU.mult, ALU.mult, r=[Bh3, Bss, BW], w=[Bot])
                P.dma("pool", y[tok, :], ot[:], r=[Bot], w=[db("y", t)])
            run_pipelined([bodyF(t) for t in range(NT)], 1)
            P.barrier()
            P.emit()
            P.end_phase()
        phFw.close()
        print("instructions recorded:", P.ninst, "sems:", len(P.sems))
    return nc


def make_inputs(S, b, r, x, p, positions, norm_mix, w_in, gla_gate_up_fwd, gla_gate_bias_fwd, gla_gate_up_bwd,
                gla_gate_bias_bwd, attn_sink, gla_norm, w_branch_attn, w_branch_gla, w_out, norm_ffn, w_router,
                w_exp_gate, w_exp_up, w_exp_down, norm_ple, w_ple_gate, w_ple, norm_final):
    f = lambda a: np.ascontiguousarray(np.asarray(a, dtype=np.float32))
    col8 = lambda v: f(np.asarray(v).reshape(-1, 128).T)
    NT = S // 128
    W = np.asarray(w_in[0])
    o_aq, o_ak, o_av, o_gq, o_gk, o_gv, o_gr, o_zf, o_zb, o_ga, o_gg = (
        0, 512, 640, 768, 1280, 1792, 2816, 3840, 3856, 3872, 4896)
    cols = np.concatenate([
        np.arange(o_aq + 256 * r, o_aq + 256 * (r + 1)),
        np.arange(o_ak + 64 * r, o_ak + 64 * (r + 1)),
        np.arange(o_av + 64 * r, o_av + 64 * (r + 1)),
        np.arange(o_gq + 256 * r, o_gq + 256 * (r + 1)),
        np.arange(o_gk + 256 * r, o_gk + 256 * (r + 1)),
        np.arange(o_gv + 512 * r, o_gv + 512 * (r + 1)),
        np.arange(o_gr + 512 * r, o_gr + 512 * (r + 1)),
        np.arange(o_zf, o_zf + 16), np.arange(o_zb, o_zb + 16),
        np.arange(o_ga, o_ga + 1024), np.arange(o_gg, o_gg + 1024)])
    assert cols.size == NCOL
    dk = slice(256 * r, 256 * (r + 1))
    upf = np.concatenate([np.asarray(gla_gate_up_fwd[0])[:, dk], np.asarray(gla_gate_bias_fwd[0])[None, dk]], 0)
    upb = np.concatenate([np.asarray(gla_gate_up_bwd[0])[:, dk], np.asarray(gla_gate_bias_bwd[0])[None, dk]], 0)
    eperm = np.concatenate([np.arange(8 * r, 8 * (r + 1)), np.arange(8 * (1 - r), 8 * (2 - r))])
    es = slice(8 * r, 8 * (r + 1))
    return {
        "x": f(x[b]),
        "p": f(p[0, b]),
        "pos_l": np.ascontiguousarray(np.asarray(positions[b], dtype=np.int32).reshape(NT, 128).T),
        "invf_rep": f(np.broadcast_to((500000.0 ** (-np.arange(0, 16, 2, dtype=np.float32) / 16.0))[None, :], (128, 8))),
        "gmix_l": col8(norm_mix[0]),
        "gple_l": col8(norm_ple[0]),
        "ggla_l": col8(np.asarray(gla_norm[0])[512 * r:512 * (r + 1)]),
        "gffn_rep": f(np.broadcast_to(np.asarray(norm_ffn[0])[None, :], (128, D))),
        "gfin_rep": f(np.broadcast_to(np.asarray(norm_final)[None, :], (128, D))),
        "sink_rep": f(np.repeat(np.asarray(attn_sink[0])[4 * r:4 * (r + 1)], 128)[None, :]),
        "upext_f": f(upf),
        "upext_b": f(upb),
        "w_in": f(W[:, cols]),
        "w_ba": f(np.asarray(w_branch_attn[0])[256 * r:256 * (r + 1)]),
        "w_bg": f(np.asarray(w_branch_gla[0])[512 * r:512 * (r + 1)]),
        "w_out": f(w_out[0]),
        "w_router": f(np.asarray(w_router[0])[:, eperm]),
        "rowmask": f(np.stack([np.arange(16) < 8, np.arange(16) >= 8], 1)),
        "w_eg": f(np.asarray(w_exp_gate[0])[es]),
        "w_eu": f(np.asarray(w_exp_up[0])[es]),
        "w_ed": f(np.asarray(w_exp_down[0])[es]),
        "w_pg": f(w_ple_gate[0]),
        "w_ple": f(w_ple[0]),
    }


def kernel(**inputs):
    x = np.asarray(inputs["x"])
    B, S, _ = x.shape
    nc = build(S)
    in_maps = []
    for c in range(8):
        in_maps.append(make_inputs(S, (c // 2) % B, c % 2, **inputs))
    res = run_bass_kernel_spmd(nc, in_maps, core_ids=list(range(8)))
    out = np.stack([np.asarray(res.results[2 * b]["y"], dtype=np.float32) for b in range(B)], 0)
    return out
```

```python
import math
import numpy as np
from contextlib import ExitStack
import concourse.bass as bass
import concourse.mybir as mybir
from concourse.bass_utils import run_bass_kernel_spmd

F32 = mybir.dt.float32
BF16 = mybir.dt.bfloat16
I32 = mybir.dt.int32
U32 = mybir.dt.uint32
AF = mybir.ActivationFunctionType
ALU = mybir.AluOpType
AX = mybir.AxisListType

D = 1024
IN_DIM = 5920
EPS = 1e-6
NEXP = 16
C_AQ, C_AK, C_AV, C_GQ, C_GK, C_GV, C_GR, C_ZF, C_ZB, C_GA, C_GG = (
    0, 512, 640, 768, 1280, 1792, 2816, 3840, 3856, 3872, 4896)


class Buf:
    __slots__ = ("name", "w", "r", "dkey", "dkey_sw", "dram")

    def __init__(self, name, dram=False):
        self.name = name
        self.w = None
        self.r = {}
        self.dkey = None
        self.dkey_sw = None
        self.dram = dram


class Prog:
    ENGS = ("pe", "act", "dve", "pool", "sp")

    def __init__(self, nc, es):
        self.nc = nc
        self.es = es
        self.streams = {e: [] for e in self.ENGS}
        self.cnt = {e: 0 for e in self.ENGS}
        self.known = {e: {} for e in self.ENGS}
        self.dtot = {}
        self.sems = {}
        self.nbuf = 0
        self.ninst = 0
        self.free_keys = []
        self.free_keys_sw = []
        self.phase_owners = []
        for e in self.ENGS:
            self.sem(e)

    def sem(self, key):
        if key not in self.sems:
            self.sems[key] = self.es.enter_context(self.nc.semaphore(f"s_{key}"))
        return self.sems[key]

    def uniq(self, n):
        self.nbuf += 1
        return f"{n}_u{self.nbuf}"

    def buf(self, name=None):
        self.nbuf += 1
        return Buf(name or f"b{self.nbuf}")

    def _deps(self, eng, reads, writes):
        need = {}

        def add(k, v):
            if k == eng and eng == "pe":
                return
            if need.get(k, 0) < v:
                need[k] = v

        for b in reads:
            if b.w is not None:
                add(*b.w)
        for b in writes:
            if b.w is not None:
                add(*b.w)
            for k, v in b.r.items():
                add(k, v)
        waits = []
        kn = self.known[eng]
        for k, v in need.items():
            if k in self.dtot:
                v = self.dtot[k]
            if kn.get(k, 0) >= v:
                continue
            kn[k] = v
            waits.append((k, v))
        return waits

    def _mark(self, ev, reads, writes):
        k, v = ev
        for b in reads:
            if b.r.get(k, 0) < v:
                b.r[k] = v
        for b in writes:
            b.w = ev
            b.r = {}

    def op(self, eng, fn, r=(), w=()):
        waits = self._deps(eng, r, w)
        self.cnt[eng] += 1
        ev = (eng, self.cnt[eng])
        self.streams[eng].append((waits, fn, (eng, 1)))
        self._mark(ev, r, w)
        self.ninst += 1

    def dma_fn(self, q, fn, r=(), w=(), owner=None, inc=16):
        if owner is None:
            cands = [b for b in w if not b.dram] or [b for b in r if not b.dram]
            owner = cands[0]
        attr = "dkey_sw" if q == "pool" else "dkey"
        if getattr(owner, attr) is None:
            fk = self.free_keys_sw if q == "pool" else self.free_keys
            if fk:
                setattr(owner, attr, fk.pop())
            else:
                k_ = f"d{len(self.dtot)}"
                self.dtot[k_] = 0
                self.sem(k_)
                setattr(owner, attr, k_)
            self.phase_owners.append((owner, q == "pool"))
        okey = getattr(owner, attr)
        waits = self._deps(q, r, w)
        self.dtot[okey] += inc
        ev = (okey, self.dtot[okey])
        self.streams[q].append((waits, fn, (okey, inc)))
        self._mark(ev, r, w)
        self.ninst += 1

    def barrier(self):
        for e in self.ENGS:
            waits = []
            kn = self.known[e]
            for k in self.ENGS:
                v = self.cnt[k]
                if k == e or v == 0 or kn.get(k, 0) >= v:
                    continue
                kn[k] = v
                waits.append((k, v))
            for k, v in self.dtot.items():
                if v == 0 or kn.get(k, 0) >= v:
                    continue
                kn[k] = v
                waits.append((k, v))
            if waits:
                self.streams[e].append((waits, None, None))

    def end_phase(self):
        for o, sw in self.phase_owners:
            if sw:
                self.free_keys_sw.append(o.dkey_sw)
                o.dkey_sw = None
            else:
                self.free_keys.append(o.dkey)
                o.dkey = None
        self.phase_owners = []

    def emit(self):
        nc = self.nc
        streams = self.streams
        sems = self.sems

        def run(name):
            def f(e):
                for waits, fn, inc in streams[name]:
                    for k, v in waits:
                        e.wait_ge(sems[k], v)
                    if fn is None:
                        continue
                    ins = fn(e)
                    if inc is not None:
                        ins.then_inc(sems[inc[0]], inc[1])
            return f

        with nc.Block() as block:
            block.tensor(run("pe"))
            block.scalar(run("act"))
            block.vector(run("dve"))
            block.gpsimd(run("pool"))
            block.sync(run("sp"))
        self.streams = {e: [] for e in self.ENGS}

    def dma(self, q, out, in_, r=(), w=(), owner=None):
        self.dma_fn(q, lambda e: e.dma_start(out=out, in_=in_), r, w, owner)

    def matmul(self, out, lhsT, rhs, start=True, stop=True, r=(), w=()):
        self.op("pe", lambda e: e.matmul(out, lhsT=lhsT, rhs=rhs, start=start, stop=stop), r, w)

    def transpose(self, out, in_, ident, r=(), w=()):
        self.op("pe", lambda e: e.transpose(out, in_, ident), r, w)

    def act(self, out, in_, func, r=(), w=(), bias=None, scale=None, accum_out=None):
        kw = {}
        if bias is not None:
            kw["bias"] = bias
        if scale is not None:
            kw["scale"] = scale
        if accum_out is not None:
            kw["accum_out"] = accum_out
        self.op("act", lambda e: e.activation(out=out, in_=in_, func=func, **kw), r, w)

    def tt(self, eng, out, in0, in1, op, r=(), w=()):
        self.op(eng, lambda e: e.tensor_tensor(out=out, in0=in0, in1=in1, op=op), r, w)

    def ts(self, eng, out, in0, s1, s2=None, op0=ALU.mult, op1=None, r=(), w=()):
        if op1 is None:
            self.op(eng, lambda e: e.tensor_scalar(out=out, in0=in0, scalar1=s1, scalar2=None, op0=op0), r, w)
        else:
            self.op(eng, lambda e: e.tensor_scalar(out=out, in0=in0, scalar1=s1, scalar2=s2, op0=op0, op1=op1), r, w)

    def stt(self, out, in0, scalar, in1, op0, op1, r=(), w=()):
        self.op("dve", lambda e: e.scalar_tensor_tensor(out=out, in0=in0, scalar=scalar, in1=in1, op0=op0, op1=op1), r, w)

    def copy(self, eng, out, in_, r=(), w=()):
        if eng == "act":
            self.op("act", lambda e: e.activation(out=out, in_=in_, func=AF.Copy), r, w)
        else:
            self.op(eng, lambda e: e.tensor_copy(out=out, in_=in_), r, w)

    def memset(self, eng, ap, val, w=()):
        self.op(eng, lambda e: e.memset(ap, val), (), w)


class TPool:
    def __init__(self, P, ph, nc, name, shape, dtype, n, psum=False):
        self.P = P
        self.items = []
        P.nbuf += 1
        name = f"{name}_u{P.nbuf}_"
        for i in range(n):
            if psum:
                t = ph.enter_context(nc.psum_tensor(f"{name}{i}", shape, dtype))
            else:
                t = ph.enter_context(nc.sbuf_tensor(f"{name}{i}", shape, dtype))
            self.items.append((t, P.buf(f"{name}{i}")))
        self.i = 0
        self.owner = P.buf(name + "_own")

    def next(self):
        t = self.items[self.i % len(self.items)]
        self.i += 1
        return t


NCOL = 4000
C_AQ, C_AK, C_AV, C_GQ, C_GK, C_GV, C_GR, C_ZF, C_ZB, C_GA, C_GG = (
    0, 256, 320, 384, 640, 896, 1408, 1920, 1936, 1952, 2976)
NEL = 8


def run_pipelined(gens, gap):
    active = []
    it = iter(gens)
    rnd = 0
    exhausted = False
    while True:
        if not exhausted and rnd % gap == 0:
            g = next(it, None)
            if g is None:
                exhausted = True
            else:
                active.append(g)
        if exhausted and not active:
            break
        for g in list(active):
            try:
                next(g)
            except StopIteration:
                active.remove(g)
        rnd += 1
GROUPS = [[0, 1], [2, 3], [4, 5], [6, 7]]


def build(S, debug=False):
    NT = S // 128
    CAP = S // 8
    NJ = CAP // 128
    NG = S // 512
    assert NJ >= 1
    nc = bass.Bass("TRN2", target_bir_lowering=False)

    def din(name, shape, dt=F32):
        return nc.dram_tensor(name, shape, dt, kind="ExternalInput").ap()

    def dscr(name, shape, dt, dbg=True):
        return nc.dram_tensor(name, shape, dt, kind="ExternalOutput" if (debug and dbg) else "Internal").ap()

    x = din("x", [S, D])
    pin = din("p", [S // 2, 256])
    rowidx_in = din("rowidx", [128, 3 * (S // 256)], I32)
    pos_l = din("pos_l", [128, NT], I32)
    invf_rep = din("invf_rep", [128, 8])
    gmix_l = din("gmix_l", [128, 8])
    gple_l = din("gple_l", [128, 8])
    ggla_l = din("ggla_l", [128, 4])
    gffn_rep = din("gffn_rep", [128, D])
    gfin_rep = din("gfin_rep", [128, D])
    sink_rep = din("sink_rep", [1, 512])
    upext_f = din("upext_f", [17, 256])
    upext_b = din("upext_b", [17, 256])
    w_in = din("w_in", [D, NCOL])
    w_ba = din("w_ba", [256, D])
    w_bg = din("w_bg", [512, D])
    w_out = din("w_out", [D, D])
    w_router = din("w_router", [D, NEXP])
    rowmask_in = din("rowmask", [NEXP, 2])
    w_eg = din("w_eg", [NEL, D, D])
    w_eu = din("w_eu", [NEL, D, D])
    w_ed = din("w_ed", [NEL, D, D])
    w_pg = din("w_pg", [D, D])
    w_ple = din("w_ple", [256, D])
    y = nc.dram_tensor("y", [S // 2, D], F32, kind="ExternalOutput").ap()

    qT_d = dscr("qT_d", [256, S], BF16)
    kT_d = dscr("kT_d", [64, S], BF16)
    va_d = dscr("va_d", [S, 64], BF16)
    gqT_d = dscr("gqT_d", [256, S], BF16)
    gkT_d = dscr("gkT_d", [256, S], BF16)
    gk_d = dscr("gk_d", [S, 256], BF16)
    gv_d = dscr("gv_d", [S, 512], BF16)
    grT_d = dscr("grT_d", [512, S], BF16)
    gaT_d = dscr("gaT_d", [1024, S], BF16)
    ggT_d = dscr("ggT_d", [1024, S], BF16)
    laf_d = dscr("laf_d", [S, 256], F32)
    lab_d = dscr("lab_d", [S, 256], F32)
    obT_d = dscr("obT_d", [512, S], F32)
    dpart_d = dscr("dpart_d", [S, D], F32, dbg=False)
    dall_d = dscr("dall_d", [NG, 1024, D], F32, dbg=False)
    h1_d = dscr("h1_d", [S, D], F32)
    xn_d = dscr("xn_d", [S, D], BF16)
    yacc_d = dscr("yacc_d", [S, D], F32, dbg=False)
    yall_d = dscr("yall_d", [NG, 1024, D], F32, dbg=False)
    if debug:
        idx_dbg = dscr("idx_dbg", [128, NJ * NEL], U32)
        val_dbg = dscr("val_dbg", [128, NJ * NEL], F32)

    def fm(dram, c0, nch, t):
        return dram[c0 * 128:(c0 + nch) * 128, t * 128:(t + 1) * 128].rearrange("(c p) s -> p c s", p=128)

    with ExitStack() as es:
        P = Prog(nc, es)
        dbufs = {}

        def db(name, idx=0):
            k = (name, idx)
            if k not in dbufs:
                dbufs[k] = Buf(f"{name}_{idx}", dram=True)
            return dbufs[k]

        pers = lambda n, s, d: es.enter_context(nc.sbuf_tensor(n, s, d))
        ident = pers("ident", [128, 128], F32)
        identb = pers("identb", [128, 128], BF16)
        mLE = pers("mLE", [128, 128], F32)
        mGT = pers("mGT", [128, 128], F32)
        mGE = pers("mGE", [128, 128], F32)
        mLT = pers("mLT", [128, 128], F32)
        onesb = pers("onesb", [128, 128], BF16)
        onesf = pers("onesf", [1, 128], F32)
        epsc = pers("epsc", [128, 1], F32)
        HS = S // 2
        affA = pers("affA", [NEXP, HS], F32)
        affB = pers("affB", [NEXP, HS], F32)
        Jm = pers("Jm", [128, 128], F32)
        rowmask = pers("rowmask_sb", [NEXP, 2], F32)
        Bconst = P.buf("const")
        BaffT = P.buf("affT")
        valsT = pers("valsT", [128, NJ, NEL], F32)
        idxT = pers("idxT", [128, NJ, NEL], U32)
        Bsel = P.buf("sel")

        def mk_mask(tile, step, cm, cmp):
            P.memset("pool", tile[:], 1.0, w=[Bconst])
            P.op("pool", lambda e: e.affine_select(out=tile[:], in_=tile[:], pattern=[[step, 128]],
                                                   compare_op=cmp, fill=0.0, base=0, channel_multiplier=cm),
                 r=[Bconst], w=[Bconst])

        mk_mask(ident, -1, 1, ALU.is_equal)
        mk_mask(mLE, 1, -1, ALU.is_ge)
        mk_mask(mGT, -1, 1, ALU.is_gt)
        mk_mask(mGE, -1, 1, ALU.is_ge)
        mk_mask(mLT, 1, -1, ALU.is_gt)
        P.copy("pool", identb[:], ident[:], r=[Bconst], w=[Bconst])
        P.memset("pool", onesb[:], 1.0, w=[Bconst])
        P.memset("pool", onesf[:], 1.0, w=[Bconst])
        P.memset("pool", epsc[:], EPS, w=[Bconst])
        P.memset("pool", Jm[:], 1.0, w=[Bconst])
        P.op("pool", lambda e: e.affine_select(out=Jm[:], in_=Jm[:], pattern=[[1, 128]], compare_op=ALU.is_equal,
                                               fill=0.0, base=-127, channel_multiplier=1), r=[Bconst], w=[Bconst])
        P.dma("sp", rowmask[:], rowmask_in, w=[Bconst], owner=Bconst)

        def rstd_from_ss(ss, Bss, a, b_, c_, scale):
            P.act(ss[:, b_:b_ + 1], ss[:, a:a + 1], AF.Sqrt, scale=scale, bias=epsc[:, 0:1], r=[Bss, Bconst], w=[Bss])
            P.op("dve", (lambda ss_: lambda e: e.reciprocal(out=ss_[:, c_:c_ + 1], in_=ss_[:, b_:b_ + 1]))(ss),
                 r=[Bss], w=[Bss])

        with ExitStack() as ph:
            sbt = lambda n, s, d: ph.enter_context(nc.sbuf_tensor(P.uniq(n), s, d))
            mkp = lambda name, shape, dt, n, psum=False: TPool(P, ph, nc, name, shape, dt, n, psum)
            Win = sbt("Win", [128, 8, NCOL], BF16)
            BWin = P.buf("Win")
            gmix = sbt("gmix", [128, 8], F32)
            Bg = P.buf("gmix")
            upf = sbt("upf", [17, 256], F32)
            upb = sbt("upb", [17, 256], F32)
            posi = sbt("posi", [128, NT], I32)
            posf = sbt("posf", [128, NT], F32)
            invf = sbt("invf", [128, 8], F32)
            ang = sbt("ang", [128, NT, 8], F32)
            cosT = sbt("cosT", [128, NT, 8], F32)
            sinT = sbt("sinT", [128, NT, 8], F32)
            Brope = P.buf("rope")
            Bup = P.buf("up")
            for c in range(8):
                P.dma("pool", Win[:, c, :], w_in[c * 128:(c + 1) * 128, :], w=[BWin], owner=BWin)
            P.dma("sp", gmix[:], gmix_l, w=[Bg], owner=Bg)
            P.dma("sp", upf[:], upext_f, w=[Bup], owner=Bup)
            P.dma("sp", upb[:], upext_b, w=[Bup], owner=Bup)
            P.dma("sp", posi[:], pos_l, w=[Brope], owner=Brope)
            for c in range(8):
                if c % 2 == 0:
                    P.ts("dve", Win[:, c, :], Win[:, c, :], gmix[:, c:c + 1], op0=ALU.mult, r=[Bg, BWin], w=[BWin])
                else:
                    P.act(Win[:, c, :], Win[:, c, :], AF.Copy, scale=gmix[:, c:c + 1], r=[Bg, BWin], w=[BWin])
            P.dma("sp", invf[:], invf_rep, w=[Brope], owner=Brope)
            P.copy("dve", posf[:], posi[:], r=[Brope], w=[Brope])
            for t in range(NT):
                P.ts("dve", ang[:, t, :], invf[:], posf[:, t:t + 1], op0=ALU.mult, r=[Brope], w=[Brope])
            TWO_PI = 2.0 * math.pi
            angf = ang[:].rearrange("p t f -> p (t f)")
            tmpa = sbt("tmpa", [128, NT * 8], F32)
            tmpb = sbt("tmpb", [128, NT * 8], F32)
            tmpk = sbt("tmpk", [128, NT * 8], I32)
            for (outT, shift) in ((sinT, 0.0), (cosT, 0.5 * math.pi)):
                P.ts("dve", tmpa[:], angf, shift, None, op0=ALU.add, r=[Brope], w=[Brope])
                P.ts("dve", tmpb[:], tmpa[:], 1.0 / TWO_PI, None, op0=ALU.mult, r=[Brope], w=[Brope])
                P.copy("dve", tmpk[:], tmpb[:], r=[Brope], w=[Brope])
                P.copy("dve", tmpb[:], tmpk[:], r=[Brope], w=[Brope])
                P.stt(tmpa[:], tmpb[:], -TWO_PI, tmpa[:], ALU.mult, ALU.add, r=[Brope], w=[Brope])
                P.ts("dve", tmpb[:], tmpa[:], math.pi, None, op0=ALU.is_gt, r=[Brope], w=[Brope])
                P.stt(tmpa[:], tmpb[:], -TWO_PI, tmpa[:], ALU.mult, ALU.add, r=[Brope], w=[Brope])
                P.ts("dve", tmpb[:], tmpa[:], -math.pi, None, op0=ALU.is_lt, r=[Brope], w=[Brope])
                P.stt(tmpa[:], tmpb[:], TWO_PI, tmpa[:], ALU.mult, ALU.add, r=[Brope], w=[Brope])
                P.act(outT[:].rearrange("p t f -> p (t f)"), tmpa[:], AF.Sin, r=[Brope], w=[Brope])

            psF = mkp("psF", [128, 512], F32, 6, psum=True)
            psB = mkp("psB", [128, 1024], BF16, 2, psum=True)
            xp = mkp("xp", [128, D], F32, 5)
            junk = mkp("junk", [128, D], BF16, 1)
            ssp = mkp("ssp", [128, 4], F32, 9)
            abp = mkp("abp", [128, D], BF16, 9)
            aTp = mkp("aTp", [128, D], BF16, 3)
            qkp = mkp("qkp", [128, 320], F32, 3)
            rtp = mkp("rtp", [128, 4, 5, 8], F32, 3)
            qkbp = mkp("qkbp", [128, 384], BF16, 3)
            qkTp = mkp("qkTp", [128, 3, 128], BF16, 3)
            vbp = mkp("vbp", [128, 64], BF16, 3)
            gkp = mkp("gkp", [128, 256], BF16, 3)
            gvp = mkp("gvp", [128, 512], BF16, 3)
            f2p = mkp("f2p", [128, 2, 128], BF16, 4)
            f4p = mkp("f4p", [128, 4, 128], BF16, 3)
            f8p = mkp("f8p", [128, 8, 128], BF16, 4)
            zfp = mkp("zfp", [32, 128], F32, 3)
            zbp = mkp("zbp", [32, 128], F32, 3)
            lt1 = mkp("lt1", [128, 256], F32, 3)
            lt2 = mkp("lt2", [128, 256], F32, 3)
            lap = mkp("lap", [128, 256], F32, 3)
            for zp in (zfp, zbp):
                for (zt, zB) in zp.items:
                    P.memset("pool", zt[:], 1.0, w=[zB])
            for (qt_, qB) in qkbp.items:
                P.memset("pool", qt_[:], 0.0, w=[qB])

            aT4p = mkp("aT4p", [128, 8, 512], BF16, 2)
            fst = mkp("fst", [128, 512], BF16, 6)

            def bodyA(g):
                aT4, BaT4 = aT4p.next()
                abs_ = []
                for i in range(4):
                    t = g * 4 + i
                    tok = slice(t * 128, (t + 1) * 128)
                    xt, Bx = xp.next()
                    P.dma("act", xt[:], x[tok, :], w=[Bx])
                    jk, Bj = junk.next()
                    ss, Bss = ssp.next()
                    P.act(jk[:], xt[:], AF.Square, accum_out=ss[:, 0:1], r=[Bx], w=[Bj, Bss])
                    rstd_from_ss(ss, Bss, 0, 1, 2, 1.0 / D)
                    ab, Bab = abp.next()
                    P.act(ab[:], xt[:], AF.Copy, scale=ss[:, 2:3], r=[Bx, Bss], w=[Bab])
                    abs_.append((ab, Bab))
                yield
                for i in range(4):
                    ab, Bab = abs_[i]
                    pT, BpT = psB.next()
                    for c in range(8):
                        P.transpose(pT[:, c * 128:(c + 1) * 128], ab[:, c * 128:(c + 1) * 128], identb[:],
                                    r=[Bab, Bconst], w=[BpT])
                    P.copy("dve", aT4[:, :, i * 128:(i + 1) * 128], pT[:].rearrange("p (c s) -> p c s", c=8),
                           r=[BpT], w=[BaT4])
                    yield
                pending = []
                for i in range(4):
                    t = g * 4 + i
                    tok = slice(t * 128, (t + 1) * 128)
                    tsl = slice(i * 128, (i + 1) * 128)
                    for fn_ in pending:
                        fn_()
                    pending = []

                    def proj_tok(c0, n, tsl=tsl):
                        ps, Bps = psF.next()
                        for c in range(8):
                            P.matmul(ps[:, 0:n], aT4[:, c, tsl], Win[:, c, c0:c0 + n],
                                     start=(c == 0), stop=(c == 7), r=[BaT4, BWin], w=[Bps])
                        return ps, Bps

                    psq, Bpsq = proj_tok(C_AQ, 320)
                    qk, Bqk = qkp.next()
                    P.copy("act", qk[:], psq[:, 0:320], r=[Bpsq], w=[Bqk])
                    qk3 = qk[:].rearrange("p (h d) -> p h d", h=5)
                    t1 = qk3[:, :, 0:8]
                    t2 = qk3[:, :, 8:16]
                    rt, Brt = rtp.next()
                    cb = cosT[:, t:t + 1, :].to_broadcast([128, 5, 8])
                    sb_ = sinT[:, t:t + 1, :].to_broadcast([128, 5, 8])
                    P.tt("pool", rt[:, 0], t1, cb, ALU.mult, r=[Bqk, Brope], w=[Brt])
                    P.tt("pool", rt[:, 1], t2, sb_, ALU.mult, r=[Bqk, Brope], w=[Brt])
                    P.tt("pool", rt[:, 2], t2, cb, ALU.mult, r=[Bqk, Brope], w=[Brt])
                    P.tt("pool", rt[:, 3], t1, sb_, ALU.mult, r=[Bqk, Brope], w=[Brt])
                    P.tt("pool", t1, rt[:, 0], rt[:, 1], ALU.subtract, r=[Brt], w=[Bqk])
                    P.tt("pool", t2, rt[:, 2], rt[:, 3], ALU.add, r=[Brt], w=[Bqk])
                    qkb, Bqkb = qkbp.next()
                    P.copy("act", qkb[:, 0:320], qk[:], r=[Bqk], w=[Bqkb])

                    def fin_qk(qkb=qkb, Bqkb=Bqkb, t=t, tok=tok):
                        pT2, BpT2 = psB.next()
                        for c in range(3):
                            P.transpose(pT2[:, c * 128:(c + 1) * 128], qkb[:, c * 128:(c + 1) * 128], identb[:],
                                        r=[Bqkb, Bconst], w=[BpT2])
                        qkT, BqkT = qkTp.next()
                        P.copy("dve", qkT[:].rearrange("p c s -> p (c s)"), pT2[:, 0:384], r=[BpT2], w=[BqkT])
                        P.dma("sp", fm(qT_d, 0, 2, t), qkT[:, 0:2, :], r=[BqkT], w=[db("qT", t)])
                        P.dma("sp", kT_d[:, tok], qkT[0:64, 2, :], r=[BqkT], w=[db("kT", t)])
                    pending.append(fin_qk)
                    ps, Bps = proj_tok(C_AV, 64)
                    vb, Bvb = vbp.next()
                    P.copy("dve", vb[:], ps[:, 0:64], r=[Bps], w=[Bvb])
                    P.dma("sp", va_d[tok, :], vb[:], r=[Bvb], w=[db("va", t)])
                    ps, Bps = proj_tok(C_GK, 256)
                    gkt, Bgk = gkp.next()
                    P.copy("dve", gkt[:], ps[:, 0:256], r=[Bps], w=[Bgk])
                    P.dma("sp", gk_d[tok, :], gkt[:], r=[Bgk], w=[db("gk", t)])
                    gvt, Bgv = gvp.next()
                    ps, Bps = proj_tok(C_GV, 512)
                    P.copy("dve", gvt[:], ps[:, 0:512], r=[Bps], w=[Bgv])
                    P.dma("sp", gv_d[tok, :], gvt[:], r=[Bgv], w=[db("gv", t)])
                    ps, Bps = psF.next()
                    for zi, c0 in enumerate((C_ZF, C_ZB)):
                        for c in range(8):
                            P.matmul(ps[0:16, zi * 128:(zi + 1) * 128], Win[:, c, c0:c0 + 16], aT4[:, c, tsl],
                                     start=(c == 0), stop=(c == 7), r=[BaT4, BWin], w=[Bps])
                    zf, Bzf = zfp.next()
                    zb, Bzb = zbp.next()
                    P.copy("dve", zf[0:16, :], ps[0:16, 0:128], r=[Bps], w=[Bzf])
                    P.copy("dve", zb[0:16, :], ps[0:16, 128:256], r=[Bps], w=[Bzb])

                    def fin_la(zf=zf, Bzf=Bzf, zb=zb, Bzb=Bzb, t=t, tok=tok):
                        for (zt, Bz, up, dram, nm) in ((zf, Bzf, upf, laf_d, "laf"), (zb, Bzb, upb, lab_d, "lab")):
                            ps2, Bps2 = psF.next()
                            P.matmul(ps2[:, 0:256], zt[0:17, :], up[0:17, :], r=[Bz, Bup], w=[Bps2])
                            a1, B1 = lt1.next()
                            P.act(a1[:], ps2[:, 0:256], AF.Exp, scale=-1.0, r=[Bps2], w=[B1])
                            a2, B2 = lt2.next()
                            P.act(a2[:], a1[:], AF.Ln, bias=1.0, r=[B1], w=[B2])
                            a3, B3 = lap.next()
                            P.ts("pool", a3[:], a2[:], -1.0 / 16.0, None, op0=ALU.mult, r=[B2], w=[B3])
                            P.dma("sp", dram[tok, :], a3[:], r=[B3], w=[db(nm, t)])
                    pending.append(fin_la)
                    yield
                for fn_ in pending:
                    fn_()
                yield
                gsl = slice(g * 512, (g + 1) * 512)
                for (c0, nch, dram, nm, fn) in ((C_GQ, 2, gqT_d, "gqT", None), (C_GK, 2, gkT_d, "gkT", None),
                                                (C_GR, 4, grT_d, "grT", AF.Silu), (C_GA, 8, gaT_d, "gaT", AF.Sigmoid),
                                                (C_GG, 8, ggT_d, "ggT", AF.Sigmoid)):
                    for k in range(nch):
                        ps, Bps = psF.next()
                        for c in range(8):
                            P.matmul(ps[:, 0:512], Win[:, c, c0 + k * 128:c0 + (k + 1) * 128], aT4[:, c, :],
                                     start=(c == 0), stop=(c == 7), r=[BaT4, BWin], w=[Bps])
                        st, Bst = fst.next()
                        if fn is None:
                            P.copy("dve", st[:], ps[:, 0:512], r=[Bps], w=[Bst])
                        else:
                            P.act(st[:], ps[:, 0:512], fn, r=[Bps], w=[Bst])
                        P.dma("sp", dram[k * 128:(k + 1) * 128, gsl], st[:], r=[Bst], w=[db(nm + "_c%d" % k, g)])
                        if k % 2 == 1:
                            yield
            run_pipelined([bodyA(g) for g in range(NT // 4)], 11)
            P.barrier()
            P.emit()
            P.end_phase()

        def gla_setup(ph):
            mkp = lambda name, shape, dt, n, psum=False: TPool(P, ph, nc, name, shape, dt, n, psum)
            G = {}
            G["gqT"] = mkp("g_gqT", [128, 2, 128], BF16, 3)
            G["gkT"] = mkp("g_gkT", [128, 2, 128], BF16, 3)
            G["gk"] = mkp("g_gk", [128, 256], BF16, 3)
            G["gv"] = mkp("g_gv", [128, 512], BF16, 3)
            G["la"] = mkp("g_la", [128, 256], F32, 3)
            G["Eq"] = mkp("g_Eq", [128, 256], F32, 3)
            G["Ek"] = mkp("g_Ek", [128, 256], F32, 3)
            G["Ee"] = mkp("g_Ee", [128, 256], F32, 3)
            G["qt"] = mkp("g_qt", [128, 2, 128], BF16, 3)
            G["kt"] = mkp("g_kt", [128, 2, 128], BF16, 3)
            G["ke"] = mkp("g_ke", [128, 256], BF16, 3)
            G["at"] = mkp("g_at", [128, 2, 128], BF16, 3)
            sbt = lambda n, s, d: ph.enter_context(nc.sbuf_tensor(P.uniq(n), s, d))
            G["S"] = sbt("g_S", [128, 2, 256], F32)
            G["Sb"] = sbt("g_Sb", [128, 2, 256], BF16)
            G["BS"] = P.buf("S")
            G["BSb"] = P.buf("Sb")
            return G

        def gla_reset(G):
            P.memset("pool", G["S"][:], 0.0, w=[G["BS"]])
            P.memset("pool", G["Sb"][:], 0.0, w=[G["BSb"]])

        def gla_tile(G, psF, t, fwd):
            tok = slice(t * 128, (t + 1) * 128)
            la_d, la_nm = (laf_d, "laf") if fwd else (lab_d, "lab")
            m_incl, m_strict = (mLE, mGT) if fwd else (mGE, mLT)
            m_attn = mLE if fwd else mGT
            gq, Bgq = G["gqT"].next()
            P.dma("sp", gq[:], fm(gqT_d, 0, 2, t), r=[db("gqT", t)], w=[Bgq])
            gkT, BgkT = G["gkT"].next()
            P.dma("sp", gkT[:], fm(gkT_d, 0, 2, t), r=[db("gkT", t)], w=[BgkT])
            gk, Bgk = G["gk"].next()
            P.dma("sp", gk[:], gk_d[tok, :], r=[db("gk", t)], w=[Bgk])
            gv, Bgv = G["gv"].next()
            P.dma("sp", gv[:], gv_d[tok, :], r=[db("gv", t)], w=[Bgv])
            la, Bla = G["la"].next()
            P.dma("sp", la[:], la_d[tok, :], r=[db(la_nm, t)], w=[Bla])
            yield
            psb, Bpsb = psF.next()
            for h in range(2):
                P.matmul(psb[:, h * 128:(h + 1) * 128], la[:, h * 128:(h + 1) * 128], m_incl[:],
                         r=[Bla, Bconst], w=[Bpsb])
            psg, Bpsg = psF.next()
            P.matmul(psg[:, 0:256], m_strict[:], la[:], r=[Bla, Bconst], w=[Bpsg])
            Eq, BEq = G["Eq"].next()
            P.act(Eq[:], psb[:, 0:256], AF.Exp, r=[Bpsb], w=[BEq])
            Ek, BEk = G["Ek"].next()
            P.act(Ek[:], psb[:, 0:256], AF.Exp, scale=-1.0, r=[Bpsb], w=[BEk])
            Ee, BEe = G["Ee"].next()
            P.act(Ee[:], psg[:, 0:256], AF.Exp, r=[Bpsg], w=[BEe])
            qt, Bqt = G["qt"].next()
            P.stt(qt[:].rearrange("p h l -> p (h l)"), gq[:].rearrange("p h l -> p (h l)"), 128.0 ** -0.5, Eq[:],
                  ALU.mult, ALU.mult, r=[Bgq, BEq], w=[Bqt])
            kt, Bkt = G["kt"].next()
            P.tt("dve", kt[:].rearrange("p h l -> p (h l)"), gkT[:].rearrange("p h l -> p (h l)"), Ek[:], ALU.mult,
                 r=[BgkT, BEk], w=[Bkt])
            ke, Bke = G["ke"].next()
            P.tt("dve", ke[:], gk[:], Ee[:], ALU.mult, r=[Bgk, BEe], w=[Bke])
            yield
            psa, Bpsa = psF.next()
            for h in range(2):
                P.matmul(psa[:, h * 128:(h + 1) * 128], kt[:, h, :], qt[:, h, :], r=[Bkt, Bqt], w=[Bpsa])
            at, Bat = G["at"].next()
            P.tt("dve", at[:], psa[:, 0:256].rearrange("p (h l) -> p h l", h=2),
                 m_attn[:].unsqueeze(1).to_broadcast([128, 2, 128]), ALU.mult, r=[Bpsa, Bconst], w=[Bat])
            dcol = 127 if fwd else 0
            pk, Bpk = psF.next()
            for h in range(2):
                P.matmul(pk[:, h * 256:(h + 1) * 256], ke[:, h * 128:(h + 1) * 128], gv[:, h * 256:(h + 1) * 256],
                         r=[Bke, Bgv], w=[Bpk])
            for h in range(2):
                P.stt(G["S"][:, h, :], G["S"][:, h, :], Eq[:, h * 128 + dcol:h * 128 + dcol + 1],
                      pk[:, h * 256:(h + 1) * 256], ALU.mult, ALU.add, r=[BEq, Bpk, G["BS"]], w=[G["BS"]])
            po, Bpo = psF.next()
            for h in range(2):
                for ec in range(2):
                    o_ap = po[:, (h * 2 + ec) * 128:(h * 2 + ec + 1) * 128]
                    P.matmul(o_ap, gv[:, h * 256 + ec * 128:h * 256 + (ec + 1) * 128], at[:, h, :],
                             start=True, stop=False, r=[Bgv, Bat], w=[Bpo])
                    P.matmul(o_ap, G["Sb"][:, h, ec * 128:(ec + 1) * 128], qt[:, h, :],
                             start=False, stop=True, r=[G["BSb"], Bqt], w=[Bpo])
            P.copy("act", G["Sb"][:].rearrange("p h e -> p (h e)"), G["S"][:].rearrange("p h e -> p (h e)"),
                   r=[G["BS"]], w=[G["BSb"]])
            return po, Bpo

        with ExitStack() as ph:
            mkp = lambda name, shape, dt, n, psum=False: TPool(P, ph, nc, name, shape, dt, n, psum)
            psF = mkp("psFb", [128, 512], F32, 8, psum=True)
            G = gla_setup(ph)
            obp = mkp("obp", [128, 4, 128], F32, 3)
            gla_reset(G)
            def bodyB(t):
                po, Bpo = yield from gla_tile(G, psF, t, False)
                ob, Bob = obp.next()
                P.copy("act", ob[:].rearrange("p c s -> p (c s)"), po[:, 0:512], r=[Bpo], w=[Bob])
                P.dma("pool", fm(obT_d, 0, 4, t), ob[:], r=[Bob], w=[db("obT", t)])
            run_pipelined([bodyB(t) for t in range(NT - 1, -1, -1)], 1)
            P.barrier()
            P.emit()
            P.end_phase()

        with ExitStack() as ph:
            sbt = lambda n, s, d: ph.enter_context(nc.sbuf_tensor(P.uniq(n), s, d))
            mkp = lambda name, shape, dt, n, psum=False: TPool(P, ph, nc, name, shape, dt, n, psum)
            psF = mkp("psFc", [128, 512], F32, 8, psum=True)
            G = gla_setup(ph)
            Wa = sbt("Wa", [64, 4, D], BF16)
            Wb = sbt("Wb", [128, 4, D], BF16)
            Wo = sbt("Wo", [128, 8, D], BF16)
            ggla = sbt("ggla", [128, 4], F32)
            sinkr = sbt("sinkr", [1, 512], F32)
            BW = P.buf("Wc")
            P.dma("pool", Wa[:], w_ba.rearrange("(h d) c -> d h c", d=64), w=[BW], owner=BW)
            P.dma("pool", Wb[:], w_bg.rearrange("(k p) c -> p k c", p=128), w=[BW], owner=BW)
            P.dma("pool", Wo[:], w_out.rearrange("(k p) c -> p k c", p=128), w=[BW], owner=BW)
            P.dma("sp", ggla[:], ggla_l, w=[BW], owner=BW)
            P.dma("sp", sinkr[:], sink_rep, w=[BW], owner=BW)
            P.act(sinkr[:], sinkr[:], AF.Exp, r=[BW], w=[BW])

            obl = mkp("obl", [128, 4, 128], F32, 3)
            grl = mkp("grl", [128, 4, 128], BF16, 3)
            gal = mkp("gal", [128, 8, 128], BF16, 3)
            ggl = mkp("ggl", [128, 8, 128], BF16, 3)
            osb = mkp("osb", [128, 4, 128], F32, 3)
            sqb = mkp("sqb", [128, 4, 128], BF16, 3)
            rsd = mkp("rsd", [128, 2, 128], F32, 3)
            ogp = mkp("ogp", [128, 4, 128], F32, 3)
            ogb = mkp("ogb", [128, 4, 128], BF16, 3)
            qTl = mkp("qTl", [64, 4, 128], BF16, 3)
            kTl = mkp("kTl", [64, 384], BF16, 3)
            val = mkp("val", [128, 3, 64], BF16, 3)
            ptp = mkp("ptp", [128, 4, 128], BF16, 4)
            rdn = mkp("rdn", [64, 512], F32, 3)
            atp = mkp("atp", [64, 4, 128], BF16, 3)
            t1p = mkp("t1p", [128, 8, 128], F32, 3)
            t2p = mkp("t2p", [128, 8, 128], F32, 3)
            mgp = mkp("mgp", [128, 8, 128], BF16, 3)
            dpp = mkp("dpp", [128, D], F32, 3)
            gla_reset(G)
            Bcc = P.buf("ccd")

            def bodyC(t):
                tok = slice(t * 128, (t + 1) * 128)
                po, Bpo = yield from gla_tile(G, psF, t, True)
                ob, Bob = obl.next()
                P.dma("sp", ob[:], fm(obT_d, 0, 4, t), r=[db("obT", t)], w=[Bob])
                gr, Bgr = grl.next()
                P.dma("sp", gr[:], fm(grT_d, 0, 4, t), r=[db("grT", t)], w=[Bgr])
                o, Bo = osb.next()
                P.tt("dve", o[:].rearrange("p c s -> p (c s)"), po[:, 0:512], ob[:].rearrange("p c s -> p (c s)"),
                     ALU.add, r=[Bpo, Bob], w=[Bo])
                sq, Bsq = sqb.next()
                P.act(sq[:].rearrange("p c s -> p (c s)"), o[:].rearrange("p c s -> p (c s)"), AF.Square, r=[Bo], w=[Bsq])
                pss, Bpss = psF.next()
                for h in range(2):
                    for ec in range(2):
                        P.matmul(pss[:, h * 128:(h + 1) * 128], onesb[:], sq[:, h * 2 + ec, :], start=(ec == 0),
                                 stop=(ec == 1), r=[Bconst, Bsq], w=[Bpss])
                rs, Brs = rsd.next()
                rsf = rs[:].rearrange("p h s -> p (h s)")
                P.act(rsf, pss[:, 0:256], AF.Sqrt, scale=1.0 / 256.0, bias=epsc[:, 0:1], r=[Bpss, Bconst], w=[Brs])
                P.op("dve", (lambda rsf: lambda e: e.reciprocal(out=rsf, in_=rsf))(rsf), r=[Brs], w=[Brs])
                og, Bog = ogp.next()
                for c in range(4):
                    P.stt(og[:, c, :], o[:, c, :], ggla[:, c:c + 1], rs[:, c // 2, :], ALU.mult, ALU.mult,
                          r=[Bo, BW, Brs], w=[Bog])
                ogT, BogT = ogb.next()
                P.tt("dve", ogT[:].rearrange("p c s -> p (c s)"), og[:].rearrange("p c s -> p (c s)"),
                     gr[:].rearrange("p c s -> p (c s)"), ALU.mult, r=[Bog, Bgr], w=[BogT])
                yield
                kbs = [kb for kb in (-1, 0, 1) if 0 <= t + kb < NT]
                qTt, BqT = qTl.next()
                P.dma("sp", qTt[:], qT_d[:, tok].rearrange("(h d) s -> d h s", d=64), r=[db("qT", t)], w=[BqT])
                kTt, BkT = kTl.next()
                vat, Bva = val.next()
                for kb in kbs:
                    tk = t + kb
                    P.dma("sp", kTt[:, (kb + 1) * 128:(kb + 2) * 128], kT_d[:, tk * 128:(tk + 1) * 128],
                          r=[db("kT", tk)], w=[BkT])
                    P.dma("sp", vat[:, kb + 1, :], va_d[tk * 128:(tk + 1) * 128, :], r=[db("va", tk)], w=[Bva])
                at_, Bat_ = atp.next()
                pts = []
                for kb in kbs:
                    psc, Bpsc = psF.next()
                    P.matmul(psc[:, 0:512], kTt[:, (kb + 1) * 128:(kb + 2) * 128],
                             qTt[:].rearrange("d h s -> d (h s)"), r=[BkT, BqT], w=[Bpsc])
                    pt, Bpt = ptp.next()
                    P.act(pt[:].rearrange("p h s -> p (h s)"), psc[:, 0:512], AF.Exp, scale=0.125, r=[Bpsc], w=[Bpt])
                    if kb != 0:
                        mk = mGE if kb == -1 else mLE
                        P.tt("dve", pt[:], pt[:], mk[:].unsqueeze(1).to_broadcast([128, 4, 128]), ALU.mult,
                             r=[Bpt, Bconst], w=[Bpt])
                    pts.append((kb, pt, Bpt))
                gg, Bgg = ggl.next()
                P.dma("sp", gg[:], fm(ggT_d, 0, 8, t), r=[db("ggT", t)], w=[Bgg])
                t2_, Bt2 = t2p.next()
                for half in range(2):
                    pyg, Bpyg = psF.next()
                    for cc in range(4):
                        c = half * 4 + cc
                        for k in range(4):
                            P.matmul(pyg[:, cc * 128:(cc + 1) * 128], Wb[:, k, c * 128:(c + 1) * 128], ogT[:, k, :],
                                     start=(k == 0), stop=(k == 3), r=[BW, BogT], w=[Bpyg])
                    P.tt("dve", t2_[:, half * 4:(half + 1) * 4, :].rearrange("p c s -> p (c s)"), pyg[:, 0:512],
                         gg[:, half * 4:(half + 1) * 4, :].rearrange("p c s -> p (c s)"), ALU.mult, r=[Bpyg, Bgg], w=[Bt2])
                pso, Bpso = psF.next()
                psd, Bpsd = psF.next()
                for i, (kb, pt, Bpt) in enumerate(pts):
                    ptf = pt[:].rearrange("p h s -> p (h s)")
                    P.matmul(pso[0:64, 0:512], vat[:, kb + 1, :], ptf, start=(i == 0),
                             stop=(i == len(pts) - 1), r=[Bva, Bpt], w=[Bpso])
                    P.matmul(psd[0:64, 0:512], onesb[:, 0:64], ptf, start=(i == 0), stop=False,
                             r=[Bconst, Bpt], w=[Bpsd])
                P.matmul(psd[0:64, 0:512], onesf[0:1, 0:64], sinkr[0:1, :], start=False,
                         stop=True, r=[Bconst, BW], w=[Bpsd])
                rd, Brd = rdn.next()
                P.op("dve", (lambda rd, psd: lambda e: e.reciprocal(out=rd[:], in_=psd[0:64, 0:512]))(rd, psd),
                     r=[Bpsd], w=[Brd])
                P.tt("dve", at_[:].rearrange("d h s -> d (h s)"), pso[0:64, 0:512], rd[:],
                     ALU.mult, r=[Bpso, Brd], w=[Bat_])
                yield
                ga, Bga = gal.next()
                P.dma("sp", ga[:], fm(gaT_d, 0, 8, t), r=[db("gaT", t)], w=[Bga])
                t1_, Bt1 = t1p.next()
                for half in range(2):
                    pya, Bpya = psF.next()
                    for cc in range(4):
                        c = half * 4 + cc
                        for h in range(4):
                            P.matmul(pya[:, cc * 128:(cc + 1) * 128], Wa[:, h, c * 128:(c + 1) * 128], at_[:, h, :],
                                     start=(h == 0), stop=(h == 3), r=[BW, Bat_], w=[Bpya])
                    P.tt("dve", t1_[:, half * 4:(half + 1) * 4, :].rearrange("p c s -> p (c s)"), pya[:, 0:512],
                         ga[:, half * 4:(half + 1) * 4, :].rearrange("p c s -> p (c s)"), ALU.mult, r=[Bpya, Bga], w=[Bt1])
                mg, Bmg = mgp.next()
                P.tt("dve", mg[:].rearrange("p c s -> p (c s)"), t1_[:].rearrange("p c s -> p (c s)"),
                     t2_[:].rearrange("p c s -> p (c s)"), ALU.add, r=[Bt1, Bt2], w=[Bmg])
                yield
                dp, Bdp = dpp.next()
                for half in range(2):
                    psh, Bpsh = psF.next()
                    for c in range(8):
                        P.matmul(psh[:, 0:512], mg[:, c, :], Wo[:, c, half * 512:(half + 1) * 512], start=(c == 0),
                                 stop=(c == 7), r=[Bmg, BW], w=[Bpsh])
                    P.copy("act" if half == 0 else "dve", dp[:, half * 512:(half + 1) * 512], psh[:, 0:512],
                           r=[Bpsh], w=[Bdp])
                g4 = t // 4
                P.dma("pool", dpart_d[tok, :], dp[:], r=[Bdp], w=[db("dpart", g4)])
                if t % 4 == 3:
                    P.dma_fn("pool", (lambda g4: lambda e: e.collective_compute(
                        "AllGather", op=ALU.bypass, replica_groups=GROUPS,
                        ins=[dpart_d[g4 * 512:(g4 + 1) * 512, :]], outs=[dall_d[g4]]))(g4),
                        r=[db("dpart", g4)], w=[db("dall", g4)], owner=Bcc, inc=1)
            run_pipelined([bodyC(t) for t in range(NT)], 2)
            P.barrier()
            P.emit()
            P.end_phase()

        with ExitStack() as ph:
            sbt = lambda n, s, d: ph.enter_context(nc.sbuf_tensor(P.uniq(n), s, d))
            mkp = lambda name, shape, dt, n, psum=False: TPool(P, ph, nc, name, shape, dt, n, psum)
            psF = mkp("psFc2", [128, 512], F32, 8, psum=True)
            Wr = sbt("Wr", [128, 8, NEXP], F32)
            WrB = sbt("WrB", [128, 8, NEXP], F32)
            gffn = sbt("gffn", [128, D], F32)
            BW = P.buf("Wc2")
            P.dma("sp", Wr[:], w_router.rearrange("(k p) e -> p k e", p=128), w=[BW], owner=BW)
            P.copy("dve", WrB[:, :, 0:8], Wr[:, :, 8:16], r=[BW], w=[BW])
            P.copy("dve", WrB[:, :, 8:16], Wr[:, :, 0:8], r=[BW], w=[BW])
            P.dma("sp", gffn[:], gffn_rep, w=[BW], owner=BW)
            xl = mkp("xl", [128, D], F32, 4)
            d0l = mkp("d0l", [128, D], F32, 4)
            d1l = mkp("d1l", [128, D], F32, 4)
            h1p = mkp("h1p", [128, D], F32, 4)
            jk2 = mkp("jk2", [128, D], BF16, 1)
            ss2 = mkp("ss2", [128, 4], F32, 4)
            xnp = mkp("xnp", [128, D], F32, 4)
            xnb = mkp("xnb", [128, D], BF16, 4)
            xnT = mkp("xnT", [128, 8, 128], F32, 4)
            lgp = mkp("lgp", [128, 16], F32, 4)
            smp = mkp("smp", [128, 4], F32, 4)
            afp = mkp("afp", [128, 16], F32, 4)
            def bodyC2(t):
                tok = slice(t * 128, (t + 1) * 128)
                g4, i4 = t // 4, t % 4
                xt, Bx = xl.next()
                P.dma("sp", xt[:], x[tok, :], w=[Bx])
                d0, Bd0 = d0l.next()
                P.dma("sp", d0[:], dall_d[g4, i4 * 128:(i4 + 1) * 128, :], r=[db("dall", g4)], w=[Bd0])
                d1, Bd1 = d1l.next()
                P.dma("sp", d1[:], dall_d[g4, 512 + i4 * 128:512 + (i4 + 1) * 128, :], r=[db("dall", g4)], w=[Bd1])
                h1, Bh1 = h1p.next()
                P.tt("dve", d0[:], d0[:], d1[:], ALU.add, r=[Bd0, Bd1], w=[Bd0])
                P.tt("dve", h1[:], d0[:], xt[:], ALU.add, r=[Bd0, Bx], w=[Bh1])
                P.dma("pool", h1_d[tok, :], h1[:], r=[Bh1], w=[db("h1", t)])
                jk, Bj = jk2.next()
                ss, Bss = ss2.next()
                P.act(jk[:], h1[:], AF.Square, accum_out=ss[:, 0:1], r=[Bh1], w=[Bj, Bss])
                rstd_from_ss(ss, Bss, 0, 1, 2, 1.0 / D)
                xn, Bxn = xnp.next()
                P.stt(xn[:], h1[:], ss[:, 2:3], gffn[:], ALU.mult, ALU.mult, r=[Bh1, Bss, BW], w=[Bxn])
                xb_, Bxb = xnb.next()
                P.copy("act", xb_[:], xn[:], r=[Bxn], w=[Bxb])
                P.dma("pool", xn_d[tok, :], xb_[:], r=[Bxb], w=[db("xn", t)])
                yield
                xT_, BxT = xnT.next()
                for half in range(2):
                    pst, Bpst = psF.next()
                    for cc in range(4):
                        c = half * 4 + cc
                        P.transpose(pst[:, cc * 128:(cc + 1) * 128], xn[:, c * 128:(c + 1) * 128], ident[:],
                                    r=[Bxn, Bconst], w=[Bpst])
                    P.copy("act", xT_[:, half * 4:(half + 1) * 4, :].rearrange("p c s -> p (c s)"), pst[:, 0:512],
                           r=[Bpst], w=[BxT])
                yield
                psl, Bpsl = psF.next()
                for c in range(8):
                    P.matmul(psl[:, 0:NEXP], xT_[:, c, :], (Wr if t < NT // 2 else WrB)[:, c, :], start=(c == 0),
                             stop=(c == 7), r=[BxT, BW], w=[Bpsl])
                lg, Blg = lgp.next()
                P.copy("dve", lg[:], psl[:, 0:NEXP], r=[Bpsl], w=[Blg])
                sm, Bsm = smp.next()
                P.op("dve", (lambda sm, lg: lambda e: e.reduce_max(out=sm[:, 0:1], in_=lg[:], axis=AX.X))(sm, lg),
                     r=[Blg], w=[Bsm])
                P.ts("dve", sm[:, 1:2], sm[:, 0:1], -1.0, None, op0=ALU.mult, r=[Bsm], w=[Bsm])
                af, Baf = afp.next()
                P.act(af[:], lg[:], AF.Exp, bias=sm[:, 1:2], accum_out=sm[:, 2:3], r=[Blg, Bsm], w=[Baf, Bsm])
                P.op("dve", (lambda sm: lambda e: e.reciprocal(out=sm[:, 3:4], in_=sm[:, 2:3]))(sm), r=[Bsm], w=[Bsm])
                P.ts("dve", af[:], af[:], sm[:, 3:4], None, op0=ALU.mult, r=[Baf, Bsm], w=[Baf])
                psf_, Bpsf = psF.next()
                P.transpose(psf_[0:NEXP, 0:128], af[:], ident[:], r=[Baf, Bconst], w=[Bpsf])
                if t < NT // 2:
                    P.copy("dve", affA[:, t * 128:(t + 1) * 128], psf_[0:NEXP, 0:128], r=[Bpsf], w=[BaffT])
                else:
                    P.copy("dve", affB[:, (t - NT // 2) * 128:(t - NT // 2 + 1) * 128], psf_[0:NEXP, 0:128], r=[Bpsf],
                           w=[BaffT])
            run_pipelined([bodyC2(t) for t in range(NT)], 1)
            P.barrier()
            P.emit()
            P.end_phase()

        phFw = ExitStack()
        Wpg = phFw.enter_context(nc.sbuf_tensor("Wpg", [128, 8, D], BF16))
        Wpl = phFw.enter_context(nc.sbuf_tensor("Wpl", [128, 2, D], BF16))
        gple = phFw.enter_context(nc.sbuf_tensor("gple", [128, 8], F32))
        gfin = phFw.enter_context(nc.sbuf_tensor("gfin", [128, D], F32))
        BWf = P.buf("Wf")
        phDE = ExitStack()
        wgp = TPool(P, phDE, nc, "wgp", [128, 8, D], BF16, 2)
        wup = TPool(P, phDE, nc, "wup", [128, 8, D], BF16, 2)
        wdp = TPool(P, phDE, nc, "wdp", [128, 8, D], BF16, 2)
        wst = TPool(P, phDE, nc, "wst", [128, D], F32, 3)
        cast_rr = [0]

        def load_w(e, engs):
            res = []
            for pool_, dram in ((wgp, w_eg), (wup, w_eu), (wdp, w_ed)):
                wt, Bw = pool_.next()
                for k in range(8):
                    st, Bst = wst.next()
                    P.dma("sp", st[:], dram[e, k * 128:(k + 1) * 128, :], w=[Bst])
                    ce = engs[cast_rr[0] % len(engs)]
                    cast_rr[0] += 1
                    P.copy(ce, wt[:, k, :], st[:], r=[Bst], w=[Bw])
                res.append((wt, Bw))
            return res

        with ExitStack() as ph:
            sbt = lambda n, s, d: ph.enter_context(nc.sbuf_tensor(P.uniq(n), s, d))
            mkp = lambda name, shape, dt, n, psum=False: TPool(P, ph, nc, name, shape, dt, n, psum)
            psF = mkp("psFd", [128, 512], F32, 2, psum=True)
            work = sbt("work", [NEXP, HS], F32)
            vals = sbt("vals", [NEXP, CAP], F32)
            idxs = sbt("idxs", [NEXP, CAP], U32)
            idxf = sbt("idxf", [NEXP, CAP], F32)
            tv = sbt("tv", [128, NJ, NEXP], F32)
            ti = sbt("ti", [128, NJ, NEXP], F32)
            itf = sbt("itf", [128, NJ, NEL], F32)
            rbp = mkp("rbp", [128, 16], F32, 2)
            wk = mkp("wkd", [128, 4, NEL], F32, 2)
            zt = sbt("zt", [128, D], F32)
            Bwork, Bvals, Bidx, Bidxf, Bitf, Bzt, Btv, Bti = [P.buf() for _ in range(8)]
            P.memset("pool", zt[:], 0.0, w=[Bzt])
            for t in range(NT):
                P.dma("pool", yacc_d[t * 128:(t + 1) * 128, :], zt[:], r=[Bzt], w=[db("yacc")], owner=Bzt)
            w_ready = [load_w(0, ("act",)), load_w(1, ("act",))]
            P.ts("dve", work[:], affA[:], rowmask[:, 0:1], op0=ALU.mult, r=[BaffT, Bconst], w=[Bwork])
            P.stt(work[:], affB[:], rowmask[:, 1:2], work[:], ALU.mult, ALU.add, r=[BaffT, Bconst, Bwork], w=[Bwork])
            for it in range(CAP // 8):
                sl = slice(it * 8, it * 8 + 8)
                P.op("dve", (lambda sl: lambda e: e.max(out=vals[:, sl], in_=work[:]))(sl), r=[Bwork], w=[Bvals])
                P.op("dve", (lambda sl: lambda e: e.max_index(out=idxs[:, sl], in_max=vals[:, sl], in_values=work[:]))(sl),
                     r=[Bwork, Bvals], w=[Bidx])
                P.op("dve", (lambda sl: lambda e: e.match_replace(out=work[:], in_to_replace=vals[:, sl],
                                                                 in_values=work[:], imm_value=-1.0))(sl),
                     r=[Bwork, Bvals], w=[Bwork])
            P.copy("dve", idxf[:], idxs[:], r=[Bidx], w=[Bidxf])
            for j in range(NJ):
                ps, Bps = psF.next()
                P.transpose(ps[:, 0:NEXP], vals[:, j * 128:(j + 1) * 128], ident[0:NEXP, 0:NEXP], r=[Bvals, Bconst], w=[Bps])
                P.copy("act", tv[:, j, :], ps[:, 0:NEXP], r=[Bps], w=[Btv])
                ps, Bps = psF.next()
                P.transpose(ps[:, 0:NEXP], idxf[:, j * 128:(j + 1) * 128], ident[0:NEXP, 0:NEXP], r=[Bidxf, Bconst], w=[Bps])
                P.copy("act", ti[:, j, :], ps[:, 0:NEXP], r=[Bps], w=[Bti])
            for j in range(NJ):
                jr = NJ - 1 - j
                ps, Bps = psF.next()
                P.matmul(ps[:, 0:8], Jm[:], tv[:, jr, 8:16], r=[Bconst, Btv], w=[Bps])
                P.matmul(ps[:, 8:16], Jm[:], ti[:, jr, 8:16], r=[Bconst, Bti], w=[Bps])
                rb, Brb = rbp.next()
                P.copy("act", rb[:], ps[:, 0:16], r=[Bps], w=[Brb])
                w4, Bw4 = wk.next()
                P.tt("dve", w4[:, 0, :], tv[:, j, 0:8], rb[:, 0:8], ALU.is_gt, r=[Btv, Brb], w=[Bw4])
                P.tt("dve", valsT[:, j, :], tv[:, j, 0:8], rb[:, 0:8], ALU.max, r=[Btv, Brb], w=[Bsel])
                P.ts("dve", w4[:, 1, :], rb[:, 8:16], float(HS), None, op0=ALU.add, r=[Brb], w=[Bw4])
                P.tt("dve", w4[:, 2, :], ti[:, j, 0:8], w4[:, 1, :], ALU.subtract, r=[Bti, Bw4], w=[Bw4])
                P.tt("dve", w4[:, 3, :], w4[:, 2, :], w4[:, 0, :], ALU.mult, r=[Bw4], w=[Bw4])
                P.tt("dve", itf[:, j, :], w4[:, 3, :], w4[:, 1, :], ALU.add, r=[Bw4], w=[Bitf])
            P.copy("dve", idxT[:], itf[:], r=[Bitf], w=[Bsel])
            if debug:
                P.dma("sp", idx_dbg, idxT[:].rearrange("p j e -> p (j e)"), r=[Bsel], w=[db("idxdbg")], owner=Bidx)
                P.dma("sp", val_dbg, valsT[:].rearrange("p j e -> p (j e)"), r=[Bsel], w=[db("valdbg")], owner=Bvals)
            P.barrier()
            P.emit()
            P.end_phase()

        with ExitStack() as ph:
            sbt = lambda n, s, d: ph.enter_context(nc.sbuf_tensor(P.uniq(n), s, d))
            mkp = lambda name, shape, dt, n, psum=False: TPool(P, ph, nc, name, shape, dt, n, psum)
            psF = mkp("psFe", [128, 512], F32, 6, psum=True)
            psB = mkp("psBe", [128, 1024], BF16, 2, psum=True)
            xgp = mkp("xgp", [128, NJ, D], BF16, 2)
            xgT = mkp("xgT", [128, 8, CAP], BF16, 1)
            sgp = mkp("sgp", [128, CAP], F32, 2)
            hdp = mkp("hdp", [128, 8, CAP], BF16, 2)
            ysb = mkp("ysb", [128, D], F32, 2)
            def gather(e):
                xg, Bxg = xgp.next()
                for j in range(NJ):
                    P.dma_fn("pool", (lambda xg, j, e: lambda en: en.indirect_dma_start(
                        out=xg[:, j, :], out_offset=None, in_=xn_d,
                        in_offset=bass.IndirectOffsetOnAxis(ap=idxT[:, j, e:e + 1], axis=0)))(xg, j, e),
                        r=[Bsel] + [db("xn", tt_) for tt_ in range(NT)], w=[Bxg])
                return xg, Bxg

            P.dma("pool", Wpg[:], w_pg.rearrange("(k p) c -> p k c", p=128), w=[BWf], owner=BWf)
            P.dma("pool", Wpl[:], w_ple.rearrange("(k p) c -> p k c", p=128), w=[BWf], owner=BWf)
            P.dma("sp", gple[:], gple_l, w=[BWf], owner=BWf)
            P.dma("sp", gfin[:], gfin_rep, w=[BWf], owner=BWf)
            for c in range(8):
                if c % 2 == 0:
                    P.ts("dve", Wpg[:, c, :], Wpg[:, c, :], gple[:, c:c + 1], op0=ALU.mult, r=[BWf], w=[BWf])
                else:
                    P.act(Wpg[:, c, :], Wpg[:, c, :], AF.Copy, scale=gple[:, c:c + 1], r=[BWf], w=[BWf])
            nxt_g = gather(0)
            for e in range(NEL):
                (wg, Bwg), (wu, Bwu), (wd, Bwd) = w_ready.pop(0)
                xg, Bxg = nxt_g
                if e + 1 < NEL:
                    nxt_g = gather(e + 1)
                xT_, BxT = xgT.next()
                for c in range(8):
                    pT, BpT = psB.next()
                    for j in range(NJ):
                        P.transpose(pT[:, j * 128:(j + 1) * 128], xg[:, j, c * 128:(c + 1) * 128], identb[:],
                                    r=[Bxg, Bconst], w=[BpT])
                    P.copy("dve" if c % 2 == 0 else "act", xT_[:, c, :], pT[:, 0:CAP], r=[BpT], w=[BxT])
                hd, Bhd = hdp.next()
                for fc in range(8):
                    psg, Bpsg = psF.next()
                    psu, Bpsu = psF.next()
                    for c in range(8):
                        P.matmul(psg[:, 0:CAP], wg[:, c, fc * 128:(fc + 1) * 128], xT_[:, c, :], start=(c == 0),
                                 stop=(c == 7), r=[Bwg, BxT], w=[Bpsg])
                    for c in range(8):
                        P.matmul(psu[:, 0:CAP], wu[:, c, fc * 128:(fc + 1) * 128], xT_[:, c, :], start=(c == 0),
                                 stop=(c == 7), r=[Bwu, BxT], w=[Bpsu])
                    sg, Bsg = sgp.next()
                    P.act(sg[:], psg[:, 0:CAP], AF.Silu, r=[Bpsg], w=[Bsg])
                    P.tt("dve", hd[:, fc, :], sg[:], psu[:, 0:CAP], ALU.mult, r=[Bsg, Bpsu], w=[Bhd])
                for j in range(NJ):
                    ys, Bys = ysb.next()
                    for half in range(2):
                        psy, Bpsy = psF.next()
                        for fc in range(8):
                            P.matmul(psy[:, 0:512], hd[:, fc, j * 128:(j + 1) * 128], wd[:, fc, half * 512:(half + 1) * 512],
                                     start=(fc == 0), stop=(fc == 7), r=[Bhd, Bwd], w=[Bpsy])
                        if half == 0:
                            P.ts("dve", ys[:, 0:512], psy[:, 0:512], valsT[:, j, e:e + 1], None, op0=ALU.mult,
                                 r=[Bpsy, Bsel], w=[Bys])
                        else:
                            P.act(ys[:, 512:1024], psy[:, 0:512], AF.Copy, scale=valsT[:, j, e:e + 1],
                                  r=[Bpsy, Bsel], w=[Bys])
                    P.dma_fn("pool", (lambda ys, j, e: lambda en: en.indirect_dma_start(
                        out=yacc_d, out_offset=bass.IndirectOffsetOnAxis(ap=idxT[:, j, e:e + 1], axis=0),
                        in_=ys[:], in_offset=None, compute_op=ALU.add))(ys, j, e),
                        r=[Bys, Bsel, db("yacc")], w=[db("yacc")])
                if e + 2 < NEL:
                    w_ready.append(load_w(e + 2, ("dve", "act")))
            P.barrier()
            P.emit()
            P.end_phase()

        phDE.close()
        with ExitStack() as ph:
            sbt = lambda n, s, d: ph.enter_context(nc.sbuf_tensor(P.uniq(n), s, d))
            mkp = lambda name, shape, dt, n, psum=False: TPool(P, ph, nc, name, shape, dt, n, psum)
            psF = mkp("psFf", [128, 512], F32, 6, psum=True)
            psB = mkp("psBf", [128, 1024], BF16, 2, psum=True)
            BW = BWf
            Bcc2 = P.buf("ccy")
            ridx = sbt("ridx", [128, 3, NT // 2], I32)
            Bridx = P.buf("ridx")
            P.dma("sp", ridx[:].rearrange("p a t -> p (a t)"), rowidx_in, w=[Bridx], owner=Bridx)
            for g4 in [g for k in range(NG // 2) for g in (k, NG // 2 + k)]:
                P.dma_fn("pool", (lambda g4: lambda e: e.collective_compute(
                    "AllGather", op=ALU.bypass, replica_groups=GROUPS,
                    ins=[yacc_d[g4 * 512:(g4 + 1) * 512, :]], outs=[yall_d[g4]]))(g4),
                    r=[db("yacc")], w=[db("yall", g4)], owner=P.buf(f"ccy{g4}"), inc=1)
            yall_flat = yall_d.rearrange("g r d -> (g r) d")

            def gather_rows(dst, Bdst, src2d, col, deps):
                P.dma_fn("pool", (lambda dst, col: lambda en: en.indirect_dma_start(
                    out=dst[:], out_offset=None, in_=src2d,
                    in_offset=bass.IndirectOffsetOnAxis(ap=ridx[:, col[0], col[1]:col[1] + 1], axis=0)))(dst, col),
                    r=[Bridx] + deps, w=[Bdst])

            hp_ = mkp("hp_", [128, D], F32, 4)
            y0l = mkp("y0l", [128, D], F32, 4)
            y1l = mkp("y1l", [128, D], F32, 4)
            pp_ = mkp("pp_", [128, 256], F32, 4)
            pb_ = mkp("pb_", [128, 256], BF16, 4)
            pTp = mkp("pTp", [128, 2, 128], BF16, 4)
            jk3 = mkp("jk3", [128, D], BF16, 1)
            ss3 = mkp("ss3", [128, 8], F32, 4)
            ab3 = mkp("ab3", [128, D], BF16, 4)
            aT3 = mkp("aT3", [128, D], BF16, 4)
            gtp = mkp("gtp", [128, D], F32, 4)
            h3p = mkp("h3p", [128, D], F32, 4)
            outp = mkp("outp", [128, D], F32, 4)
            def bodyF(t):
                tok = slice(t * 128, (t + 1) * 128)
                g4, i4 = t // 4, t % 4
                ydeps = [db("yall", t // 4), db("yall", NG // 2 + t // 4)]
                h2, Bh2 = hp_.next()
                gather_rows(h2, Bh2, h1_d, (0, t), [])
                y0, By0 = y0l.next()
                gather_rows(y0, By0, yall_flat, (1, t), ydeps)
                y1, By1 = y1l.next()
                gather_rows(y1, By1, yall_flat, (2, t), ydeps)
                pt_, Bp = pp_.next()
                P.dma("sp", pt_[:], pin[tok, :], w=[Bp])
                P.tt("dve", y0[:], y0[:], y1[:], ALU.add, r=[By0, By1], w=[By0])
                P.tt("dve", h2[:], h2[:], y0[:], ALU.add, r=[Bh2, By0], w=[Bh2])
                jk, Bj = jk3.next()
                ss, Bss = ss3.next()
                P.act(jk[:], h2[:], AF.Square, accum_out=ss[:, 0:1], r=[Bh2], w=[Bj, Bss])
                rstd_from_ss(ss, Bss, 0, 1, 2, 1.0 / D)
                ab, Bab = ab3.next()
                P.act(ab[:], h2[:], AF.Copy, scale=ss[:, 2:3], r=[Bh2, Bss], w=[Bab])
                yield
                pT, BpT = psB.next()
                for c in range(8):
                    P.transpose(pT[:, c * 128:(c + 1) * 128], ab[:, c * 128:(c + 1) * 128], identb[:], r=[Bab, Bconst], w=[BpT])
                aT, BaT = aT3.next()
                P.copy("dve", aT[:], pT[:], r=[BpT], w=[BaT])
                pb, Bpb = pb_.next()
                P.copy("act", pb[:], pt_[:], r=[Bp], w=[Bpb])
                pT2, BpT2 = psB.next()
                for c in range(2):
                    P.transpose(pT2[:, c * 128:(c + 1) * 128], pb[:, c * 128:(c + 1) * 128], identb[:], r=[Bpb, Bconst], w=[BpT2])
                pTt, BpTt = pTp.next()
                P.copy("dve", pTt[:].rearrange("p c s -> p (c s)"), pT2[:, 0:256], r=[BpT2], w=[BpTt])
                yield
                gt, Bgt = gtp.next()
                h3, Bh3 = h3p.next()
                for half in range(2):
                    hs = slice(half * 512, (half + 1) * 512)
                    psg, Bpsg = psF.next()
                    for c in range(8):
                        P.matmul(psg[:, 0:512], aT[:, c * 128:(c + 1) * 128], Wpg[:, c, hs], start=(c == 0), stop=(c == 7),
                                 r=[BaT, BW], w=[Bpsg])
                    P.act(gt[:, hs], psg[:, 0:512], AF.Sigmoid, r=[Bpsg], w=[Bgt])
                    psp, Bpsp = psF.next()
                    for c in range(2):
                        P.matmul(psp[:, 0:512], pTt[:, c, :], Wpl[:, c, hs], start=(c == 0), stop=(c == 1), r=[BpTt, BW], w=[Bpsp])
                    P.tt("dve", gt[:, hs], gt[:, hs], psp[:, 0:512], ALU.mult, r=[Bgt, Bpsp], w=[Bgt])
                    P.tt("dve", h3[:, hs], gt[:, hs], h2[:, hs], ALU.add, r=[Bgt, Bh2], w=[Bh3])
                yield
                jk, Bj = jk3.next()
                P.act(jk[:], h3[:], AF.Square, accum_out=ss[:, 4:5], r=[Bh3], w=[Bj, Bss])
                rstd_from_ss(ss, Bss, 4, 5, 6, 1.0 / D)
                ot, Bot = outp.next()
                P.stt(ot[:], h3[:], ss[:, 6:7], gfin[:], ALU.mult, ALU.mult, r=[Bh3, Bss, BW], w=[Bot])
                P.dma("pool", y[tok, :], ot[:], r=[Bot], w=[db("y", t)])
            run_pipelined([bodyF(t) for t in range(NT // 2)], 1)
            P.barrier()
            P.emit()
            P.end_phase()
        phFw.close()
        print("instructions recorded:", P.ninst, "sems:", len(P.sems))
    return nc


def _rowidx(S, r):
    NT = S // 128
    out = np.zeros((128, 3, NT // 2), np.int32)
    pp = np.arange(128)
    for tl in range(NT // 2):
        t = r * (NT // 2) + tl
        g4, i4 = t // 4, t % 4
        out[:, 0, tl] = t * 128 + pp
        out[:, 1, tl] = g4 * 1024 + i4 * 128 + pp
        out[:, 2, tl] = g4 * 1024 + 512 + i4 * 128 + pp
    return np.ascontiguousarray(out.reshape(128, -1))


def make_inputs(S, b, r, x, p, positions, norm_mix, w_in, gla_gate_up_fwd, gla_gate_bias_fwd, gla_gate_up_bwd,
                gla_gate_bias_bwd, attn_sink, gla_norm, w_branch_attn, w_branch_gla, w_out, norm_ffn, w_router,
                w_exp_gate, w_exp_up, w_exp_down, norm_ple, w_ple_gate, w_ple, norm_final):
    f = lambda a: np.ascontiguousarray(np.asarray(a, dtype=np.float32))
    col8 = lambda v: f(np.asarray(v).reshape(-1, 128).T)
    NT = S // 128
    W = np.asarray(w_in[0])
    o_aq, o_ak, o_av, o_gq, o_gk, o_gv, o_gr, o_zf, o_zb, o_ga, o_gg = (
        0, 512, 640, 768, 1280, 1792, 2816, 3840, 3856, 3872, 4896)
    cols = np.concatenate([
        np.arange(o_aq + 256 * r, o_aq + 256 * (r + 1)),
        np.arange(o_ak + 64 * r, o_ak + 64 * (r + 1)),
        np.arange(o_av + 64 * r, o_av + 64 * (r + 1)),
        np.arange(o_gq + 256 * r, o_gq + 256 * (r + 1)),
        np.arange(o_gk + 256 * r, o_gk + 256 * (r + 1)),
        np.arange(o_gv + 512 * r, o_gv + 512 * (r + 1)),
        np.arange(o_gr + 512 * r, o_gr + 512 * (r + 1)),
        np.arange(o_zf, o_zf + 16), np.arange(o_zb, o_zb + 16),
        np.arange(o_ga, o_ga + 1024), np.arange(o_gg, o_gg + 1024)])
    assert cols.size == NCOL
    dk = slice(256 * r, 256 * (r + 1))
    upf = np.concatenate([np.asarray(gla_gate_up_fwd[0])[:, dk], np.asarray(gla_gate_bias_fwd[0])[None, dk]], 0)
    upb = np.concatenate([np.asarray(gla_gate_up_bwd[0])[:, dk], np.asarray(gla_gate_bias_bwd[0])[None, dk]], 0)
    eperm = np.concatenate([np.arange(8 * r, 8 * (r + 1)), np.arange(8 * (1 - r), 8 * (2 - r))])
    es = slice(8 * r, 8 * (r + 1))
    return {
        "x": f(x[b]),
        "p": f(np.asarray(p[0, b])[r * (S // 2):(r + 1) * (S // 2)]),
        "rowidx": _rowidx(S, r),
        "pos_l": np.ascontiguousarray(np.asarray(positions[b], dtype=np.int32).reshape(NT, 128).T),
        "invf_rep": f(np.broadcast_to((500000.0 ** (-np.arange(0, 16, 2, dtype=np.float32) / 16.0))[None, :], (128, 8))),
        "gmix_l": col8(norm_mix[0]),
        "gple_l": col8(norm_ple[0]),
        "ggla_l": col8(np.asarray(gla_norm[0])[512 * r:512 * (r + 1)]),
        "gffn_rep": f(np.broadcast_to(np.asarray(norm_ffn[0])[None, :], (128, D))),
        "gfin_rep": f(np.broadcast_to(np.asarray(norm_final)[None, :], (128, D))),
        "sink_rep": f(np.repeat(np.asarray(attn_sink[0])[4 * r:4 * (r + 1)], 128)[None, :]),
        "upext_f": f(upf),
        "upext_b": f(upb),
        "w_in": f(W[:, cols]),
        "w_ba": f(np.asarray(w_branch_attn[0])[256 * r:256 * (r + 1)]),
        "w_bg": f(np.asarray(w_branch_gla[0])[512 * r:512 * (r + 1)]),
        "w_out": f(w_out[0]),
        "w_router": f(np.asarray(w_router[0])[:, eperm]),
        "rowmask": f(np.stack([np.arange(16) < 8, np.arange(16) >= 8], 1)),
        "w_eg": f(np.asarray(w_exp_gate[0])[es]),
        "w_eu": f(np.asarray(w_exp_up[0])[es]),
        "w_ed": f(np.asarray(w_exp_down[0])[es]),
        "w_pg": f(w_ple_gate[0]),
        "w_ple": f(w_ple[0]),
    }


def kernel(**inputs):
    x = np.asarray(inputs["x"])
    B, S, _ = x.shape
    nc = build(S)
    in_maps = []
    for c in range(8):
        in_maps.append(make_inputs(S, (c // 2) % B, c % 2, **inputs))
    res = run_bass_kernel_spmd(nc, in_maps, core_ids=list(range(8)))
    out = np.stack([np.concatenate([np.asarray(res.results[2 * b]["y"], dtype=np.float32),
                                    np.asarray(res.results[2 * b + 1]["y"], dtype=np.float32)], 0) for b in range(B)], 0)
    return out
```

```python
import math
import numpy as np
from contextlib import ExitStack
import concourse.bass as bass
import concourse.mybir as mybir
from concourse.bass_utils import run_bass_kernel_spmd

F32 = mybir.dt.float32
BF16 = mybir.dt.bfloat16
I32 = mybir.dt.int32
U32 = mybir.dt.uint32
AF = mybir.ActivationFunctionType
ALU = mybir.AluOpType
AX = mybir.AxisListType

D = 1024
IN_DIM = 5920
EPS = 1e-6
NEXP = 16
C_AQ, C_AK, C_AV, C_GQ, C_GK, C_GV, C_GR, C_ZF, C_ZB, C_GA, C_GG = (
    0, 512, 640, 768, 1280, 1792, 2816, 3840, 3856, 3872, 4896)


class Buf:
    __slots__ = ("name", "w", "r", "dkey", "dkey_sw", "dram")

    def __init__(self, name, dram=False):
        self.name = name
        self.w = None
        self.r = {}
        self.dkey = None
        self.dkey_sw = None
        self.dram = dram


class Prog:
    ENGS = ("pe", "act", "dve", "pool", "sp")

    def __init__(self, nc, es):
        self.nc = nc
        self.es = es
        self.streams = {e: [] for e in self.ENGS}
        self.cnt = {e: 0 for e in self.ENGS}
        self.known = {e: {} for e in self.ENGS}
        self.dtot = {}
        self.sems = {}
        self.nbuf = 0
        self.ninst = 0
        self.free_keys = []
        self.free_keys_sw = []
        self.phase_owners = []
        for e in self.ENGS:
            self.sem(e)

    def sem(self, key):
        if key not in self.sems:
            self.sems[key] = self.es.enter_context(self.nc.semaphore(f"s_{key}"))
        return self.sems[key]

    def uniq(self, n):
        self.nbuf += 1
        return f"{n}_u{self.nbuf}"

    def buf(self, name=None):
        self.nbuf += 1
        return Buf(name or f"b{self.nbuf}")

    def _deps(self, eng, reads, writes):
        need = {}

        def add(k, v):
            if k == eng and eng == "pe":
                return
            if need.get(k, 0) < v:
                need[k] = v

        for b in reads:
            if b.w is not None:
                add(*b.w)
        for b in writes:
            if b.w is not None:
                add(*b.w)
            for k, v in b.r.items():
                add(k, v)
        waits = []
        kn = self.known[eng]
        for k, v in need.items():
            if k in self.dtot:
                v = self.dtot[k]
            if kn.get(k, 0) >= v:
                continue
            kn[k] = v
            waits.append((k, v))
        return waits

    def _mark(self, ev, reads, writes):
        k, v = ev
        for b in reads:
            if b.r.get(k, 0) < v:
                b.r[k] = v
        for b in writes:
            b.w = ev
            b.r = {}

    def op(self, eng, fn, r=(), w=()):
        waits = self._deps(eng, r, w)
        self.cnt[eng] += 1
        ev = (eng, self.cnt[eng])
        self.streams[eng].append((waits, fn, (eng, 1)))
        self._mark(ev, r, w)
        self.ninst += 1

    def dma_fn(self, q, fn, r=(), w=(), owner=None, inc=16):
        if owner is None:
            cands = [b for b in w if not b.dram] or [b for b in r if not b.dram]
            owner = cands[0]
        attr = "dkey_sw" if q == "pool" else "dkey"
        if getattr(owner, attr) is None:
            fk = self.free_keys_sw if q == "pool" else self.free_keys
            if fk:
                setattr(owner, attr, fk.pop())
            else:
                k_ = f"d{len(self.dtot)}"
                self.dtot[k_] = 0
                self.sem(k_)
                setattr(owner, attr, k_)
            if inc == 16:
                self.phase_owners.append((owner, q == "pool"))
        okey = getattr(owner, attr)
        waits = self._deps(q, r, w)
        self.dtot[okey] += inc
        ev = (okey, self.dtot[okey])
        self.streams[q].append((waits, fn, (okey, inc)))
        self._mark(ev, r, w)
        self.ninst += 1

    def barrier(self):
        for e in self.ENGS:
            waits = []
            kn = self.known[e]
            for k in self.ENGS:
                v = self.cnt[k]
                if k == e or v == 0 or kn.get(k, 0) >= v:
                    continue
                kn[k] = v
                waits.append((k, v))
            for k, v in self.dtot.items():
                if v == 0 or kn.get(k, 0) >= v:
                    continue
                kn[k] = v
                waits.append((k, v))
            if waits:
                self.streams[e].append((waits, None, None))

    def end_phase(self):
        for o, sw in self.phase_owners:
            if sw:
                self.free_keys_sw.append(o.dkey_sw)
                o.dkey_sw = None
            else:
                self.free_keys.append(o.dkey)
                o.dkey = None
        self.phase_owners = []

    def emit(self):
        nc = self.nc
        streams = self.streams
        sems = self.sems

        def run(name):
            def f(e):
                for waits, fn, inc in streams[name]:
                    for k, v in waits:
                        e.wait_ge(sems[k], v)
                    if fn is None:
                        continue
                    ins = fn(e)
                    if inc is not None:
                        ins.then_inc(sems[inc[0]], inc[1])
            return f

        with nc.Block() as block:
            block.tensor(run("pe"))
            block.scalar(run("act"))
            block.vector(run("dve"))
            block.gpsimd(run("pool"))
            block.sync(run("sp"))
        self.streams = {e: [] for e in self.ENGS}

    def dma(self, q, out, in_, r=(), w=(), owner=None):
        self.dma_fn(q, lambda e: e.dma_start(out=out, in_=in_), r, w, owner)

    def matmul(self, out, lhsT, rhs, start=True, stop=True, r=(), w=()):
        self.op("pe", lambda e: e.matmul(out, lhsT=lhsT, rhs=rhs, start=start, stop=stop), r, w)

    def transpose(self, out, in_, ident, r=(), w=()):
        self.op("pe", lambda e: e.transpose(out, in_, ident), r, w)

    def act(self, out, in_, func, r=(), w=(), bias=None, scale=None, accum_out=None):
        kw = {}
        if bias is not None:
            kw["bias"] = bias
        if scale is not None:
            kw["scale"] = scale
        if accum_out is not None:
            kw["accum_out"] = accum_out
        self.op("act", lambda e: e.activation(out=out, in_=in_, func=func, **kw), r, w)

    def tt(self, eng, out, in0, in1, op, r=(), w=()):
        self.op(eng, lambda e: e.tensor_tensor(out=out, in0=in0, in1=in1, op=op), r, w)

    def ts(self, eng, out, in0, s1, s2=None, op0=ALU.mult, op1=None, r=(), w=()):
        if op1 is None:
            self.op(eng, lambda e: e.tensor_scalar(out=out, in0=in0, scalar1=s1, scalar2=None, op0=op0), r, w)
        else:
            self.op(eng, lambda e: e.tensor_scalar(out=out, in0=in0, scalar1=s1, scalar2=s2, op0=op0, op1=op1), r, w)

    def stt(self, out, in0, scalar, in1, op0, op1, r=(), w=()):
        self.op("dve", lambda e: e.scalar_tensor_tensor(out=out, in0=in0, scalar=scalar, in1=in1, op0=op0, op1=op1), r, w)

    def copy(self, eng, out, in_, r=(), w=()):
        if eng == "act":
            self.op("act", lambda e: e.activation(out=out, in_=in_, func=AF.Copy), r, w)
        else:
            self.op(eng, lambda e: e.tensor_copy(out=out, in_=in_), r, w)

    def memset(self, eng, ap, val, w=()):
        self.op(eng, lambda e: e.memset(ap, val), (), w)


class TPool:
    def __init__(self, P, ph, nc, name, shape, dtype, n, psum=False):
        self.P = P
        self.items = []
        P.nbuf += 1
        name = f"{name}_u{P.nbuf}_"
        for i in range(n):
            if psum:
                t = ph.enter_context(nc.psum_tensor(f"{name}{i}", shape, dtype))
            else:
                t = ph.enter_context(nc.sbuf_tensor(f"{name}{i}", shape, dtype))
            self.items.append((t, P.buf(f"{name}{i}")))
        self.i = 0
        self.owner = P.buf(name + "_own")

    def next(self):
        t = self.items[self.i % len(self.items)]
        self.i += 1
        return t


NCOL = 4000
C_AQ, C_AK, C_AV, C_GQ, C_GK, C_GV, C_GR, C_ZF, C_ZB, C_GA, C_GG = (
    0, 256, 320, 384, 640, 896, 1408, 1920, 1936, 1952, 2976)
NEL = 8


def run_pipelined(gens, gap):
    active = []
    it = iter(gens)
    rnd = 0
    exhausted = False
    while True:
        if not exhausted and rnd % gap == 0:
            g = next(it, None)
            if g is None:
                exhausted = True
            else:
                active.append(g)
        if exhausted and not active:
            break
        for g in list(active):
            try:
                next(g)
            except StopIteration:
                active.remove(g)
        rnd += 1
GROUPS = [[0, 1], [2, 3], [4, 5], [6, 7]]


def build(S, debug=False):
    NT = S // 128
    CAP = S // 8
    NJ = CAP // 128
    NG = S // 512
    assert NJ >= 1
    nc = bass.Bass("TRN2", target_bir_lowering=False)

    def din(name, shape, dt=F32):
        return nc.dram_tensor(name, shape, dt, kind="ExternalInput").ap()

    def dscr(name, shape, dt, dbg=True):
        return nc.dram_tensor(name, shape, dt, kind="ExternalOutput" if (debug and dbg) else "Internal").ap()

    x = din("x", [S, D])
    pin = din("p", [S // 2, 256])
    rowidx_in = din("rowidx", [128, 3 * (S // 256)], I32)
    pos_l = din("pos_l", [128, NT], I32)
    invf_rep = din("invf_rep", [128, 8])
    gmix_l = din("gmix_l", [128, 8])
    gple_l = din("gple_l", [128, 8])
    ggla_l = din("ggla_l", [128, 4])
    gffn_rep = din("gffn_rep", [128, D])
    gfin_rep = din("gfin_rep", [128, D])
    sink_rep = din("sink_rep", [1, 512])
    upext_f = din("upext_f", [17, 256])
    upext_b = din("upext_b", [17, 256])
    w_in = din("w_in", [D, NCOL])
    w_ba = din("w_ba", [256, D])
    w_bg = din("w_bg", [512, D])
    w_out = din("w_out", [D, D])
    w_router = din("w_router", [D, NEXP])
    rowmask_in = din("rowmask", [NEXP, 2])
    w_eg = din("w_eg", [NEL, D, D])
    w_eu = din("w_eu", [NEL, D, D])
    w_ed = din("w_ed", [NEL, D, D])
    w_pg = din("w_pg", [D, D])
    w_ple = din("w_ple", [256, D])
    y = nc.dram_tensor("y", [S // 2, D], F32, kind="ExternalOutput").ap()

    qT_d = dscr("qT_d", [256, S], BF16)
    kT_d = dscr("kT_d", [64, S], BF16)
    va_d = dscr("va_d", [S, 64], BF16)
    gqT_d = dscr("gqT_d", [256, S], BF16)
    gkT_d = dscr("gkT_d", [256, S], BF16)
    gk_d = dscr("gk_d", [S, 256], BF16)
    gv_d = dscr("gv_d", [S, 512], BF16)
    grT_d = dscr("grT_d", [512, S], BF16)
    gaT_d = dscr("gaT_d", [1024, S], BF16)
    ggT_d = dscr("ggT_d", [1024, S], BF16)
    laf_d = dscr("laf_d", [S, 256], F32)
    lab_d = dscr("lab_d", [S, 256], F32)
    obT_d = dscr("obT_d", [512, S], F32)
    dpart_d = dscr("dpart_d", [S, D], F32, dbg=False)
    dall_d = dscr("dall_d", [NG, 1024, D], F32, dbg=False)
    h1_d = dscr("h1_d", [S, D], F32)
    xn_d = dscr("xn_d", [S, D], BF16)
    yacc_d = dscr("yacc_d", [S, D], F32, dbg=False)
    yall_d = dscr("yall_d", [NG, 1024, D], F32, dbg=False)
    if debug:
        idx_dbg = dscr("idx_dbg", [128, NJ * NEL], U32)
        val_dbg = dscr("val_dbg", [128, NJ * NEL], F32)

    def fm(dram, c0, nch, t):
        return dram[c0 * 128:(c0 + nch) * 128, t * 128:(t + 1) * 128].rearrange("(c p) s -> p c s", p=128)

    with ExitStack() as es:
        P = Prog(nc, es)
        dbufs = {}

        def db(name, idx=0):
            k = (name, idx)
            if k not in dbufs:
                dbufs[k] = Buf(f"{name}_{idx}", dram=True)
            return dbufs[k]

        pers = lambda n, s, d: es.enter_context(nc.sbuf_tensor(n, s, d))
        ident = pers("ident", [128, 128], F32)
        identb = pers("identb", [128, 128], BF16)
        mLE = pers("mLE", [128, 128], F32)
        mGT = pers("mGT", [128, 128], F32)
        mGE = pers("mGE", [128, 128], F32)
        mLT = pers("mLT", [128, 128], F32)
        onesb = pers("onesb", [128, 128], BF16)
        onesf = pers("onesf", [1, 128], F32)
        epsc = pers("epsc", [128, 1], F32)
        HS = S // 2
        affA = pers("affA", [NEXP, HS], F32)
        affB = pers("affB", [NEXP, HS], F32)
        Jm = pers("Jm", [128, 128], F32)
        rowmask = pers("rowmask_sb", [NEXP, 2], F32)
        Bconst = P.buf("const")
        BaffT = P.buf("affT")
        valsT = pers("valsT", [128, NJ, NEL], F32)
        idxT = pers("idxT", [128, NJ, NEL], U32)
        Bsel = P.buf("sel")

        def mk_mask(tile, step, cm, cmp):
            P.memset("pool", tile[:], 1.0, w=[Bconst])
            P.op("pool", lambda e: e.affine_select(out=tile[:], in_=tile[:], pattern=[[step, 128]],
                                                   compare_op=cmp, fill=0.0, base=0, channel_multiplier=cm),
                 r=[Bconst], w=[Bconst])

        mk_mask(ident, -1, 1, ALU.is_equal)
        mk_mask(mLE, 1, -1, ALU.is_ge)
        mk_mask(mGT, -1, 1, ALU.is_gt)
        mk_mask(mGE, -1, 1, ALU.is_ge)
        mk_mask(mLT, 1, -1, ALU.is_gt)
        P.copy("pool", identb[:], ident[:], r=[Bconst], w=[Bconst])
        P.memset("pool", onesb[:], 1.0, w=[Bconst])
        P.memset("pool", onesf[:], 1.0, w=[Bconst])
        P.memset("pool", epsc[:], EPS, w=[Bconst])
        P.memset("pool", Jm[:], 1.0, w=[Bconst])
        P.op("pool", lambda e: e.affine_select(out=Jm[:], in_=Jm[:], pattern=[[1, 128]], compare_op=ALU.is_equal,
                                               fill=0.0, base=-127, channel_multiplier=1), r=[Bconst], w=[Bconst])
        P.dma("sp", rowmask[:], rowmask_in, w=[Bconst], owner=Bconst)

        def rstd_from_ss(ss, Bss, a, b_, c_, scale):
            P.act(ss[:, b_:b_ + 1], ss[:, a:a + 1], AF.Sqrt, scale=scale, bias=epsc[:, 0:1], r=[Bss, Bconst], w=[Bss])
            P.op("dve", (lambda ss_: lambda e: e.reciprocal(out=ss_[:, c_:c_ + 1], in_=ss_[:, b_:b_ + 1]))(ss),
                 r=[Bss], w=[Bss])

        with ExitStack() as ph:
            sbt = lambda n, s, d: ph.enter_context(nc.sbuf_tensor(P.uniq(n), s, d))
            mkp = lambda name, shape, dt, n, psum=False: TPool(P, ph, nc, name, shape, dt, n, psum)
            Win = sbt("Win", [128, 8, NCOL], BF16)
            BWin = P.buf("Win")
            gmix = sbt("gmix", [128, 8], F32)
            Bg = P.buf("gmix")
            upf = sbt("upf", [17, 256], F32)
            upb = sbt("upb", [17, 256], F32)
            posi = sbt("posi", [128, NT], I32)
            posf = sbt("posf", [128, NT], F32)
            invf = sbt("invf", [128, 8], F32)
            ang = sbt("ang", [128, NT, 8], F32)
            cosT = sbt("cosT", [128, NT, 8], F32)
            sinT = sbt("sinT", [128, NT, 8], F32)
            Brope = P.buf("rope")
            Bup = P.buf("up")
            for c in range(8):
                P.dma("pool", Win[:, c, :], w_in[c * 128:(c + 1) * 128, :], w=[BWin], owner=BWin)
            P.dma("sp", gmix[:], gmix_l, w=[Bg], owner=Bg)
            P.dma("sp", upf[:], upext_f, w=[Bup], owner=Bup)
            P.dma("sp", upb[:], upext_b, w=[Bup], owner=Bup)
            P.dma("sp", posi[:], pos_l, w=[Brope], owner=Brope)
            for c in range(8):
                if c % 2 == 0:
                    P.ts("dve", Win[:, c, :], Win[:, c, :], gmix[:, c:c + 1], op0=ALU.mult, r=[Bg, BWin], w=[BWin])
                else:
                    P.act(Win[:, c, :], Win[:, c, :], AF.Copy, scale=gmix[:, c:c + 1], r=[Bg, BWin], w=[BWin])
            P.dma("sp", invf[:], invf_rep, w=[Brope], owner=Brope)
            P.copy("dve", posf[:], posi[:], r=[Brope], w=[Brope])
            for t in range(NT):
                P.ts("dve", ang[:, t, :], invf[:], posf[:, t:t + 1], op0=ALU.mult, r=[Brope], w=[Brope])
            TWO_PI = 2.0 * math.pi
            angf = ang[:].rearrange("p t f -> p (t f)")
            tmpa = sbt("tmpa", [128, NT * 8], F32)
            tmpb = sbt("tmpb", [128, NT * 8], F32)
            tmpk = sbt("tmpk", [128, NT * 8], I32)
            for (outT, shift) in ((sinT, 0.0), (cosT, 0.5 * math.pi)):
                P.ts("dve", tmpa[:], angf, shift, None, op0=ALU.add, r=[Brope], w=[Brope])
                P.ts("dve", tmpb[:], tmpa[:], 1.0 / TWO_PI, None, op0=ALU.mult, r=[Brope], w=[Brope])
                P.copy("dve", tmpk[:], tmpb[:], r=[Brope], w=[Brope])
                P.copy("dve", tmpb[:], tmpk[:], r=[Brope], w=[Brope])
                P.stt(tmpa[:], tmpb[:], -TWO_PI, tmpa[:], ALU.mult, ALU.add, r=[Brope], w=[Brope])
                P.ts("dve", tmpb[:], tmpa[:], math.pi, None, op0=ALU.is_gt, r=[Brope], w=[Brope])
                P.stt(tmpa[:], tmpb[:], -TWO_PI, tmpa[:], ALU.mult, ALU.add, r=[Brope], w=[Brope])
                P.ts("dve", tmpb[:], tmpa[:], -math.pi, None, op0=ALU.is_lt, r=[Brope], w=[Brope])
                P.stt(tmpa[:], tmpb[:], TWO_PI, tmpa[:], ALU.mult, ALU.add, r=[Brope], w=[Brope])
                P.act(outT[:].rearrange("p t f -> p (t f)"), tmpa[:], AF.Sin, r=[Brope], w=[Brope])

            psF = mkp("psF", [128, 512], F32, 6, psum=True)
            psB = mkp("psB", [128, 1024], BF16, 2, psum=True)
            xp = mkp("xp", [128, D], F32, 5)
            junk = mkp("junk", [128, D], BF16, 1)
            ssp = mkp("ssp", [128, 4], F32, 9)
            abp = mkp("abp", [128, D], BF16, 9)
            aTp = mkp("aTp", [128, D], BF16, 3)
            qkp = mkp("qkp", [128, 320], F32, 3)
            rtp = mkp("rtp", [128, 4, 5, 8], F32, 3)
            qkbp = mkp("qkbp", [128, 384], BF16, 3)
            qkTp = mkp("qkTp", [128, 3, 128], BF16, 3)
            vbp = mkp("vbp", [128, 64], BF16, 3)
            gkp = mkp("gkp", [128, 256], BF16, 3)
            gvp = mkp("gvp", [128, 512], BF16, 3)
            f2p = mkp("f2p", [128, 2, 128], BF16, 4)
            f4p = mkp("f4p", [128, 4, 128], BF16, 3)
            f8p = mkp("f8p", [128, 8, 128], BF16, 4)
            zfp = mkp("zfp", [32, 128], F32, 3)
            zbp = mkp("zbp", [32, 128], F32, 3)
            lt1 = mkp("lt1", [128, 256], F32, 3)
            lt2 = mkp("lt2", [128, 256], F32, 3)
            lap = mkp("lap", [128, 256], F32, 3)
            for zp in (zfp, zbp):
                for (zt, zB) in zp.items:
                    P.memset("pool", zt[:], 1.0, w=[zB])
            for (qt_, qB) in qkbp.items:
                P.memset("pool", qt_[:], 0.0, w=[qB])

            aT4p = mkp("aT4p", [128, 8, 512], BF16, 2)
            fst = mkp("fst", [128, 512], BF16, 6)

            def bodyA(g):
                aT4, BaT4 = aT4p.next()
                abs_ = []
                for i in range(4):
                    t = g * 4 + i
                    tok = slice(t * 128, (t + 1) * 128)
                    xt, Bx = xp.next()
                    P.dma("act", xt[:], x[tok, :], w=[Bx])
                    jk, Bj = junk.next()
                    ss, Bss = ssp.next()
                    P.act(jk[:], xt[:], AF.Square, accum_out=ss[:, 0:1], r=[Bx], w=[Bj, Bss])
                    rstd_from_ss(ss, Bss, 0, 1, 2, 1.0 / D)
                    ab, Bab = abp.next()
                    P.act(ab[:], xt[:], AF.Copy, scale=ss[:, 2:3], r=[Bx, Bss], w=[Bab])
                    abs_.append((ab, Bab))
                yield
                for i in range(4):
                    ab, Bab = abs_[i]
                    pT, BpT = psB.next()
                    for c in range(8):
                        P.transpose(pT[:, c * 128:(c + 1) * 128], ab[:, c * 128:(c + 1) * 128], identb[:],
                                    r=[Bab, Bconst], w=[BpT])
                    P.copy("dve", aT4[:, :, i * 128:(i + 1) * 128], pT[:].rearrange("p (c s) -> p c s", c=8),
                           r=[BpT], w=[BaT4])
                    yield
                pending = []
                for i in range(4):
                    t = g * 4 + i
                    tok = slice(t * 128, (t + 1) * 128)
                    tsl = slice(i * 128, (i + 1) * 128)
                    for fn_ in pending:
                        fn_()
                    pending = []

                    def proj_tok(c0, n, tsl=tsl):
                        ps, Bps = psF.next()
                        for c in range(8):
                            P.matmul(ps[:, 0:n], aT4[:, c, tsl], Win[:, c, c0:c0 + n],
                                     start=(c == 0), stop=(c == 7), r=[BaT4, BWin], w=[Bps])
                        return ps, Bps

                    psq, Bpsq = proj_tok(C_AQ, 320)
                    qk, Bqk = qkp.next()
                    P.copy("act", qk[:], psq[:, 0:320], r=[Bpsq], w=[Bqk])
                    qk3 = qk[:].rearrange("p (h d) -> p h d", h=5)
                    t1 = qk3[:, :, 0:8]
                    t2 = qk3[:, :, 8:16]
                    rt, Brt = rtp.next()
                    cb = cosT[:, t:t + 1, :].to_broadcast([128, 5, 8])
                    sb_ = sinT[:, t:t + 1, :].to_broadcast([128, 5, 8])
                    P.tt("pool", rt[:, 0], t1, cb, ALU.mult, r=[Bqk, Brope], w=[Brt])
                    P.tt("pool", rt[:, 1], t2, sb_, ALU.mult, r=[Bqk, Brope], w=[Brt])
                    P.tt("pool", rt[:, 2], t2, cb, ALU.mult, r=[Bqk, Brope], w=[Brt])
                    P.tt("pool", rt[:, 3], t1, sb_, ALU.mult, r=[Bqk, Brope], w=[Brt])
                    P.tt("pool", t1, rt[:, 0], rt[:, 1], ALU.subtract, r=[Brt], w=[Bqk])
                    P.tt("pool", t2, rt[:, 2], rt[:, 3], ALU.add, r=[Brt], w=[Bqk])
                    qkb, Bqkb = qkbp.next()
                    P.copy("act", qkb[:, 0:320], qk[:], r=[Bqk], w=[Bqkb])

                    def fin_qk(qkb=qkb, Bqkb=Bqkb, t=t, tok=tok):
                        pT2, BpT2 = psB.next()
                        for c in range(3):
                            P.transpose(pT2[:, c * 128:(c + 1) * 128], qkb[:, c * 128:(c + 1) * 128], identb[:],
                                        r=[Bqkb, Bconst], w=[BpT2])
                        qkT, BqkT = qkTp.next()
                        P.copy("dve", qkT[:].rearrange("p c s -> p (c s)"), pT2[:, 0:384], r=[BpT2], w=[BqkT])
                        P.dma("sp", fm(qT_d, 0, 2, t), qkT[:, 0:2, :], r=[BqkT], w=[db("qT", t)])
                        P.dma("sp", kT_d[:, tok], qkT[0:64, 2, :], r=[BqkT], w=[db("kT", t)])
                    pending.append(fin_qk)
                    ps, Bps = proj_tok(C_AV, 64)
                    vb, Bvb = vbp.next()
                    P.copy("dve", vb[:], ps[:, 0:64], r=[Bps], w=[Bvb])
                    P.dma("sp", va_d[tok, :], vb[:], r=[Bvb], w=[db("va", t)])
                    ps, Bps = proj_tok(C_GK, 256)
                    gkt, Bgk = gkp.next()
                    P.copy("dve", gkt[:], ps[:, 0:256], r=[Bps], w=[Bgk])
                    P.dma("sp", gk_d[tok, :], gkt[:], r=[Bgk], w=[db("gk", t)])
                    gvt, Bgv = gvp.next()
                    ps, Bps = proj_tok(C_GV, 512)
                    P.copy("dve", gvt[:], ps[:, 0:512], r=[Bps], w=[Bgv])
                    P.dma("sp", gv_d[tok, :], gvt[:], r=[Bgv], w=[db("gv", t)])
                    ps, Bps = psF.next()
                    for zi, c0 in enumerate((C_ZF, C_ZB)):
                        for c in range(8):
                            P.matmul(ps[0:16, zi * 128:(zi + 1) * 128], Win[:, c, c0:c0 + 16], aT4[:, c, tsl],
                                     start=(c == 0), stop=(c == 7), r=[BaT4, BWin], w=[Bps])
                    zf, Bzf = zfp.next()
                    zb, Bzb = zbp.next()
                    P.copy("dve", zf[0:16, :], ps[0:16, 0:128], r=[Bps], w=[Bzf])
                    P.copy("dve", zb[0:16, :], ps[0:16, 128:256], r=[Bps], w=[Bzb])

                    def fin_la(zf=zf, Bzf=Bzf, zb=zb, Bzb=Bzb, t=t, tok=tok):
                        for (zt, Bz, up, dram, nm) in ((zf, Bzf, upf, laf_d, "laf"), (zb, Bzb, upb, lab_d, "lab")):
                            ps2, Bps2 = psF.next()
                            P.matmul(ps2[:, 0:256], zt[0:17, :], up[0:17, :], r=[Bz, Bup], w=[Bps2])
                            a1, B1 = lt1.next()
                            P.act(a1[:], ps2[:, 0:256], AF.Exp, scale=-1.0, r=[Bps2], w=[B1])
                            a2, B2 = lt2.next()
                            P.act(a2[:], a1[:], AF.Ln, bias=1.0, r=[B1], w=[B2])
                            a3, B3 = lap.next()
                            P.ts("pool", a3[:], a2[:], -1.0 / 16.0, None, op0=ALU.mult, r=[B2], w=[B3])
                            P.dma("sp", dram[tok, :], a3[:], r=[B3], w=[db(nm, t)])
                    pending.append(fin_la)
                    yield
                for fn_ in pending:
                    fn_()
                yield
                gsl = slice(g * 512, (g + 1) * 512)
                for (c0, nch, dram, nm, fn) in ((C_GQ, 2, gqT_d, "gqT", None), (C_GK, 2, gkT_d, "gkT", None),
                                                (C_GR, 4, grT_d, "grT", AF.Silu), (C_GA, 8, gaT_d, "gaT", AF.Sigmoid),
                                                (C_GG, 8, ggT_d, "ggT", AF.Sigmoid)):
                    for k in range(nch):
                        ps, Bps = psF.next()
                        for c in range(8):
                            P.matmul(ps[:, 0:512], Win[:, c, c0 + k * 128:c0 + (k + 1) * 128], aT4[:, c, :],
                                     start=(c == 0), stop=(c == 7), r=[BaT4, BWin], w=[Bps])
                        st, Bst = fst.next()
                        if fn is None:
                            P.copy("dve", st[:], ps[:, 0:512], r=[Bps], w=[Bst])
                        else:
                            P.act(st[:], ps[:, 0:512], fn, r=[Bps], w=[Bst])
                        P.dma("sp", dram[k * 128:(k + 1) * 128, gsl], st[:], r=[Bst], w=[db(nm + "_c%d" % k, g)])
                        if k % 2 == 1:
                            yield
            run_pipelined([bodyA(g) for g in range(NT // 4)], 11)
            P.barrier()
            P.emit()
            P.end_phase()

        def gla_setup(ph):
            mkp = lambda name, shape, dt, n, psum=False: TPool(P, ph, nc, name, shape, dt, n, psum)
            G = {}
            G["gqT"] = mkp("g_gqT", [128, 2, 128], BF16, 3)
            G["gkT"] = mkp("g_gkT", [128, 2, 128], BF16, 3)
            G["gk"] = mkp("g_gk", [128, 256], BF16, 3)
            G["gv"] = mkp("g_gv", [128, 512], BF16, 3)
            G["la"] = mkp("g_la", [128, 256], F32, 3)
            G["Eq"] = mkp("g_Eq", [128, 256], F32, 3)
            G["Ek"] = mkp("g_Ek", [128, 256], F32, 3)
            G["Ee"] = mkp("g_Ee", [128, 256], F32, 3)
            G["qt"] = mkp("g_qt", [128, 2, 128], BF16, 3)
            G["kt"] = mkp("g_kt", [128, 2, 128], BF16, 3)
            G["ke"] = mkp("g_ke", [128, 256], BF16, 3)
            G["at"] = mkp("g_at", [128, 2, 128], BF16, 3)
            sbt = lambda n, s, d: ph.enter_context(nc.sbuf_tensor(P.uniq(n), s, d))
            G["S"] = sbt("g_S", [128, 2, 256], F32)
            G["Sb"] = sbt("g_Sb", [128, 2, 256], BF16)
            G["BS"] = P.buf("S")
            G["BSb"] = P.buf("Sb")
            return G

        def gla_reset(G):
            P.memset("pool", G["S"][:], 0.0, w=[G["BS"]])
            P.memset("pool", G["Sb"][:], 0.0, w=[G["BSb"]])

        def gla_tile(G, psF, t, fwd):
            tok = slice(t * 128, (t + 1) * 128)
            la_d, la_nm = (laf_d, "laf") if fwd else (lab_d, "lab")
            m_incl, m_strict = (mLE, mGT) if fwd else (mGE, mLT)
            m_attn = mLE if fwd else mGT
            gq, Bgq = G["gqT"].next()
            P.dma("sp", gq[:], fm(gqT_d, 0, 2, t), r=[db("gqT", t)], w=[Bgq])
            gkT, BgkT = G["gkT"].next()
            P.dma("sp", gkT[:], fm(gkT_d, 0, 2, t), r=[db("gkT", t)], w=[BgkT])
            gk, Bgk = G["gk"].next()
            P.dma("sp", gk[:], gk_d[tok, :], r=[db("gk", t)], w=[Bgk])
            gv, Bgv = G["gv"].next()
            P.dma("sp", gv[:], gv_d[tok, :], r=[db("gv", t)], w=[Bgv])
            la, Bla = G["la"].next()
            P.dma("sp", la[:], la_d[tok, :], r=[db(la_nm, t)], w=[Bla])
            yield
            psb, Bpsb = psF.next()
            for h in range(2):
                P.matmul(psb[:, h * 128:(h + 1) * 128], la[:, h * 128:(h + 1) * 128], m_incl[:],
                         r=[Bla, Bconst], w=[Bpsb])
            psg, Bpsg = psF.next()
            P.matmul(psg[:, 0:256], m_strict[:], la[:], r=[Bla, Bconst], w=[Bpsg])
            Eq, BEq = G["Eq"].next()
            P.act(Eq[:], psb[:, 0:256], AF.Exp, r=[Bpsb], w=[BEq])
            Ek, BEk = G["Ek"].next()
            P.act(Ek[:], psb[:, 0:256], AF.Exp, scale=-1.0, r=[Bpsb], w=[BEk])
            Ee, BEe = G["Ee"].next()
            P.act(Ee[:], psg[:, 0:256], AF.Exp, r=[Bpsg], w=[BEe])
            qt, Bqt = G["qt"].next()
            P.stt(qt[:].rearrange("p h l -> p (h l)"), gq[:].rearrange("p h l -> p (h l)"), 128.0 ** -0.5, Eq[:],
                  ALU.mult, ALU.mult, r=[Bgq, BEq], w=[Bqt])
            kt, Bkt = G["kt"].next()
            P.tt("dve", kt[:].rearrange("p h l -> p (h l)"), gkT[:].rearrange("p h l -> p (h l)"), Ek[:], ALU.mult,
                 r=[BgkT, BEk], w=[Bkt])
            ke, Bke = G["ke"].next()
            P.tt("dve", ke[:], gk[:], Ee[:], ALU.mult, r=[Bgk, BEe], w=[Bke])
            yield
            psa, Bpsa = psF.next()
            for h in range(2):
                P.matmul(psa[:, h * 128:(h + 1) * 128], kt[:, h, :], qt[:, h, :], r=[Bkt, Bqt], w=[Bpsa])
            at, Bat = G["at"].next()
            P.tt("dve", at[:], psa[:, 0:256].rearrange("p (h l) -> p h l", h=2),
                 m_attn[:].unsqueeze(1).to_broadcast([128, 2, 128]), ALU.mult, r=[Bpsa, Bconst], w=[Bat])
            dcol = 127 if fwd else 0
            pk, Bpk = psF.next()
            for h in range(2):
                P.matmul(pk[:, h * 256:(h + 1) * 256], ke[:, h * 128:(h + 1) * 128], gv[:, h * 256:(h + 1) * 256],
                         r=[Bke, Bgv], w=[Bpk])
            for h in range(2):
                P.stt(G["S"][:, h, :], G["S"][:, h, :], Eq[:, h * 128 + dcol:h * 128 + dcol + 1],
                      pk[:, h * 256:(h + 1) * 256], ALU.mult, ALU.add, r=[BEq, Bpk, G["BS"]], w=[G["BS"]])
            po, Bpo = psF.next()
            for h in range(2):
                for ec in range(2):
                    o_ap = po[:, (h * 2 + ec) * 128:(h * 2 + ec + 1) * 128]
                    P.matmul(o_ap, gv[:, h * 256 + ec * 128:h * 256 + (ec + 1) * 128], at[:, h, :],
                             start=True, stop=False, r=[Bgv, Bat], w=[Bpo])
                    P.matmul(o_ap, G["Sb"][:, h, ec * 128:(ec + 1) * 128], qt[:, h, :],
                             start=False, stop=True, r=[G["BSb"], Bqt], w=[Bpo])
            P.copy("act", G["Sb"][:].rearrange("p h e -> p (h e)"), G["S"][:].rearrange("p h e -> p (h e)"),
                   r=[G["BS"]], w=[G["BSb"]])
            return po, Bpo

        with ExitStack() as ph:
            mkp = lambda name, shape, dt, n, psum=False: TPool(P, ph, nc, name, shape, dt, n, psum)
            psF = mkp("psFb", [128, 512], F32, 8, psum=True)
            G = gla_setup(ph)
            obp = mkp("obp", [128, 4, 128], F32, 3)
            gla_reset(G)
            def bodyB(t):
                po, Bpo = yield from gla_tile(G, psF, t, False)
                ob, Bob = obp.next()
                P.copy("act", ob[:].rearrange("p c s -> p (c s)"), po[:, 0:512], r=[Bpo], w=[Bob])
                P.dma("pool", fm(obT_d, 0, 4, t), ob[:], r=[Bob], w=[db("obT", t)])
            run_pipelined([bodyB(t) for t in range(NT - 1, -1, -1)], 1)
            P.barrier()
            P.emit()
            P.end_phase()

        with ExitStack() as ph:
            sbt = lambda n, s, d: ph.enter_context(nc.sbuf_tensor(P.uniq(n), s, d))
            mkp = lambda name, shape, dt, n, psum=False: TPool(P, ph, nc, name, shape, dt, n, psum)
            psF = mkp("psFc", [128, 512], F32, 8, psum=True)
            G = gla_setup(ph)
            Wa = sbt("Wa", [64, 4, D], BF16)
            Wb = sbt("Wb", [128, 4, D], BF16)
            Wo = sbt("Wo", [128, 8, D], BF16)
            ggla = sbt("ggla", [128, 4], F32)
            sinkr = sbt("sinkr", [1, 512], F32)
            BW = P.buf("Wc")
            P.dma("pool", Wa[:], w_ba.rearrange("(h d) c -> d h c", d=64), w=[BW], owner=BW)
            P.dma("pool", Wb[:], w_bg.rearrange("(k p) c -> p k c", p=128), w=[BW], owner=BW)
            P.dma("pool", Wo[:], w_out.rearrange("(k p) c -> p k c", p=128), w=[BW], owner=BW)
            P.dma("sp", ggla[:], ggla_l, w=[BW], owner=BW)
            P.dma("sp", sinkr[:], sink_rep, w=[BW], owner=BW)
            P.act(sinkr[:], sinkr[:], AF.Exp, r=[BW], w=[BW])

            obl = mkp("obl", [128, 4, 128], F32, 3)
            grl = mkp("grl", [128, 4, 128], BF16, 3)
            gal = mkp("gal", [128, 8, 128], BF16, 3)
            ggl = mkp("ggl", [128, 8, 128], BF16, 3)
            osb = mkp("osb", [128, 4, 128], F32, 3)
            sqb = mkp("sqb", [128, 4, 128], BF16, 3)
            rsd = mkp("rsd", [128, 2, 128], F32, 3)
            ogp = mkp("ogp", [128, 4, 128], F32, 3)
            ogb = mkp("ogb", [128, 4, 128], BF16, 3)
            qTl = mkp("qTl", [64, 4, 128], BF16, 3)
            kTl = mkp("kTl", [64, 384], BF16, 3)
            val = mkp("val", [128, 3, 64], BF16, 3)
            ptp = mkp("ptp", [128, 4, 128], BF16, 4)
            rdn = mkp("rdn", [64, 512], F32, 3)
            atp = mkp("atp", [64, 4, 128], BF16, 3)
            t1p = mkp("t1p", [128, 8, 128], F32, 3)
            t2p = mkp("t2p", [128, 8, 128], F32, 3)
            mgp = mkp("mgp", [128, 8, 128], BF16, 3)
            dpp = mkp("dpp", [128, D], F32, 3)
            gla_reset(G)
            Bcc = P.buf("ccd")

            def bodyC(t):
                tok = slice(t * 128, (t + 1) * 128)
                po, Bpo = yield from gla_tile(G, psF, t, True)
                ob, Bob = obl.next()
                P.dma("sp", ob[:], fm(obT_d, 0, 4, t), r=[db("obT", t)], w=[Bob])
                gr, Bgr = grl.next()
                P.dma("sp", gr[:], fm(grT_d, 0, 4, t), r=[db("grT", t)], w=[Bgr])
                o, Bo = osb.next()
                P.tt("dve", o[:].rearrange("p c s -> p (c s)"), po[:, 0:512], ob[:].rearrange("p c s -> p (c s)"),
                     ALU.add, r=[Bpo, Bob], w=[Bo])
                sq, Bsq = sqb.next()
                P.act(sq[:].rearrange("p c s -> p (c s)"), o[:].rearrange("p c s -> p (c s)"), AF.Square, r=[Bo], w=[Bsq])
                pss, Bpss = psF.next()
                for h in range(2):
                    for ec in range(2):
                        P.matmul(pss[:, h * 128:(h + 1) * 128], onesb[:], sq[:, h * 2 + ec, :], start=(ec == 0),
                                 stop=(ec == 1), r=[Bconst, Bsq], w=[Bpss])
                rs, Brs = rsd.next()
                rsf = rs[:].rearrange("p h s -> p (h s)")
                P.act(rsf, pss[:, 0:256], AF.Sqrt, scale=1.0 / 256.0, bias=epsc[:, 0:1], r=[Bpss, Bconst], w=[Brs])
                P.op("dve", (lambda rsf: lambda e: e.reciprocal(out=rsf, in_=rsf))(rsf), r=[Brs], w=[Brs])
                og, Bog = ogp.next()
                for c in range(4):
                    P.stt(og[:, c, :], o[:, c, :], ggla[:, c:c + 1], rs[:, c // 2, :], ALU.mult, ALU.mult,
                          r=[Bo, BW, Brs], w=[Bog])
                ogT, BogT = ogb.next()
                P.tt("dve", ogT[:].rearrange("p c s -> p (c s)"), og[:].rearrange("p c s -> p (c s)"),
                     gr[:].rearrange("p c s -> p (c s)"), ALU.mult, r=[Bog, Bgr], w=[BogT])
                yield
                kbs = [kb for kb in (-1, 0, 1) if 0 <= t + kb < NT]
                qTt, BqT = qTl.next()
                P.dma("sp", qTt[:], qT_d[:, tok].rearrange("(h d) s -> d h s", d=64), r=[db("qT", t)], w=[BqT])
                kTt, BkT = kTl.next()
                vat, Bva = val.next()
                for kb in kbs:
                    tk = t + kb
                    P.dma("sp", kTt[:, (kb + 1) * 128:(kb + 2) * 128], kT_d[:, tk * 128:(tk + 1) * 128],
                          r=[db("kT", tk)], w=[BkT])
                    P.dma("sp", vat[:, kb + 1, :], va_d[tk * 128:(tk + 1) * 128, :], r=[db("va", tk)], w=[Bva])
                at_, Bat_ = atp.next()
                pts = []
                for kb in kbs:
                    psc, Bpsc = psF.next()
                    P.matmul(psc[:, 0:512], kTt[:, (kb + 1) * 128:(kb + 2) * 128],
                             qTt[:].rearrange("d h s -> d (h s)"), r=[BkT, BqT], w=[Bpsc])
                    pt, Bpt = ptp.next()
                    P.act(pt[:].rearrange("p h s -> p (h s)"), psc[:, 0:512], AF.Exp, scale=0.125, r=[Bpsc], w=[Bpt])
                    if kb != 0:
                        mk = mGE if kb == -1 else mLE
                        P.tt("dve", pt[:], pt[:], mk[:].unsqueeze(1).to_broadcast([128, 4, 128]), ALU.mult,
                             r=[Bpt, Bconst], w=[Bpt])
                    pts.append((kb, pt, Bpt))
                gg, Bgg = ggl.next()
                P.dma("sp", gg[:], fm(ggT_d, 0, 8, t), r=[db("ggT", t)], w=[Bgg])
                t2_, Bt2 = t2p.next()
                for half in range(2):
                    pyg, Bpyg = psF.next()
                    for cc in range(4):
                        c = half * 4 + cc
                        for k in range(4):
                            P.matmul(pyg[:, cc * 128:(cc + 1) * 128], Wb[:, k, c * 128:(c + 1) * 128], ogT[:, k, :],
                                     start=(k == 0), stop=(k == 3), r=[BW, BogT], w=[Bpyg])
                    P.tt("dve", t2_[:, half * 4:(half + 1) * 4, :].rearrange("p c s -> p (c s)"), pyg[:, 0:512],
                         gg[:, half * 4:(half + 1) * 4, :].rearrange("p c s -> p (c s)"), ALU.mult, r=[Bpyg, Bgg], w=[Bt2])
                pso, Bpso = psF.next()
                psd, Bpsd = psF.next()
                for i, (kb, pt, Bpt) in enumerate(pts):
                    ptf = pt[:].rearrange("p h s -> p (h s)")
                    P.matmul(pso[0:64, 0:512], vat[:, kb + 1, :], ptf, start=(i == 0),
                             stop=(i == len(pts) - 1), r=[Bva, Bpt], w=[Bpso])
                    P.matmul(psd[0:64, 0:512], onesb[:, 0:64], ptf, start=(i == 0), stop=False,
                             r=[Bconst, Bpt], w=[Bpsd])
                P.matmul(psd[0:64, 0:512], onesf[0:1, 0:64], sinkr[0:1, :], start=False,
                         stop=True, r=[Bconst, BW], w=[Bpsd])
                rd, Brd = rdn.next()
                P.op("dve", (lambda rd, psd: lambda e: e.reciprocal(out=rd[:], in_=psd[0:64, 0:512]))(rd, psd),
                     r=[Bpsd], w=[Brd])
                P.tt("dve", at_[:].rearrange("d h s -> d (h s)"), pso[0:64, 0:512], rd[:],
                     ALU.mult, r=[Bpso, Brd], w=[Bat_])
                yield
                ga, Bga = gal.next()
                P.dma("sp", ga[:], fm(gaT_d, 0, 8, t), r=[db("gaT", t)], w=[Bga])
                t1_, Bt1 = t1p.next()
                for half in range(2):
                    pya, Bpya = psF.next()
                    for cc in range(4):
                        c = half * 4 + cc
                        for h in range(4):
                            P.matmul(pya[:, cc * 128:(cc + 1) * 128], Wa[:, h, c * 128:(c + 1) * 128], at_[:, h, :],
                                     start=(h == 0), stop=(h == 3), r=[BW, Bat_], w=[Bpya])
                    P.tt("dve", t1_[:, half * 4:(half + 1) * 4, :].rearrange("p c s -> p (c s)"), pya[:, 0:512],
                         ga[:, half * 4:(half + 1) * 4, :].rearrange("p c s -> p (c s)"), ALU.mult, r=[Bpya, Bga], w=[Bt1])
                mg, Bmg = mgp.next()
                P.tt("dve", mg[:].rearrange("p c s -> p (c s)"), t1_[:].rearrange("p c s -> p (c s)"),
                     t2_[:].rearrange("p c s -> p (c s)"), ALU.add, r=[Bt1, Bt2], w=[Bmg])
                yield
                dp, Bdp = dpp.next()
                for half in range(2):
                    psh, Bpsh = psF.next()
                    for c in range(8):
                        P.matmul(psh[:, 0:512], mg[:, c, :], Wo[:, c, half * 512:(half + 1) * 512], start=(c == 0),
                                 stop=(c == 7), r=[Bmg, BW], w=[Bpsh])
                    P.copy("act" if half == 0 else "dve", dp[:, half * 512:(half + 1) * 512], psh[:, 0:512],
                           r=[Bpsh], w=[Bdp])
                g4 = t // 4
                P.dma("pool", dpart_d[tok, :], dp[:], r=[Bdp], w=[db("dpart", g4)])
                if t % 4 == 3:
                    P.dma_fn("pool", (lambda g4: lambda e: e.collective_compute(
                        "AllGather", op=ALU.bypass, replica_groups=GROUPS,
                        ins=[dpart_d[g4 * 512:(g4 + 1) * 512, :]], outs=[dall_d[g4]]))(g4),
                        r=[db("dpart", g4)], w=[db("dall", g4)], owner=Bcc, inc=1)
            run_pipelined([bodyC(t) for t in range(NT)], 2)
            P.barrier()
            P.emit()
            P.end_phase()

        with ExitStack() as ph:
            sbt = lambda n, s, d: ph.enter_context(nc.sbuf_tensor(P.uniq(n), s, d))
            mkp = lambda name, shape, dt, n, psum=False: TPool(P, ph, nc, name, shape, dt, n, psum)
            psF = mkp("psFc2", [128, 512], F32, 8, psum=True)
            Wr = sbt("Wr", [128, 8, NEXP], F32)
            WrB = sbt("WrB", [128, 8, NEXP], F32)
            gffn = sbt("gffn", [128, D], F32)
            BW = P.buf("Wc2")
            P.dma("sp", Wr[:], w_router.rearrange("(k p) e -> p k e", p=128), w=[BW], owner=BW)
            P.copy("dve", WrB[:, :, 0:8], Wr[:, :, 8:16], r=[BW], w=[BW])
            P.copy("dve", WrB[:, :, 8:16], Wr[:, :, 0:8], r=[BW], w=[BW])
            P.dma("sp", gffn[:], gffn_rep, w=[BW], owner=BW)
            xl = mkp("xl", [128, D], F32, 4)
            d0l = mkp("d0l", [128, D], F32, 4)
            d1l = mkp("d1l", [128, D], F32, 4)
            h1p = mkp("h1p", [128, D], F32, 4)
            jk2 = mkp("jk2", [128, D], BF16, 1)
            ss2 = mkp("ss2", [128, 4], F32, 4)
            xnp = mkp("xnp", [128, D], F32, 4)
            xnb = mkp("xnb", [128, D], BF16, 4)
            xnT = mkp("xnT", [128, 8, 128], F32, 4)
            lgp = mkp("lgp", [128, 16], F32, 4)
            smp = mkp("smp", [128, 4], F32, 4)
            afp = mkp("afp", [128, 16], F32, 4)
            def bodyC2(t):
                tok = slice(t * 128, (t + 1) * 128)
                g4, i4 = t // 4, t % 4
                xt, Bx = xl.next()
                P.dma("sp", xt[:], x[tok, :], w=[Bx])
                d0, Bd0 = d0l.next()
                P.dma("sp", d0[:], dall_d[g4, i4 * 128:(i4 + 1) * 128, :], r=[db("dall", g4)], w=[Bd0])
                d1, Bd1 = d1l.next()
                P.dma("sp", d1[:], dall_d[g4, 512 + i4 * 128:512 + (i4 + 1) * 128, :], r=[db("dall", g4)], w=[Bd1])
                h1, Bh1 = h1p.next()
                P.tt("dve", d0[:], d0[:], d1[:], ALU.add, r=[Bd0, Bd1], w=[Bd0])
                P.tt("dve", h1[:], d0[:], xt[:], ALU.add, r=[Bd0, Bx], w=[Bh1])
                P.dma("pool", h1_d[tok, :], h1[:], r=[Bh1], w=[db("h1", t)])
                jk, Bj = jk2.next()
                ss, Bss = ss2.next()
                P.act(jk[:], h1[:], AF.Square, accum_out=ss[:, 0:1], r=[Bh1], w=[Bj, Bss])
                rstd_from_ss(ss, Bss, 0, 1, 2, 1.0 / D)
                xn, Bxn = xnp.next()
                P.stt(xn[:], h1[:], ss[:, 2:3], gffn[:], ALU.mult, ALU.mult, r=[Bh1, Bss, BW], w=[Bxn])
                xb_, Bxb = xnb.next()
                P.copy("act", xb_[:], xn[:], r=[Bxn], w=[Bxb])
                P.dma("pool", xn_d[tok, :], xb_[:], r=[Bxb], w=[db("xn", t)])
                yield
                xT_, BxT = xnT.next()
                for half in range(2):
                    pst, Bpst = psF.next()
                    for cc in range(4):
                        c = half * 4 + cc
                        P.transpose(pst[:, cc * 128:(cc + 1) * 128], xn[:, c * 128:(c + 1) * 128], ident[:],
                                    r=[Bxn, Bconst], w=[Bpst])
                    P.copy("act", xT_[:, half * 4:(half + 1) * 4, :].rearrange("p c s -> p (c s)"), pst[:, 0:512],
                           r=[Bpst], w=[BxT])
                yield
                psl, Bpsl = psF.next()
                for c in range(8):
                    P.matmul(psl[:, 0:NEXP], xT_[:, c, :], (Wr if t < NT // 2 else WrB)[:, c, :], start=(c == 0),
                             stop=(c == 7), r=[BxT, BW], w=[Bpsl])
                lg, Blg = lgp.next()
                P.copy("dve", lg[:], psl[:, 0:NEXP], r=[Bpsl], w=[Blg])
                sm, Bsm = smp.next()
                P.op("dve", (lambda sm, lg: lambda e: e.reduce_max(out=sm[:, 0:1], in_=lg[:], axis=AX.X))(sm, lg),
                     r=[Blg], w=[Bsm])
                P.ts("dve", sm[:, 1:2], sm[:, 0:1], -1.0, None, op0=ALU.mult, r=[Bsm], w=[Bsm])
                af, Baf = afp.next()
                P.act(af[:], lg[:], AF.Exp, bias=sm[:, 1:2], accum_out=sm[:, 2:3], r=[Blg, Bsm], w=[Baf, Bsm])
                P.op("dve", (lambda sm: lambda e: e.reciprocal(out=sm[:, 3:4], in_=sm[:, 2:3]))(sm), r=[Bsm], w=[Bsm])
                P.ts("dve", af[:], af[:], sm[:, 3:4], None, op0=ALU.mult, r=[Baf, Bsm], w=[Baf])
                psf_, Bpsf = psF.next()
                P.transpose(psf_[0:NEXP, 0:128], af[:], ident[:], r=[Baf, Bconst], w=[Bpsf])
                if t < NT // 2:
                    P.copy("dve", affA[:, t * 128:(t + 1) * 128], psf_[0:NEXP, 0:128], r=[Bpsf], w=[BaffT])
                else:
                    P.copy("dve", affB[:, (t - NT // 2) * 128:(t - NT // 2 + 1) * 128], psf_[0:NEXP, 0:128], r=[Bpsf],
                           w=[BaffT])
            run_pipelined([bodyC2(t) for t in range(NT)], 1)
            P.barrier()
            P.emit()
            P.end_phase()

        phFw = ExitStack()
        Wpg = phFw.enter_context(nc.sbuf_tensor("Wpg", [128, 8, D], BF16))
        Wpl = phFw.enter_context(nc.sbuf_tensor("Wpl", [128, 2, D], BF16))
        gple = phFw.enter_context(nc.sbuf_tensor("gple", [128, 8], F32))
        gfin = phFw.enter_context(nc.sbuf_tensor("gfin", [128, D], F32))
        BWf = P.buf("Wf")
        phDE = ExitStack()
        wgp = TPool(P, phDE, nc, "wgp", [128, 8, D], BF16, 2)
        wup = TPool(P, phDE, nc, "wup", [128, 8, D], BF16, 2)
        wdp = TPool(P, phDE, nc, "wdp", [128, 8, D], BF16, 2)
        wst = TPool(P, phDE, nc, "wst", [128, D], F32, 3)
        cast_rr = [0]

        def load_w(e, engs):
            res = []
            for pool_, dram in ((wgp, w_eg), (wup, w_eu), (wdp, w_ed)):
                wt, Bw = pool_.next()
                for k in range(8):
                    st, Bst = wst.next()
                    P.dma("sp", st[:], dram[e, k * 128:(k + 1) * 128, :], w=[Bst])
                    ce = engs[cast_rr[0] % len(engs)]
                    cast_rr[0] += 1
                    P.copy(ce, wt[:, k, :], st[:], r=[Bst], w=[Bw])
                res.append((wt, Bw))
            return res

        with ExitStack() as ph:
            sbt = lambda n, s, d: ph.enter_context(nc.sbuf_tensor(P.uniq(n), s, d))
            mkp = lambda name, shape, dt, n, psum=False: TPool(P, ph, nc, name, shape, dt, n, psum)
            psF = mkp("psFd", [128, 512], F32, 2, psum=True)
            work = sbt("work", [NEXP, HS], F32)
            vals = sbt("vals", [NEXP, CAP], F32)
            idxs = sbt("idxs", [NEXP, CAP], U32)
            idxf = sbt("idxf", [NEXP, CAP], F32)
            tv = sbt("tv", [128, NJ, NEXP], F32)
            ti = sbt("ti", [128, NJ, NEXP], F32)
            itf = sbt("itf", [128, NJ, NEL], F32)
            rbp = mkp("rbp", [128, 16], F32, 2)
            wk = mkp("wkd", [128, 4, NEL], F32, 2)
            zt = sbt("zt", [128, D], F32)
            Bwork, Bvals, Bidx, Bidxf, Bitf, Bzt, Btv, Bti = [P.buf() for _ in range(8)]
            P.memset("pool", zt[:], 0.0, w=[Bzt])
            for t in range(NT):
                P.dma("pool", yacc_d[t * 128:(t + 1) * 128, :], zt[:], r=[Bzt], w=[db("yacc")], owner=Bzt)
            w_ready = [load_w(0, ("act",)), load_w(1, ("act",))]
            P.ts("dve", work[:], affA[:], rowmask[:, 0:1], op0=ALU.mult, r=[BaffT, Bconst], w=[Bwork])
            P.stt(work[:], affB[:], rowmask[:, 1:2], work[:], ALU.mult, ALU.add, r=[BaffT, Bconst, Bwork], w=[Bwork])
            for it in range(CAP // 8):
                sl = slice(it * 8, it * 8 + 8)
                P.op("dve", (lambda sl: lambda e: e.max(out=vals[:, sl], in_=work[:]))(sl), r=[Bwork], w=[Bvals])
                P.op("dve", (lambda sl: lambda e: e.max_index(out=idxs[:, sl], in_max=vals[:, sl], in_values=work[:]))(sl),
                     r=[Bwork, Bvals], w=[Bidx])
                P.op("dve", (lambda sl: lambda e: e.match_replace(out=work[:], in_to_replace=vals[:, sl],
                                                                 in_values=work[:], imm_value=-1.0))(sl),
                     r=[Bwork, Bvals], w=[Bwork])
            P.copy("dve", idxf[:], idxs[:], r=[Bidx], w=[Bidxf])
            for j in range(NJ):
                ps, Bps = psF.next()
                P.transpose(ps[:, 0:NEXP], vals[:, j * 128:(j + 1) * 128], ident[0:NEXP, 0:NEXP], r=[Bvals, Bconst], w=[Bps])
                P.copy("act", tv[:, j, :], ps[:, 0:NEXP], r=[Bps], w=[Btv])
                ps, Bps = psF.next()
                P.transpose(ps[:, 0:NEXP], idxf[:, j * 128:(j + 1) * 128], ident[0:NEXP, 0:NEXP], r=[Bidxf, Bconst], w=[Bps])
                P.copy("act", ti[:, j, :], ps[:, 0:NEXP], r=[Bps], w=[Bti])
            for j in range(NJ):
                jr = NJ - 1 - j
                ps, Bps = psF.next()
                P.matmul(ps[:, 0:8], Jm[:], tv[:, jr, 8:16], r=[Bconst, Btv], w=[Bps])
                P.matmul(ps[:, 8:16], Jm[:], ti[:, jr, 8:16], r=[Bconst, Bti], w=[Bps])
                rb, Brb = rbp.next()
                P.copy("act", rb[:], ps[:, 0:16], r=[Bps], w=[Brb])
                w4, Bw4 = wk.next()
                P.tt("dve", w4[:, 0, :], tv[:, j, 0:8], rb[:, 0:8], ALU.is_gt, r=[Btv, Brb], w=[Bw4])
                P.tt("dve", valsT[:, j, :], tv[:, j, 0:8], rb[:, 0:8], ALU.max, r=[Btv, Brb], w=[Bsel])
                P.ts("dve", w4[:, 1, :], rb[:, 8:16], float(HS), None, op0=ALU.add, r=[Brb], w=[Bw4])
                P.tt("dve", w4[:, 2, :], ti[:, j, 0:8], w4[:, 1, :], ALU.subtract, r=[Bti, Bw4], w=[Bw4])
                P.tt("dve", w4[:, 3, :], w4[:, 2, :], w4[:, 0, :], ALU.mult, r=[Bw4], w=[Bw4])
                P.tt("dve", itf[:, j, :], w4[:, 3, :], w4[:, 1, :], ALU.add, r=[Bw4], w=[Bitf])
            P.copy("dve", idxT[:], itf[:], r=[Bitf], w=[Bsel])
            if debug:
                P.dma("sp", idx_dbg, idxT[:].rearrange("p j e -> p (j e)"), r=[Bsel], w=[db("idxdbg")], owner=Bidx)
                P.dma("sp", val_dbg, valsT[:].rearrange("p j e -> p (j e)"), r=[Bsel], w=[db("valdbg")], owner=Bvals)
            P.barrier()
            P.emit()
            P.end_phase()

        with ExitStack() as ph:
            sbt = lambda n, s, d: ph.enter_context(nc.sbuf_tensor(P.uniq(n), s, d))
            mkp = lambda name, shape, dt, n, psum=False: TPool(P, ph, nc, name, shape, dt, n, psum)
            psF = mkp("psFe", [128, 512], F32, 6, psum=True)
            psB = mkp("psBe", [128, 1024], BF16, 2, psum=True)
            xgp = mkp("xgp", [128, NJ, D], BF16, 2)
            xgT = mkp("xgT", [128, 8, CAP], BF16, 1)
            sgp = mkp("sgp", [128, CAP], F32, 2)
            hdp = mkp("hdp", [128, 8, CAP], BF16, 2)
            ysb = mkp("ysb", [128, D], F32, 2)
            def gather(e):
                xg, Bxg = xgp.next()
                for j in range(NJ):
                    P.dma_fn("pool", (lambda xg, j, e: lambda en: en.indirect_dma_start(
                        out=xg[:, j, :], out_offset=None, in_=xn_d,
                        in_offset=bass.IndirectOffsetOnAxis(ap=idxT[:, j, e:e + 1], axis=0)))(xg, j, e),
                        r=[Bsel] + [db("xn", tt_) for tt_ in range(NT)], w=[Bxg])
                return xg, Bxg

            P.dma("pool", Wpg[:], w_pg.rearrange("(k p) c -> p k c", p=128), w=[BWf], owner=BWf)
            P.dma("pool", Wpl[:], w_ple.rearrange("(k p) c -> p k c", p=128), w=[BWf], owner=BWf)
            P.dma("sp", gple[:], gple_l, w=[BWf], owner=BWf)
            P.dma("sp", gfin[:], gfin_rep, w=[BWf], owner=BWf)
            for c in range(8):
                if c % 2 == 0:
                    P.ts("dve", Wpg[:, c, :], Wpg[:, c, :], gple[:, c:c + 1], op0=ALU.mult, r=[BWf], w=[BWf])
                else:
                    P.act(Wpg[:, c, :], Wpg[:, c, :], AF.Copy, scale=gple[:, c:c + 1], r=[BWf], w=[BWf])
            nxt_g = gather(0)
            for e in range(NEL):
                (wg, Bwg), (wu, Bwu), (wd, Bwd) = w_ready.pop(0)
                xg, Bxg = nxt_g
                if e + 1 < NEL:
                    nxt_g = gather(e + 1)
                xT_, BxT = xgT.next()
                for c in range(8):
                    pT, BpT = psB.next()
                    for j in range(NJ):
                        P.transpose(pT[:, j * 128:(j + 1) * 128], xg[:, j, c * 128:(c + 1) * 128], identb[:],
                                    r=[Bxg, Bconst], w=[BpT])
                    P.copy("dve" if c % 2 == 0 else "act", xT_[:, c, :], pT[:, 0:CAP], r=[BpT], w=[BxT])
                hd, Bhd = hdp.next()
                for fc in range(8):
                    psg, Bpsg = psF.next()
                    psu, Bpsu = psF.next()
                    for c in range(8):
                        P.matmul(psg[:, 0:CAP], wg[:, c, fc * 128:(fc + 1) * 128], xT_[:, c, :], start=(c == 0),
                                 stop=(c == 7), r=[Bwg, BxT], w=[Bpsg])
                    for c in range(8):
                        P.matmul(psu[:, 0:CAP], wu[:, c, fc * 128:(fc + 1) * 128], xT_[:, c, :], start=(c == 0),
                                 stop=(c == 7), r=[Bwu, BxT], w=[Bpsu])
                    sg, Bsg = sgp.next()
                    P.act(sg[:], psg[:, 0:CAP], AF.Silu, r=[Bpsg], w=[Bsg])
                    P.tt("dve", hd[:, fc, :], sg[:], psu[:, 0:CAP], ALU.mult, r=[Bsg, Bpsu], w=[Bhd])
                for j in range(NJ):
                    ys, Bys = ysb.next()
                    for half in range(2):
                        psy, Bpsy = psF.next()
                        for fc in range(8):
                            P.matmul(psy[:, 0:512], hd[:, fc, j * 128:(j + 1) * 128], wd[:, fc, half * 512:(half + 1) * 512],
                                     start=(fc == 0), stop=(fc == 7), r=[Bhd, Bwd], w=[Bpsy])
                        if half == 0:
                            P.ts("dve", ys[:, 0:512], psy[:, 0:512], valsT[:, j, e:e + 1], None, op0=ALU.mult,
                                 r=[Bpsy, Bsel], w=[Bys])
                        else:
                            P.act(ys[:, 512:1024], psy[:, 0:512], AF.Copy, scale=valsT[:, j, e:e + 1],
                                  r=[Bpsy, Bsel], w=[Bys])
                    P.dma_fn("pool", (lambda ys, j, e: lambda en: en.indirect_dma_start(
                        out=yacc_d, out_offset=bass.IndirectOffsetOnAxis(ap=idxT[:, j, e:e + 1], axis=0),
                        in_=ys[:], in_offset=None, compute_op=ALU.add))(ys, j, e),
                        r=[Bys, Bsel, db("yacc")], w=[db("yacc")])
                if e + 2 < NEL:
                    w_ready.append(load_w(e + 2, ("dve", "act")))
            P.barrier()
            P.emit()
            P.end_phase()

        phDE.close()
        with ExitStack() as ph:
            sbt = lambda n, s, d: ph.enter_context(nc.sbuf_tensor(P.uniq(n), s, d))
            mkp = lambda name, shape, dt, n, psum=False: TPool(P, ph, nc, name, shape, dt, n, psum)
            psF = mkp("psFf", [128, 512], F32, 6, psum=True)
            psB = mkp("psBf", [128, 1024], BF16, 2, psum=True)
            BW = BWf
            Bcc2 = P.buf("ccy")
            ridx = sbt("ridx", [128, 3, NT // 2], I32)
            Bridx = P.buf("ridx")
            P.dma("sp", ridx[:].rearrange("p a t -> p (a t)"), rowidx_in, w=[Bridx], owner=Bridx)
            for g4 in [g for k in range(NG // 2) for g in (k, NG // 2 + k)]:
                P.dma_fn("pool", (lambda g4: lambda e: e.collective_compute(
                    "AllGather", op=ALU.bypass, replica_groups=GROUPS,
                    ins=[yacc_d[g4 * 512:(g4 + 1) * 512, :]], outs=[yall_d[g4]]))(g4),
                    r=[db("yacc")], w=[db("yall", g4)], owner=P.buf(f"ccy{g4}"), inc=1)
            yall_flat = yall_d.rearrange("g r d -> (g r) d")

            def gather_rows(dst, Bdst, src2d, col, deps):
                P.dma_fn("pool", (lambda dst, col: lambda en: en.indirect_dma_start(
                    out=dst[:], out_offset=None, in_=src2d,
                    in_offset=bass.IndirectOffsetOnAxis(ap=ridx[:, col[0], col[1]:col[1] + 1], axis=0)))(dst, col),
                    r=[Bridx] + deps, w=[Bdst])

            hp_ = mkp("hp_", [128, D], F32, 4)
            y0l = mkp("y0l", [128, D], F32, 4)
            y1l = mkp("y1l", [128, D], F32, 4)
            pp_ = mkp("pp_", [128, 256], F32, 4)
            pb_ = mkp("pb_", [128, 256], BF16, 4)
            pTp = mkp("pTp", [128, 2, 128], BF16, 4)
            jk3 = mkp("jk3", [128, D], BF16, 1)
            ss3 = mkp("ss3", [128, 8], F32, 4)
            ab3 = mkp("ab3", [128, D], BF16, 4)
            aT3 = mkp("aT3", [128, D], BF16, 4)
            gtp = mkp("gtp", [128, D], F32, 4)
            h3p = mkp("h3p", [128, D], F32, 4)
            outp = mkp("outp", [128, D], F32, 4)
            def bodyF(t):
                tok = slice(t * 128, (t + 1) * 128)
                g4, i4 = t // 4, t % 4
                ydeps = [db("yall", t // 4), db("yall", NG // 2 + t // 4)]
                h2, Bh2 = hp_.next()
                gather_rows(h2, Bh2, h1_d, (0, t), [])
                y0, By0 = y0l.next()
                gather_rows(y0, By0, yall_flat, (1, t), ydeps)
                y1, By1 = y1l.next()
                gather_rows(y1, By1, yall_flat, (2, t), ydeps)
                pt_, Bp = pp_.next()
                P.dma("sp", pt_[:], pin[tok, :], w=[Bp])
                P.tt("dve", y0[:], y0[:], y1[:], ALU.add, r=[By0, By1], w=[By0])
                P.tt("dve", h2[:], h2[:], y0[:], ALU.add, r=[Bh2, By0], w=[Bh2])
                jk, Bj = jk3.next()
                ss, Bss = ss3.next()
                P.act(jk[:], h2[:], AF.Square, accum_out=ss[:, 0:1], r=[Bh2], w=[Bj, Bss])
                rstd_from_ss(ss, Bss, 0, 1, 2, 1.0 / D)
                ab, Bab = ab3.next()
                P.act(ab[:], h2[:], AF.Copy, scale=ss[:, 2:3], r=[Bh2, Bss], w=[Bab])
                yield
                pT, BpT = psB.next()
                for c in range(8):
                    P.transpose(pT[:, c * 128:(c + 1) * 128], ab[:, c * 128:(c + 1) * 128], identb[:], r=[Bab, Bconst], w=[BpT])
                aT, BaT = aT3.next()
                P.copy("dve", aT[:], pT[:], r=[BpT], w=[BaT])
                pb, Bpb = pb_.next()
                P.copy("act", pb[:], pt_[:], r=[Bp], w=[Bpb])
                pT2, BpT2 = psB.next()
                for c in range(2):
                    P.transpose(pT2[:, c * 128:(c + 1) * 128], pb[:, c * 128:(c + 1) * 128], identb[:], r=[Bpb, Bconst], w=[BpT2])
                pTt, BpTt = pTp.next()
                P.copy("dve", pTt[:].rearrange("p c s -> p (c s)"), pT2[:, 0:256], r=[BpT2], w=[BpTt])
                yield
                gt, Bgt = gtp.next()
                h3, Bh3 = h3p.next()
                for half in range(2):
                    hs = slice(half * 512, (half + 1) * 512)
                    psg, Bpsg = psF.next()
                    for c in range(8):
                        P.matmul(psg[:, 0:512], aT[:, c * 128:(c + 1) * 128], Wpg[:, c, hs], start=(c == 0), stop=(c == 7),
                                 r=[BaT, BW], w=[Bpsg])
                    P.act(gt[:, hs], psg[:, 0:512], AF.Sigmoid, r=[Bpsg], w=[Bgt])
                    psp, Bpsp = psF.next()
                    for c in range(2):
                        P.matmul(psp[:, 0:512], pTt[:, c, :], Wpl[:, c, hs], start=(c == 0), stop=(c == 1), r=[BpTt, BW], w=[Bpsp])
                    P.tt("dve", gt[:, hs], gt[:, hs], psp[:, 0:512], ALU.mult, r=[Bgt, Bpsp], w=[Bgt])
                    P.tt("dve", h3[:, hs], gt[:, hs], h2[:, hs], ALU.add, r=[Bgt, Bh2], w=[Bh3])
                yield
                jk, Bj = jk3.next()
                P.act(jk[:], h3[:], AF.Square, accum_out=ss[:, 4:5], r=[Bh3], w=[Bj, Bss])
                rstd_from_ss(ss, Bss, 4, 5, 6, 1.0 / D)
                ot, Bot = outp.next()
                P.stt(ot[:], h3[:], ss[:, 6:7], gfin[:], ALU.mult, ALU.mult, r=[Bh3, Bss, BW], w=[Bot])
                P.dma("pool", y[tok, :], ot[:], r=[Bot], w=[db("y", t)])
            run_pipelined([bodyF(t) for t in range(NT // 2)], 1)
            P.barrier()
            P.emit()
            P.end_phase()
        phFw.close()
        print("instructions recorded:", P.ninst, "sems:", len(P.sems))
    return nc


def _rowidx(S, r):
    NT = S // 128
    out = np.zeros((128, 3, NT // 2), np.int32)
    pp = np.arange(128)
    for tl in range(NT // 2):
        t = r * (NT // 2) + tl
        g4, i4 = t // 4, t % 4
        out[:, 0, tl] = t * 128 + pp
        out[:, 1, tl] = g4 * 1024 + i4 * 128 + pp
        out[:, 2, tl] = g4 * 1024 + 512 + i4 * 128 + pp
    return np.ascontiguousarray(out.reshape(128, -1))


def make_inputs(S, b, r, x, p, positions, norm_mix, w_in, gla_gate_up_fwd, gla_gate_bias_fwd, gla_gate_up_bwd,
                gla_gate_bias_bwd, attn_sink, gla_norm, w_branch_attn, w_branch_gla, w_out, norm_ffn, w_router,
                w_exp_gate, w_exp_up, w_exp_down, norm_ple, w_ple_gate, w_ple, norm_final):
    f = lambda a: np.ascontiguousarray(np.asarray(a, dtype=np.float32))
    col8 = lambda v: f(np.asarray(v).reshape(-1, 128).T)
    NT = S // 128
    W = np.asarray(w_in[0])
    o_aq, o_ak, o_av, o_gq, o_gk, o_gv, o_gr, o_zf, o_zb, o_ga, o_gg = (
        0, 512, 640, 768, 1280, 1792, 2816, 3840, 3856, 3872, 4896)
    cols = np.concatenate([
        np.arange(o_aq + 256 * r, o_aq + 256 * (r + 1)),
        np.arange(o_ak + 64 * r, o_ak + 64 * (r + 1)),
        np.arange(o_av + 64 * r, o_av + 64 * (r + 1)),
        np.arange(o_gq + 256 * r, o_gq + 256 * (r + 1)),
        np.arange(o_gk + 256 * r, o_gk + 256 * (r + 1)),
        np.arange(o_gv + 512 * r, o_gv + 512 * (r + 1)),
        np.arange(o_gr + 512 * r, o_gr + 512 * (r + 1)),
        np.arange(o_zf, o_zf + 16), np.arange(o_zb, o_zb + 16),
        np.arange(o_ga, o_ga + 1024), np.arange(o_gg, o_gg + 1024)])
    assert cols.size == NCOL
    dk = slice(256 * r, 256 * (r + 1))
    upf = np.concatenate([np.asarray(gla_gate_up_fwd[0])[:, dk], np.asarray(gla_gate_bias_fwd[0])[None, dk]], 0)
    upb = np.concatenate([np.asarray(gla_gate_up_bwd[0])[:, dk], np.asarray(gla_gate_bias_bwd[0])[None, dk]], 0)
    eperm = np.concatenate([np.arange(8 * r, 8 * (r + 1)), np.arange(8 * (1 - r), 8 * (2 - r))])
    es = slice(8 * r, 8 * (r + 1))
    return {
        "x": f(x[b]),
        "p": f(np.asarray(p[0, b])[r * (S // 2):(r + 1) * (S // 2)]),
        "rowidx": _rowidx(S, r),
        "pos_l": np.ascontiguousarray(np.asarray(positions[b], dtype=np.int32).reshape(NT, 128).T),
        "invf_rep": f(np.broadcast_to((500000.0 ** (-np.arange(0, 16, 2, dtype=np.float32) / 16.0))[None, :], (128, 8))),
        "gmix_l": col8(norm_mix[0]),
        "gple_l": col8(norm_ple[0]),
        "ggla_l": col8(np.asarray(gla_norm[0])[512 * r:512 * (r + 1)]),
        "gffn_rep": f(np.broadcast_to(np.asarray(norm_ffn[0])[None, :], (128, D))),
        "gfin_rep": f(np.broadcast_to(np.asarray(norm_final)[None, :], (128, D))),
        "sink_rep": f(np.repeat(np.asarray(attn_sink[0])[4 * r:4 * (r + 1)], 128)[None, :]),
        "upext_f": f(upf),
        "upext_b": f(upb),
        "w_in": f(W[:, cols]),
        "w_ba": f(np.asarray(w_branch_attn[0])[256 * r:256 * (r + 1)]),
        "w_bg": f(np.asarray(w_branch_gla[0])[512 * r:512 * (r + 1)]),
        "w_out": f(w_out[0]),
        "w_router": f(np.asarray(w_router[0])[:, eperm]),
        "rowmask": f(np.stack([np.arange(16) < 8, np.arange(16) >= 8], 1)),
        "w_eg": f(np.asarray(w_exp_gate[0])[es]),
        "w_eu": f(np.asarray(w_exp_up[0])[es]),
        "w_ed": f(np.asarray(w_exp_down[0])[es]),
        "w_pg": f(w_ple_gate[0]),
        "w_ple": f(w_ple[0]),
    }


def kernel(**inputs):
    x = np.asarray(inputs["x"])
    B, S, _ = x.shape
    nc = build(S)
    in_maps = []
    for c in range(8):
        in_maps.append(make_inputs(S, (c // 2) % B, c % 2, **inputs))
    res = run_bass_kernel_spmd(nc, in_maps, core_ids=list(range(8)))
    out = np.stack([np.concatenate([np.asarray(res.results[2 * b]["y"], dtype=np.float32),
                                    np.asarray(res.results[2 * b + 1]["y"], dtype=np.float32)], 0) for b in range(B)], 0)
    return out
```
